# Optimizing a Trainium2 kernel written in Bass

```python
import math
import jax, jax.numpy as jnp
from jax import lax
import numpy as np

D_MODEL = 1024
BATCH = 8
SEQ = 4096
DEPTH = 2

N_META = 16
BLOCK = 128
N_PAD = BLOCK - N_META
NEG_INF = -1e30

FOX_HEADS = 8
FOX_HEAD_DIM = 64
FOX_WIDTH = FOX_HEADS * FOX_HEAD_DIM

MLA_HEADS = 8
MLA_NOPE_DIM = 64
MLA_ROPE_DIM = 32
MLA_V_DIM = 64
MLA_Q_RANK = 384
MLA_KV_RANK = 256
MLA_WIDTH = MLA_HEADS * MLA_V_DIM
ROPE_THETA = 10000.0

N_EXPERTS = 16
N_GROUPS = 4
EXPERTS_PER_GROUP = N_EXPERTS // N_GROUPS
TOP_K = 2
D_EXPERT = 256

LN_EPS = 1e-5
RMS_EPS = 1e-6
ALPHA = (2 * DEPTH) ** 0.25
BETA = (8 * DEPTH) ** -0.25

OFF_FOX_Q = FOX_WIDTH
OFF_FOX_K = OFF_FOX_Q + FOX_WIDTH
OFF_FOX_V = OFF_FOX_K + FOX_WIDTH
OFF_FOX_F = OFF_FOX_V + FOX_HEADS
OFF_MLA_CQ = OFF_FOX_F + MLA_Q_RANK
OFF_MLA_CKV = OFF_MLA_CQ + MLA_KV_RANK
OFF_MLA_KR = OFF_MLA_CKV + MLA_ROPE_DIM
OFF_G_FOX = OFF_MLA_KR + D_MODEL
IN_COLS = OFF_G_FOX + D_MODEL
SPLITS = (OFF_FOX_Q, OFF_FOX_K, OFF_FOX_V, OFF_FOX_F, OFF_MLA_CQ, OFF_MLA_CKV, OFF_MLA_KR, OFF_G_FOX)

kernel_name = "fox_mla_gated_hybrid_moe_deepnorm"


def layer_norm(x, g, b):
    xf = x.astype(jnp.float32)
    mu = jnp.mean(xf, axis=-1, keepdims=True)
    var = jnp.mean(jnp.square(xf - mu), axis=-1, keepdims=True)
    return ((xf - mu) * lax.rsqrt(var + LN_EPS) * g.astype(jnp.float32) + b.astype(jnp.float32)).astype(x.dtype)


def rms_norm(x, g):
    xf = x.astype(jnp.float32)
    ms = jnp.mean(jnp.square(xf), axis=-1, keepdims=True)
    return (xf * lax.rsqrt(ms + RMS_EPS) * g.astype(jnp.float32)).astype(x.dtype)


def rope(x, pos):
    half = x.shape[-1] // 2
    inv = ROPE_THETA ** (-jnp.arange(half, dtype=jnp.float32) / half)
    ang = pos.astype(jnp.float32)[:, None] * inv[None, :]
    cos = jnp.cos(ang)[:, None, :]
    sin = jnp.sin(ang)[:, None, :]
    xf = x.astype(jnp.float32)
    x1, x2 = xf[..., :half], xf[..., half:]
    return jnp.concatenate([x1 * cos - x2 * sin, x2 * cos + x1 * sin], axis=-1).astype(x.dtype)


def blocked_causal_attention(q, k, v, scale, decay=None):
    lp = q.shape[2]
    outs = []
    for i in range(lp // BLOCK):
        q0, q1 = i * BLOCK, (i + 1) * BLOCK
        s = jnp.einsum("bhqd,bhkd->bhqk", q[:, :, q0:q1], k[:, :, :q1],
                       preferred_element_type=jnp.float32) * scale
        if decay is not None:
            s = s + decay[:, :, q0:q1, None] - decay[:, :, None, :q1]
        qi = jnp.arange(q0, q1)[:, None]
        ki = jnp.arange(q1)[None, :]
        s = jnp.where((ki <= qi) & (ki >= N_PAD), s, NEG_INF)
        p = jax.nn.softmax(s, axis=-1)
        outs.append(jnp.einsum("bhqk,bhkd->bhqd", p.astype(v.dtype), v[:, :, :q1]))
    return jnp.concatenate(outs, axis=2)


def _pad_seq(t):
    widths = [(0, 0)] * t.ndim
    widths[2] = (N_PAD, 0)
    return jnp.pad(t, widths)


def hybrid_mixer(h, pos, w_in, fox_f_bias, fox_w_o, mla_q_norm, mla_w_uq, mla_kv_norm, mla_w_ukv,
                 mla_w_o, w_out):
    b, l, _ = h.shape
    proj = h @ w_in
    q_f, k_f, v_f, f_logit, c_q, c_kv, k_r, g_fox, g_mla = jnp.split(proj, SPLITS, axis=-1)

    def to_heads(t, n_heads):
        return t.reshape(b, l, n_heads, -1).transpose(0, 2, 1, 3)

    log_f = jax.nn.log_sigmoid(f_logit.astype(jnp.float32) + fox_f_bias.astype(jnp.float32))
    decay = jnp.cumsum(log_f, axis=1).transpose(0, 2, 1)
    decay = jnp.pad(decay, ((0, 0), (0, 0), (N_PAD, 0)))
    o_fox = blocked_causal_attention(_pad_seq(to_heads(q_f, FOX_HEADS)), _pad_seq(to_heads(k_f, FOX_HEADS)),
                                     _pad_seq(to_heads(v_f, FOX_HEADS)), FOX_HEAD_DIM ** -0.5, decay)
    o_fox = o_fox[:, :, N_PAD:].transpose(0, 2, 1, 3).reshape(b, l, FOX_WIDTH)
    y_fox = o_fox @ fox_w_o

    q_all = (rms_norm(c_q, mla_q_norm) @ mla_w_uq).reshape(b, l, MLA_HEADS, MLA_NOPE_DIM + MLA_ROPE_DIM)
    q_nope, q_rot = q_all[..., :MLA_NOPE_DIM], rope(q_all[..., MLA_NOPE_DIM:], pos)
    kv = (rms_norm(c_kv, mla_kv_norm) @ mla_w_ukv).reshape(b, l, MLA_HEADS, MLA_NOPE_DIM + MLA_V_DIM)
    k_nope, v_m = kv[..., :MLA_NOPE_DIM], kv[..., MLA_NOPE_DIM:]
    k_rot = jnp.broadcast_to(rope(k_r[:, :, None, :], pos), (b, l, MLA_HEADS, MLA_ROPE_DIM))
    q_m = jnp.concatenate([q_nope, q_rot], axis=-1).transpose(0, 2, 1, 3)
    k_m = jnp.concatenate([k_nope, k_rot], axis=-1).transpose(0, 2, 1, 3)
    v_m = v_m.transpose(0, 2, 1, 3)
    o_mla = blocked_causal_attention(_pad_seq(q_m), _pad_seq(k_m), _pad_seq(v_m),
                                     (MLA_NOPE_DIM + MLA_ROPE_DIM) ** -0.5)
    o_mla = o_mla[:, :, N_PAD:].transpose(0, 2, 1, 3).reshape(b, l, MLA_WIDTH)
    y_mla = o_mla @ mla_w_o

    merged = jax.nn.sigmoid(g_fox) * y_fox + jax.nn.sigmoid(g_mla) * y_mla
    return merged @ w_out


def grouped_moe(x, router_w, router_b, w_gate, w_up, w_down):
    b, l, d = x.shape
    t = x.reshape(b * l, d)
    scores = jax.nn.sigmoid((t @ router_w).astype(jnp.float32))
    biased = (scores + router_b.astype(jnp.float32)).reshape(-1, N_GROUPS, EXPERTS_PER_GROUP)
    group_score = jnp.sum(lax.top_k(biased, TOP_K)[0], axis=-1)
    g_sel = jnp.argmax(group_score, axis=-1)
    in_group = jnp.take_along_axis(biased, g_sel[:, None, None], axis=1)[:, 0]
    _, local = lax.top_k(in_group, TOP_K)
    expert_idx = g_sel[:, None] * EXPERTS_PER_GROUP + local
    sel = jnp.take_along_axis(scores, expert_idx, axis=1)
    gates = sel / jnp.sum(sel, axis=-1, keepdims=True)
    combine = jnp.sum(jax.nn.one_hot(expert_idx, N_EXPERTS, dtype=jnp.float32) * gates[..., None], axis=1)
    combine = combine.astype(t.dtype)
    y = jnp.zeros_like(t)
    for e in range(N_EXPERTS):
        hid = jax.nn.silu(t @ w_gate[e]) * (t @ w_up[e])
        y = y + combine[:, e:e + 1] * (hid @ w_down[e])
    return y.reshape(b, l, d)


def setup_inputs(seed: int = 0) -> dict:
    key = jax.random.key(seed)
    ks = jax.random.split(key, 24)
    f32 = jnp.float32
    nrm = lambda k, shape, scale: jax.random.normal(k, shape, f32) * scale
    gain = lambda k, shape: 1.0 + 0.02 * jax.random.normal(k, shape, f32)
    return {
        "x": jax.random.normal(ks[0], (BATCH, SEQ, D_MODEL), f32),
        "meta_tokens": nrm(ks[1], (N_META, D_MODEL), 1.0),
        "ln_in_g": gain(ks[2], (D_MODEL,)),
        "ln_in_b": nrm(ks[3], (D_MODEL,), 0.02),
        "w_in": nrm(ks[4], (DEPTH, D_MODEL, IN_COLS), D_MODEL ** -0.5),
        "fox_f_bias": 2.0 + nrm(ks[5], (DEPTH, FOX_HEADS), 0.1),
        "fox_w_o": nrm(ks[6], (DEPTH, FOX_WIDTH, D_MODEL), BETA * FOX_WIDTH ** -0.5),
        "mla_q_norm": gain(ks[7], (DEPTH, MLA_Q_RANK)),
        "mla_w_uq": nrm(ks[8], (DEPTH, MLA_Q_RANK, MLA_HEADS * (MLA_NOPE_DIM + MLA_ROPE_DIM)), MLA_Q_RANK ** -0.5),
        "mla_kv_norm": gain(ks[9], (DEPTH, MLA_KV_RANK)),
        "mla_w_ukv": nrm(ks[10], (DEPTH, MLA_KV_RANK, MLA_HEADS * (MLA_NOPE_DIM + MLA_V_DIM)), MLA_KV_RANK ** -0.5),
        "mla_w_o": nrm(ks[11], (DEPTH, MLA_WIDTH, D_MODEL), BETA * MLA_WIDTH ** -0.5),
        "w_out": nrm(ks[12], (DEPTH, D_MODEL, D_MODEL), BETA * D_MODEL ** -0.5),
        "ln1_g": gain(ks[13], (DEPTH, D_MODEL)),
        "ln1_b": nrm(ks[14], (DEPTH, D_MODEL), 0.02),
        "router_w": nrm(ks[15], (D_MODEL, N_EXPERTS), D_MODEL ** -0.5),
        "router_b": nrm(ks[16], (N_EXPERTS,), 0.01),
        "w_gate": nrm(ks[17], (DEPTH, N_EXPERTS, D_MODEL, D_EXPERT), D_MODEL ** -0.5),
        "w_up": nrm(ks[18], (DEPTH, N_EXPERTS, D_MODEL, D_EXPERT), D_MODEL ** -0.5),
        "w_down": nrm(ks[19], (DEPTH, N_EXPERTS, D_EXPERT, D_MODEL), BETA * D_EXPERT ** -0.5),
        "ln2_g": gain(ks[20], (DEPTH, D_MODEL)),
        "ln2_b": nrm(ks[21], (DEPTH, D_MODEL), 0.02),
    }


def reference(x, meta_tokens, ln_in_g, ln_in_b, w_in, fox_f_bias, fox_w_o, mla_q_norm, mla_w_uq,
              mla_kv_norm, mla_w_ukv, mla_w_o, w_out, ln1_g, ln1_b, router_w, router_b,
              w_gate, w_up, w_down, ln2_g, ln2_b):
    b = x.shape[0]
    meta = jnp.broadcast_to(meta_tokens[None].astype(x.dtype), (b, N_META, D_MODEL))
    h = jnp.concatenate([meta, x], axis=1)
    pos = jnp.arange(h.shape[1], dtype=jnp.int32)
    h = layer_norm(h, ln_in_g, ln_in_b)
    for i in range(DEPTH):
        mix = hybrid_mixer(h, pos, w_in[i], fox_f_bias[i], fox_w_o[i], mla_q_norm[i], mla_w_uq[i],
                           mla_kv_norm[i], mla_w_ukv[i], mla_w_o[i], w_out[i])
        h = layer_norm(ALPHA * h + mix, ln1_g[i], ln1_b[i])
        ffn = grouped_moe(h, router_w, router_b, w_gate[i], w_up[i], w_down[i])
        h = layer_norm(ALPHA * h + ffn, ln2_g[i], ln2_b[i])
    return h[:, N_META:]
```

```python
import numpy as np
import concourse.bass as bass
import concourse.mybir as mybir
from concourse.bass_utils import run_bass_kernel_spmd

F32 = mybir.dt.float32
BF16 = mybir.dt.bfloat16
AF = mybir.ActivationFunctionType
ALU = mybir.AluOpType
AX = mybir.AxisListType

D = 1024
KC = 8
SEQ = 4096
NMETA = 16
NPAD = 112
T = SEQ + 128
NT = T // 128
DEPTH = 2
NE = 16
DE = 256
IN_COLS = 4264
ALPHA = (2 * DEPTH) ** 0.25
LN_EPS = 1e-5
RMS_EPS = 1e-6
NEG = -30000.0
GROUPS = [(g * 512, min(512, T - g * 512)) for g in range((T + 511) // 512)]
AGROUPS = [(0, 128)] + [(128 + 512 * g, 512) for g in range(8)]
OQ, OK_, OV, OF, OCQ, OCKV, OKR, OGF, OGM = 0, 512, 1024, 1536, 1544, 1928, 2184, 2216, 3240


class Op:
    __slots__ = ("eng", "meth", "args", "kw", "dma", "idx", "marked", "waits", "dsem", "dval", "ticket")

    def __init__(self, eng, meth, args, kw, dma):
        self.eng, self.meth, self.args, self.kw, self.dma = eng, meth, args, kw, dma
        self.marked = False
        self.waits = []
        self.dsem = None
        self.dval = 0


class Rec:
    ENGS = ("pe", "act", "dve", "pool", "sp")
    NQ = 12

    def __init__(self):
        self.ops = {e: [] for e in self.ENGS}
        self.state = {}
        self.seen = {e: {p: -1 for p in self.ENGS} for e in self.ENGS}
        self.seen_dma = {e: {} for e in self.ENGS}
        self.ndma = {e: 0 for e in self.ENGS}
        self.out_dmas = []
        self.pending = {}

    def _st(self, key):
        buf, sub = key if isinstance(key, tuple) else (key, None)
        d = self.state.setdefault(buf, {})
        return buf, sub, d

    def _deps(self, reads, writes):
        deps = []
        for key in reads:
            buf, sub, d = self._st(key)
            subs = list(d.keys()) if sub is None else [s for s in (sub, None) if s in d]
            for s in subs:
                if d[s][0] is not None:
                    deps.append(d[s][0])
        for key in writes:
            buf, sub, d = self._st(key)
            subs = list(d.keys()) if sub is None else [s for s in (sub, None) if s in d]
            for s in subs:
                if d[s][0] is not None:
                    deps.append(d[s][0])
                deps.extend(d[s][1])
        return deps

    def _update(self, op, reads, writes):
        for key in reads:
            buf, sub, d = self._st(key)
            if sub is None:
                for s in list(d.keys()):
                    d[s][1].append(op)
                d.setdefault(None, [None, []])
                if op not in d[None][1]:
                    d[None][1].append(op)
            else:
                d.setdefault(sub, [None, []])[1].append(op)
        for key in writes:
            buf, sub, d = self._st(key)
            if sub is None:
                d.clear()
                d[None] = [op, []]
            else:
                d[sub] = [op, []]

    def op(self, eng, meth, *args, r=(), w=(), dma=False, final=False, **kw):
        o = Op(eng, meth, args, kw, dma)
        lst = self.ops[eng]
        o.idx = len(lst)
        if self.pending.get(eng):
            o.waits.extend(self.pending[eng])
            self.pending[eng] = []
        for dpo in self._deps(r, w):
            if dpo is o:
                continue
            if dpo.dma:
                sd = self.seen_dma[eng]
                if sd.get(dpo.dsem, 0) >= dpo.dval:
                    continue
                sd[dpo.dsem] = dpo.dval
                o.waits.append(dpo)
            else:
                if dpo.eng == eng and eng in ("pe", "sp"):
                    continue
                if self.seen[eng][dpo.eng] >= dpo.idx:
                    continue
                self.seen[eng][dpo.eng] = dpo.idx
                dpo.marked = True
                o.waits.append(dpo)
        if dma:
            j = self.ndma[eng]
            self.ndma[eng] = j + 1
            o.dsem = (eng, j % self.NQ)
            o.dval = 16 * (j // self.NQ + 1)
            prev = 16 * (j // self.NQ)
            if prev > 0 and self.seen_dma[eng].get(o.dsem, 0) < prev:
                self.seen_dma[eng][o.dsem] = prev
                o.waits.append(("dma", o.dsem, prev))
            if final:
                self.out_dmas.append(o)
        lst.append(o)
        self._update(o, r, w)
        return o

    def barrier(self):
        lastop = {}
        for e in self.ENGS:
            for o in reversed(self.ops[e]):
                if not o.dma:
                    lastop[e] = o
                    break
        dmas = {}
        for e in self.ENGS:
            for o in self.ops[e]:
                if o.dma:
                    dmas[o.dsem] = max(dmas.get(o.dsem, 0), o.dval)
        for e in self.ENGS:
            pw = self.pending.setdefault(e, [])
            for p, o in lastop.items():
                if p == e and e in ("pe", "sp"):
                    continue
                if self.seen[e][p] >= o.idx:
                    continue
                self.seen[e][p] = o.idx
                o.marked = True
                pw.append(o)
            for ds, v in dmas.items():
                if self.seen_dma[e].get(ds, 0) < v:
                    self.seen_dma[e][ds] = v
                    pw.append(("dma", ds, v))
        self.state = {}

    def replay(self, nc, sems, dsems):
        engs = {"pe": "tensor", "act": "scalar", "dve": "vector", "pool": "gpsimd", "sp": "sync"}
        for e in self.ENGS:
            t = 0
            for o in self.ops[e]:
                if o.marked:
                    t += 1
                o.ticket = t
        with nc.Block() as block:
            for e in self.ENGS:
                ops = self.ops[e]
                last = (e == "sp")
                outs = self.out_dmas

                def body(eng, ops=ops, e=e, last=last):
                    for o in ops:
                        for wdep in o.waits:
                            if isinstance(wdep, tuple):
                                eng.wait_ge(dsems[wdep[1]], wdep[2])
                            elif wdep.dma:
                                eng.wait_ge(dsems[wdep.dsem], wdep.dval)
                            else:
                                eng.wait_ge(sems[wdep.eng], wdep.ticket)
                        ins = getattr(eng, o.meth)(*o.args, **o.kw)
                        if o.dma:
                            ins.then_inc(dsems[o.dsem], 16)
                        elif o.marked:
                            ins.then_inc(sems[o.eng], 1)
                    if last:
                        done = {}
                        for o in outs:
                            done[o.dsem] = max(done.get(o.dsem, 0), o.dval)
                        for s, v in done.items():
                            eng.wait_ge(dsems[s], v)

                getattr(block, engs[e])(body)


class K:
    def __init__(self, upto=99, debug=False):
        self.upto = upto
        self.debug = debug
        self.nc = nc = bass.Bass("TRN2", target_bir_lowering=False)
        self.R = Rec()
        self.sb_off = 17536
        self.ph_off = None
        self.nalloc = 0
        self.inp = {}
        self.tcount = 0

    def dram_in(self, name, shape, dt=F32):
        h = self.nc.dram_tensor(name, list(shape), dt, kind="ExternalInput")
        self.inp[name] = h
        return h.ap()

    def dram_out(self, name, shape, dt=F32):
        return self.nc.dram_tensor(name, list(shape), dt, kind="ExternalOutput").ap()

    def dram(self, name, shape, dt):
        return self.nc.dram_tensor(name, list(shape), dt).ap()

    def sb(self, name, shape, dt):
        return self._at(name, shape, dt, "persist")

    def _at(self, name, shape, dt, region):
        esz = {F32: 4, BF16: 2}[dt]
        nbytes = int(np.prod(shape[1:])) * esz
        nbytes = (nbytes + 63) // 64 * 64
        if region == "persist":
            off = self.sb_off
            self.sb_off += nbytes
            assert self.ph_off is None, "persistent alloc after phase alloc"
        else:
            if self.ph_off is None:
                self.ph_off = self.sb_off
            off = self.ph_off
            self.ph_off += nbytes
        assert off + nbytes <= 228000, ("SBUF overflow", name, off + nbytes)
        self.nalloc += 1
        return self.nc.alloc_sbuf_tensor_at("%s_%d" % (name, self.nalloc), list(shape), dt, offset=off)

    def psb(self, name, shape, dt):
        return self._at(name, shape, dt, "phase")

    def new_phase(self):
        self.R.barrier()
        self.ph_off = self.sb_off

    def ps(self, name, shape, dt=F32):
        return self.nc.alloc_psum_tensor(name, list(shape), dt)

    def pe(self, meth, *a, **k):
        return self.R.op("pe", meth, *a, **k)

    def act(self, meth, *a, **k):
        return self.R.op("act", meth, *a, **k)

    def dve(self, meth, *a, **k):
        return self.R.op("dve", meth, *a, **k)

    def pool(self, meth, *a, **k):
        return self.R.op("pool", meth, *a, **k)

    def dma(self, out, in_, r=(), w=(), cast=False, final=False):
        eng = "pool" if cast else "sp"
        return self.R.op(eng, "dma_start", out=out, in_=in_, r=r, w=w, dma=True, final=final)


def bcast_row(ap_1d, n, parts=128):
    return bass.AP(ap_1d.tensor, ap_1d.offset, [[0, parts], [1, n]])


class Ctx:
    pass


def setup(k):
    nc = k.nc
    C = Ctx()
    k.C = C
    C.x = k.dram_in("x", [T, D])
    C.ln_in_g = k.dram_in("ln_in_g", [D]); C.ln_in_b = k.dram_in("ln_in_b", [D])
    C.w_in = k.dram_in("w_in", [DEPTH, D, IN_COLS])
    C.fox_f_bias = k.dram_in("fox_f_bias", [DEPTH, 8])
    C.fox_w_o = k.dram_in("fox_w_o", [DEPTH, 512, D])
    C.mla_q_norm = k.dram_in("mla_q_norm", [DEPTH, 384])
    C.mla_w_uq = k.dram_in("mla_w_uq", [DEPTH, 384, 768])
    C.mla_kv_norm = k.dram_in("mla_kv_norm", [DEPTH, 256])
    C.mla_w_ukv = k.dram_in("mla_w_ukv", [DEPTH, 256, 1024])
    C.mla_w_o = k.dram_in("mla_w_o", [DEPTH, 512, D])
    C.w_out = k.dram_in("w_out", [DEPTH, D, D])
    C.ln1_g = k.dram_in("ln1_g", [DEPTH, D]); C.ln1_b = k.dram_in("ln1_b", [DEPTH, D])
    C.router_w = k.dram_in("router_w", [D, NE]); C.router_b = k.dram_in("router_b", [NE])
    C.w_gate = k.dram_in("w_gate", [DEPTH, NE, D, DE])
    C.w_up = k.dram_in("w_up", [DEPTH, NE, D, DE])
    C.w_down = k.dram_in("w_down", [DEPTH, NE, DE, D])
    C.ln2_g = k.dram_in("ln2_g", [DEPTH, D]); C.ln2_b = k.dram_in("ln2_b", [DEPTH, D])
    C.c_ident = k.dram_in("c_ident", [128, 128])
    C.c_cmask = k.dram_in("c_cmask", [128, 128])
    C.c_pmask = k.dram_in("c_pmask", [128, 512])
    C.c_cmask0 = k.dram_in("c_cmask0", [128, 128])
    C.c_ut = k.dram_in("c_ut", [128, 128])
    C.c_padcol = k.dram_in("c_padcol", [128, 1])
    C.c_sel = k.dram_in("c_sel", [48, NE * 128])
    C.c_ones8 = k.dram_in("c_ones8", [8, T])
    C.c_cs = k.dram_in("c_cs", [128, T])
    C.out = k.dram_out("out", [SEQ, D])
    C.H = [k.dram("Hs%d" % i, [T, D], F32) for i in range(2)]
    C.QTf = k.dram("QTf", [8, 68, T], BF16); C.KTf = k.dram("KTf", [8, 68, T], BF16)
    C.QTm = k.dram("QTm", [8, 96, T], BF16); C.KTm = k.dram("KTm", [8, 96, T], BF16)
    C.Vf = k.dram("Vf", [8, 128, NT, 128], BF16); C.Vm = k.dram("Vm", [8, 128, NT, 128], BF16)
    C.OTf = k.dram("OTf", [128, 4, T], BF16); C.OTm = k.dram("OTm", [128, 4, T], BF16)
    C.psall = k.ps("psall", [128, 8, 512], F32)
    C.bank = [C.psall[:, i, :] for i in range(8)]
    C.hT = k.sb("hT", [128, KC, T], BF16)
    C.identb = k.sb("identb", [128, 128], BF16)
    C.identf = k.sb("identf", [128, 128], F32)
    C.padcol = k.sb("padcol", [128, 1], F32)
    C.onesf = k.sb("onesf", [128, 128], F32)
    C.utf = k.sb("utf", [128, 128], F32)
    C.cmask = k.sb("cmask", [128, 128], BF16)
    C.pmask = k.sb("pmask", [128, 512], BF16)
    C.cmask0 = k.sb("cmask0", [128, 128], BF16)
    C.ln_n = 0
    C.nhalf = k.sb("nhalf", [128, 2], F32)
    k.pool("memset", C.nhalf[:], -0.5, w=["nhalf"])
    k.dma(C.utf[:], C.c_ut, w=["utf"])
    k.dma(C.cmask[:], C.c_cmask, w=["cmask"], cast=True)
    k.dma(C.pmask[:], C.c_pmask, w=["pmask"], cast=True)
    k.dma(C.cmask0[:], C.c_cmask0, w=["cmask0"], cast=True)
    k.pool("memset", C.onesf[:], 1.0, w=["onesf"])
    for row in (64, 65, 66):
        k.dma(C.QTf[:, row, :], C.c_ones8, w=[("QTf", "ones%d" % row)], cast=True)
    k.dma(C.KTf[:, 67, :], C.c_ones8, w=[("KTf", "ones")], cast=True)
    k.dma(C.identb[:], C.c_ident, w=["identb"], cast=True)
    k.dma(C.identf[:], C.c_ident, w=["identf"])
    k.dma(C.padcol[:], C.c_padcol, w=["padcol"])
    return C


NLN = 4


def alloc_ln(k):
    C = k.C
    C.lnx = [k.psb("lnx%d" % i, [128, D], F32) for i in range(NLN)]
    C.hb = [k.psb("hb%d" % i, [128, D], BF16) for i in range(NLN)]
    C.lst = [k.psb("lst%d" % i, [128, 12], F32) for i in range(NLN)]
    C.lmv = [k.psb("lmv%d" % i, [128, 8], F32) for i in range(NLN)]
    C.gbc = k.psb("gbc", [128, D], F32); C.bbc = k.psb("bbc", [128, D], F32)
    C.ln_pending = []


def load_ln_params(k, g1d, b1d):
    C = k.C
    k.dma(C.gbc[:], bcast_row(g1d, D), w=["gbc"])
    k.dma(C.bbc[:], bcast_row(b1d, D), w=["bbc"])


def ln_slots(k, n):
    C = k.C
    ln_flush(k, 0)
    return list(range(n))


def ln_flush(k, keep=0):
    C = k.C
    while len(C.ln_pending) > keep:
        C.ln_pending.pop(0)()


def ln_batch(k, items):
    C = k.C
    def names(s):
        return C.lnx[s], C.lst[s], C.lmv[s], C.hb[s], ("lnx", s), ("lst", s), ("lmv", s), ("hb", s)
    for (s, i, out_ap, outkey, final) in items:
        x, st, mv, hb, X, ST, MV, HB = names(s)
        k.dve("bn_stats", out=st[:, 0:6], in_=x[:, 0:512], r=[X], w=[ST])
        k.dve("bn_stats", out=st[:, 6:12], in_=x[:, 512:1024], r=[X], w=[ST])
        k.dve("bn_aggr", out=mv[:, 0:2], in_=st[:, 0:12], r=[ST], w=[MV])
    for (s, i, out_ap, outkey, final) in items:
        x, st, mv, hb, X, ST, MV, HB = names(s)
        k.pool("tensor_scalar", out=mv[:, 2:3], in0=mv[:, 1:2], scalar1=1.0, scalar2=LN_EPS, op0=ALU.mult, op1=ALU.add, r=[MV], w=[MV])
        k.pool("tensor_tensor", out=mv[:, 4:5], in0=mv[:, 2:3], in1=C.nhalf[:, 0:1], op=ALU.pow, r=[MV, "nhalf"], w=[MV])
    for (s, i, out_ap, outkey, final) in items:
        x, st, mv, hb, X, ST, MV, HB = names(s)
        k.dve("scalar_tensor_tensor", out=mv[:, 5:6], in0=mv[:, 0:1], scalar=-1.0, in1=mv[:, 4:5],
              op0=ALU.mult, op1=ALU.mult, r=[MV], w=[MV])
    for (s, i, out_ap, outkey, final) in items:
        x, st, mv, hb, X, ST, MV, HB = names(s)
        k.act("activation", out=x[:], in_=x[:], func=AF.Identity, scale=mv[:, 4:5], bias=mv[:, 5:6], r=[X, MV], w=[X])
    for (s, i, out_ap, outkey, final) in items:
        x, st, mv, hb, X, ST, MV, HB = names(s)
        k.dve("tensor_tensor", out=x[:], in0=x[:], in1=C.gbc[:], op=ALU.mult, r=[X, "gbc"], w=[X])
    for (s, i, out_ap, outkey, final) in items:
        x, st, mv, hb, X, ST, MV, HB = names(s)
        k.pool("tensor_tensor", out=x[:], in0=x[:], in1=C.bbc[:], op=ALU.add, r=[X, "bbc"], w=[X])
    for (s, i, out_ap, outkey, final) in items:
        x, st, mv, hb, X, ST, MV, HB = names(s)
        k.act("activation", out=hb[:], in_=x[:], func=AF.Copy, r=[X], w=[HB])
        k.dma(out_ap, x[:], r=[X], w=[outkey], final=final)
        C.ln_n += 1

        def deferred(i=i, hb=hb, HB=HB):
            bk = 6 + (i % 2)
            pst = C.bank[bk][:].bitcast(BF16)
            for kc in range(KC):
                k.pe("transpose", out=pst[:, kc * 128:(kc + 1) * 128], in_=hb[:, kc * 128:(kc + 1) * 128],
                     identity=C.identb[:], r=[HB, "identb"], w=[("bank", bk)])
            k.act("activation", out=C.hT[:, :, i * 128:(i + 1) * 128],
                  in_=pst.rearrange("p (k t) -> p k t", k=KC), func=AF.Copy,
                  r=[("bank", bk)], w=[("hT", i)])
        C.ln_pending.append(deferred)


def phase0(k):
    C = k.C
    k.new_phase()
    alloc_ln(k)
    load_ln_params(k, C.ln_in_g, C.ln_in_b)
    for i0 in range(0, NT, NLN):
        tiles = list(range(i0, min(NT, i0 + NLN)))
        sl = ln_slots(k, len(tiles))
        items = []
        for s, i in zip(sl, tiles):
            k.dma(C.lnx[s][:], C.x[i * 128:(i + 1) * 128, :], w=[("lnx", s)])
            items.append((s, i, C.H[0][i * 128:(i + 1) * 128, :], ("H0", i), False))
        ln_batch(k, items)
    ln_flush(k)


def evac_v(k, src_bank_ap, vst, s, bankkey, dst_dram, i):
    C = k.C
    sv = src_bank_ap.rearrange("p (m two d) -> p m two d", two=2, d=64)
    k.act("activation", out=vst[:, :, 0, 0:64], in_=sv[:, :, 0, :], func=AF.Copy, r=[bankkey], w=[("vst", s)])
    k.dve("tensor_copy", out=vst[:, :, 1, 64:128], in_=sv[:, :, 1, :], r=[bankkey], w=[("vst", s)])
    k.dma(dst_dram[:, :, i, :].rearrange("h p d -> p h d"), vst[:].rearrange("p m two d -> p (m two) d"),
          r=[("vst", s)], w=[("V", i)])


def phase1(k, l, with_p0=False):
    C = k.C
    k.new_phase()
    win = C.w_in[l].rearrange("(kc p) c -> p kc c", p=128)
    wv = k.psb("wv", [128, KC, 512], BF16)
    wcqf = k.psb("wcqf", [128, KC, 392], BF16)
    wckv = k.psb("wckv", [128, KC, 256], BF16)
    wt = [k.psb("wt%d" % i, [128, KC, 128], BF16) for i in range(2)]
    wkr = k.psb("wkr", [128, KC, 32], BF16); wkrR = k.psb("wkrR", [128, KC, 32], BF16)
    vst = [k.psb("vst%d" % i, [128, 4, 2, 128], BF16) for i in range(2)]
    cqnT = k.psb("cqnT", [128, 3, T], BF16); ckvnT = k.psb("ckvnT", [128, 2, T], BF16)
    augT = k.psb("augT", [32, T], BF16)
    fbias = k.psb("fbias", [128, 8], F32)
    sm = [k.psb("sm%d" % i, [128, 64], F32) for i in range(2)]
    junk = k.psb("junk", [128, 384], BF16)
    cqn = [k.psb("cqn%d" % i, [128, 640], BF16) for i in range(2)]
    stg = [k.psb("stg%d" % i, [128, 512], BF16) for i in range(3)]
    rt = [k.psb("rt%d" % i, [128, 512], F32) for i in range(2)]
    gq = k.psb("gq", [128, 8], F32)
    z_all = k.psb("z_all", [128, NT, 8], F32)
    lf_all = k.psb("lf_all", [128, NT, 8], F32)
    carry = k.psb("carry", [128, NT, 8], F32)
    aug_all = k.psb("aug_all", [128, NT, 32], F32)
    hb_all = k.psb("hb_all", [128, NT, 8], BF16)
    ov_off = k.ph_off
    cs = k.psb("cs", [128, T], F32)
    wuq = k.psb("wuq", [128, 3, 768], BF16); wuqR = k.psb("wuqR", [128, 3, 768], BF16)
    wukv = k.psb("wukv", [128, 2, 1024], BF16)
    stgw = k.psb("stgw", [128, 1024], F32)
    ALIAS = ["lnx", "hb", "lst", "lmv", "gbc", "bbc"] if with_p0 else []

    k.dma(wv[:], win[:, :, OV:OV + 512], w=["wv"], cast=True)
    k.dma(wcqf[:], win[:, :, OF:OF + 392], w=["wcqf"], cast=True)
    k.dma(wckv[:], win[:, :, OCKV:OCKV + 256], w=["wckv"], cast=True)
    k.dma(wkr[:], win[:, :, OKR:OKR + 32], w=["wkr"], cast=True)
    k.dma(fbias[:], bcast_row(C.fox_f_bias[l], 8), w=["fbias"])
    for kc in range(3):
        k.dma(gq[:, kc:kc + 1], C.mla_q_norm[l][kc * 128:(kc + 1) * 128].rearrange("(p o) -> p o", o=1), w=["gq"])
    for kc in range(2):
        k.dma(gq[:, 4 + kc:5 + kc], C.mla_kv_norm[l][kc * 128:(kc + 1) * 128].rearrange("(p o) -> p o", o=1), w=["gq"])
    for s_ in range(2):
        k.pool("memset", vst[s_][:, :, 0, 64:128], 1.0, w=[("vst", s_)])
        k.pool("memset", vst[s_][:, :, 1, 0:64], 1.0, w=[("vst", s_)])
    k.dve("tensor_scalar", out=wkrR[:, :, 0:16], in0=wkr[:, :, 16:32], scalar1=-1.0, scalar2=None, op0=ALU.mult, r=["wkr"], w=["wkrR"])
    k.dve("tensor_copy", out=wkrR[:, :, 16:32], in_=wkr[:, :, 0:16], r=["wkr"], w=["wkrR"])

    def mm_tile(i):
        s_ = i % 2
        tc = slice(i * 128, (i + 1) * 128)
        b0 = 3 * s_
        for (bk, w_, wn, ncol) in ((b0, wv, "wv", 512), (b0 + 1, wcqf, "wcqf", 392), (b0 + 2, wckv, "wckv", 256)):
            for kc in range(KC):
                k.pe("matmul", C.bank[bk][:, 0:ncol], lhsT=C.hT[:, kc, tc], rhs=w_[:, kc, :], start=(kc == 0), stop=(kc == KC - 1),
                     r=[("hT", i), wn], w=[("bank", bk)])

    def chain_tile(i):
        s_ = i % 2
        tc = slice(i * 128, (i + 1) * 128)
        b0 = 3 * s_
        bv, ba, bb = C.bank[b0], C.bank[b0 + 1], C.bank[b0 + 2]
        m = sm[s_]; M = ("sm", s_)
        k.act("activation", out=junk[:, 0:384], in_=ba[:, 8:392], func=AF.Square, scale=384 ** -0.5, accum_out=m[:, 0:1], r=[("bank", b0 + 1)], w=[M, "junk"])
        k.act("activation", out=junk[:, 0:256], in_=bb[:, 0:256], func=AF.Square, scale=256 ** -0.5, accum_out=m[:, 1:2], r=[("bank", b0 + 2)], w=[M, "junk"])
        evac_v(k, bv[:, 0:512], vst[s_], s_, ("bank", b0), C.Vf, i)
        k.dve("tensor_tensor", out=z_all[:, i, :], in0=ba[:, 0:8], in1=fbias[:], op=ALU.add, r=[("bank", b0 + 1), "fbias"], w=[("z_all", i)])
        k.pool("tensor_scalar", out=m[:, 2:4], in0=m[:, 0:2], scalar1=1.0, scalar2=RMS_EPS, op0=ALU.mult, op1=ALU.add, r=[M], w=[M])
        k.pool("tensor_tensor", out=m[:, 6:8], in0=m[:, 2:4], in1=C.nhalf[:, 0:2], op=ALU.pow, r=[M, "nhalf"], w=[M])
        cq = cqn[s_]; CQ = ("cqn", s_)
        k.dve("tensor_scalar", out=cq[:, 0:384], in0=ba[:, 8:392], scalar1=m[:, 6:7], scalar2=None, op0=ALU.mult, r=[M, ("bank", b0 + 1)], w=[CQ])
        k.dve("tensor_scalar", out=cq[:, 384:640], in0=bb[:, 0:256], scalar1=m[:, 7:8], scalar2=None, op0=ALU.mult, r=[M, ("bank", b0 + 2)], w=[CQ])
        pst = C.bank[6 + s_][:].bitcast(BF16)
        for j in range(5):
            k.pe("transpose", out=pst[:, j * 128:(j + 1) * 128], in_=cq[:, j * 128:(j + 1) * 128], identity=C.identb[:],
                 r=[CQ, "identb"], w=[("bank", 6 + s_)])
        k.act("activation", out=cqnT[:, :, tc], in_=pst[:, 0:384].rearrange("p (k t) -> p k t", k=3), func=AF.Copy,
              r=[("bank", 6 + s_)], w=[("cqnT", i)])
        k.act("activation", out=ckvnT[:, :, tc], in_=pst[:, 384:640].rearrange("p (k t) -> p k t", k=2), func=AF.Copy,
              r=[("bank", 6 + s_)], w=[("ckvnT", i)])

    pend_chain = []

    def stepA_push(i):
        mm_tile(i)
        if pend_chain:
            chain_tile(pend_chain.pop(0))
        pend_chain.append(i)

    if with_p0:
        end_off = k.ph_off
        k.ph_off = ov_off
        alloc_ln(k)
        assert k.ph_off <= end_off, "LN overlay too large"
        k.ph_off = end_off
        load_ln_params(k, C.ln_in_g, C.ln_in_b)
        prev_tiles = []
        for i0 in range(0, NT, NLN):
            tiles = list(range(i0, min(NT, i0 + NLN)))
            sl = ln_slots(k, len(tiles))
            for i in prev_tiles:
                stepA_push(i)
            items = []
            for s__, i in zip(sl, tiles):
                k.dma(C.lnx[s__][:], C.x[i * 128:(i + 1) * 128, :], w=[("lnx", s__)])
                items.append((s__, i, C.H[0][i * 128:(i + 1) * 128, :], ("H0", i), False))
            ln_batch(k, items)
            prev_tiles = tiles
        ln_flush(k)
        for i in prev_tiles:
            stepA_push(i)
    else:
        for i in range(NT):
            stepA_push(i)
    chain_tile(pend_chain.pop(0))

    NF = NT * 8
    zf = z_all[:].rearrange("p i h -> p (i h)")
    lff = lf_all[:].rearrange("p i h -> p (i h)")
    k.act("activation", out=lff, in_=zf, func=AF.Exp, scale=-1.0, r=["z_all"], w=["lf_all"])
    k.act("activation", out=lff, in_=lff, func=AF.Ln, bias=1.0, r=["lf_all"], w=["lf_all"])
    k.dve("tensor_scalar", out=lff, in0=lff, scalar1=-1.0, scalar2=None, op0=ALU.mult, r=["lf_all"], w=["lf_all"])
    k.dve("tensor_scalar", out=lf_all[:, 0, :], in0=lf_all[:, 0, :], scalar1=C.padcol[:, 0:1], scalar2=None, op0=ALU.mult, r=["lf_all", "padcol"], w=["lf_all"])
    k.pe("matmul", C.bank[0][:, 0:NF], lhsT=C.utf[:], rhs=lff, start=True, stop=True, r=["lf_all", "utf"], w=[("bank", 0)])
    k.pe("matmul", C.bank[1][:, 0:NF], lhsT=C.onesf[:], rhs=lff, start=True, stop=True, r=["lf_all", "onesf"], w=[("bank", 1)])
    tot = C.bank[1][:, 0:NF].rearrange("p (i h) -> p i h", h=8)
    k.dve("memset", carry[:, 0, :], 0.0, w=["carry"])
    for i in range(1, NT):
        k.dve("tensor_tensor", out=carry[:, i, :], in0=carry[:, i - 1, :], in1=tot[:, i - 1, :], op=ALU.add, r=["carry", ("bank", 1)], w=["carry"])
    au = aug_all
    c3 = C.bank[0][:, 0:NF].rearrange("p (i h) -> p i h", h=8)
    k.dve("tensor_tensor", out=au[:, :, 0:8], in0=c3, in1=carry[:], op=ALU.add, r=[("bank", 0), "carry"], w=["aug_all"])
    k.dve("tensor_scalar", out=lf_all[:], in0=au[:, :, 0:8], scalar1=-1.0, scalar2=None, op0=ALU.mult, r=["aug_all"], w=["lf_all"])
    k.dve("tensor_copy", out=hb_all[:], in_=lf_all[:], r=["lf_all"], w=["hb_all"])
    k.dve("tensor_copy", out=au[:, :, 8:16], in_=hb_all[:], r=["hb_all"], w=["aug_all"])
    k.dve("tensor_tensor", out=lf_all[:], in0=lf_all[:], in1=au[:, :, 8:16], op=ALU.subtract, r=["lf_all", "aug_all"], w=["lf_all"])
    k.dve("tensor_copy", out=hb_all[:], in_=lf_all[:], r=["lf_all"], w=["hb_all"])
    k.dve("tensor_copy", out=au[:, :, 16:24], in_=hb_all[:], r=["hb_all"], w=["aug_all"])
    k.dve("tensor_tensor", out=au[:, :, 24:32], in0=lf_all[:], in1=au[:, :, 16:24], op=ALU.subtract, r=["lf_all", "aug_all"], w=["aug_all"])
    for i0 in range(0, NT, 4):
        n4 = min(4, NT - i0)
        bk = 2 + (i0 // 4) % 2
        for j in range(n4):
            k.pe("transpose", out=C.bank[bk][0:32, j * 128:(j + 1) * 128], in_=au[:, i0 + j, :], identity=C.identf[:],
                 r=["aug_all", "identf"], w=[("bank", bk)])
        k.dve("tensor_copy", out=augT[:, i0 * 128:(i0 + n4) * 128], in_=C.bank[bk][0:32, 0:n4 * 128], r=[("bank", bk)], w=["augT"])
    k.dma(C.QTf[:, 67, :], augT[0:8, :], r=["augT"], w=[("QTf", "c")])
    k.dma(C.KTf[:, 64, :], augT[8:16, :], r=["augT"], w=[("KTf", "hi")])
    k.dma(C.KTf[:, 65, :], augT[16:24, :], r=["augT"], w=[("KTf", "mid")])
    k.dma(C.KTf[:, 66, :], augT[24:32, :], r=["augT"], w=[("KTf", "lo")])

    k.dma(cs[:], C.c_cs, w=["cs"] + ALIAS)
    cnt = 0
    for c in range(8):
        s_ = c % 2
        k.dma(wt[s_][:], win[:, :, c * 128:(c + 1) * 128], w=[("wt", s_)], cast=True)
        dst = C.QTf if c < 4 else C.KTf
        dn = "QTf" if c < 4 else "KTf"
        h0 = 2 * (c % 4)
        for gi, (g0, n) in enumerate(GROUPS):
            bk = cnt % 6
            st_ = stg[cnt % 3]; SG = ("stg", cnt % 3)
            cnt += 1
            for kc in range(KC):
                k.pe("matmul", C.bank[bk][:, 0:n], lhsT=wt[s_][:, kc, :], rhs=C.hT[:, kc, g0:g0 + n], start=(kc == 0), stop=(kc == KC - 1),
                     r=[("wt", s_), "hT"], w=[("bank", bk)])
            if c < 4:
                k.act("activation", out=st_[:, 0:n], in_=C.bank[bk][:, 0:n], func=AF.Copy, scale=0.125, r=[("bank", bk)], w=[SG])
            else:
                k.dve("tensor_copy", out=st_[:, 0:n], in_=C.bank[bk][:, 0:n], r=[("bank", bk)], w=[SG])
            k.dma(dst[h0, 0:64, g0:g0 + n], st_[0:64, 0:n], r=[SG], w=[(dn, (h0, gi))])
            k.dma(dst[h0 + 1, 0:64, g0:g0 + n], st_[64:128, 0:n], r=[SG], w=[(dn, (h0 + 1, gi))])
    for gi, (g0, n) in enumerate(GROUPS):
        bA, bB = 6, 7
        for kc in range(KC):
            k.pe("matmul", C.bank[bA][0:32, 0:n], lhsT=wkr[:, kc, :], rhs=C.hT[:, kc, g0:g0 + n], start=(kc == 0), stop=(kc == KC - 1),
                 r=["wkr", "hT"], w=[("bank", bA)])
        for kc in range(KC):
            k.pe("matmul", C.bank[bB][0:32, 0:n], lhsT=wkrR[:, kc, :], rhs=C.hT[:, kc, g0:g0 + n], start=(kc == 0), stop=(kc == KC - 1),
                 r=["wkrR", "hT"], w=[("bank", bB)])
        st_ = stg[gi % 3]; SG = ("stg", gi % 3)
        k.dve("tensor_tensor", out=rt[0][0:32, 0:n], in0=C.bank[bA][0:32, 0:n], in1=cs[0:32, g0:g0 + n], op=ALU.mult, r=[("bank", bA), "cs"], w=[("rt", 0)])
        k.dve("tensor_tensor", out=rt[1][0:32, 0:n], in0=C.bank[bB][0:32, 0:n], in1=cs[32:64, g0:g0 + n], op=ALU.mult, r=[("bank", bB), "cs"], w=[("rt", 1)])
        k.dve("tensor_tensor", out=st_[0:32, 0:n], in0=rt[0][0:32, 0:n], in1=rt[1][0:32, 0:n], op=ALU.add, r=[("rt", 0), ("rt", 1)], w=[SG])
        for h in range(8):
            k.dma(C.KTm[h, 64:96, g0:g0 + n], st_[0:32, 0:n], r=[SG], w=[("KTm", (h, gi, "r"))])

    for kc in range(3):
        k.dma(stgw[:, 0:768], C.mla_w_uq[l][kc * 128:(kc + 1) * 128, :], w=["stgw"] + ALIAS)
        k.dve("tensor_scalar", out=wuq[:, kc, :], in0=stgw[:, 0:768], scalar1=gq[:, kc:kc + 1], scalar2=None, op0=ALU.mult, r=["stgw", "gq"], w=["wuq"] + ALIAS)
        k.dve("tensor_copy", out=wuqR[:, kc, :], in_=wuq[:, kc, :], r=["wuq"], w=["wuqR"] + ALIAS)
        wv_ = wuq[:, kc, :].rearrange("p (h d) -> p h d", d=96)
        wr_ = wuqR[:, kc, :].rearrange("p (h d) -> p h d", d=96)
        k.dve("tensor_scalar", out=wr_[:, :, 64:80], in0=wv_[:, :, 80:96], scalar1=-1.0, scalar2=None, op0=ALU.mult, r=["wuq"], w=["wuqR"])
        k.dve("tensor_copy", out=wr_[:, :, 80:96], in_=wv_[:, :, 64:80], r=["wuq"], w=["wuqR"])
    for kc in range(2):
        k.dma(stgw[:, 0:1024], C.mla_w_ukv[l][kc * 128:(kc + 1) * 128, :], w=["stgw"])
        k.dve("tensor_scalar", out=wukv[:, kc, :], in0=stgw[:, 0:1024], scalar1=gq[:, 4 + kc:5 + kc], scalar2=None, op0=ALU.mult, r=["stgw", "gq"], w=["wukv"] + ALIAS)
    cnt = 0
    for h in range(8):
        for gi, (g0, n) in enumerate(GROUPS):
            bA, bB, bC = 3 * (cnt % 2), 3 * (cnt % 2) + 1, 3 * (cnt % 2) + 2
            qs = stg[cnt % 3]; QS = ("stg", cnt % 3)
            cnt += 1
            for kc in range(3):
                k.pe("matmul", C.bank[bA][0:96, 0:n], lhsT=wuq[:, kc, h * 96:(h + 1) * 96], rhs=cqnT[:, kc, g0:g0 + n], start=(kc == 0), stop=(kc == 2),
                     r=["wuq", "cqnT"], w=[("bank", bA)])
            for kc in range(3):
                k.pe("matmul", C.bank[bB][0:96, 0:n], lhsT=wuqR[:, kc, h * 96:(h + 1) * 96], rhs=cqnT[:, kc, g0:g0 + n], start=(kc == 0), stop=(kc == 2),
                     r=["wuqR", "cqnT"], w=[("bank", bB)])
            for kc in range(2):
                k.pe("matmul", C.bank[bC][0:64, 0:n], lhsT=wukv[:, kc, h * 64:(h + 1) * 64], rhs=ckvnT[:, kc, g0:g0 + n], start=(kc == 0), stop=(kc == 1),
                     r=["wukv", "ckvnT"], w=[("bank", bC)])
            k.act("activation", out=qs[0:64, 0:n], in_=C.bank[bA][0:64, 0:n], func=AF.Copy, r=[("bank", bA)], w=[QS])
            k.dve("tensor_tensor", out=rt[0][64:96, 0:n], in0=C.bank[bA][64:96, 0:n], in1=cs[64:96, g0:g0 + n], op=ALU.mult, r=[("bank", bA), "cs"], w=[("rt", 0)])
            k.dve("tensor_tensor", out=rt[1][64:96, 0:n], in0=C.bank[bB][64:96, 0:n], in1=cs[96:128, g0:g0 + n], op=ALU.mult, r=[("bank", bB), "cs"], w=[("rt", 1)])
            k.dve("tensor_tensor", out=qs[64:96, 0:n], in0=rt[0][64:96, 0:n], in1=rt[1][64:96, 0:n], op=ALU.add, r=[("rt", 0), ("rt", 1)], w=[QS])
            k.dma(C.QTm[h, :, g0:g0 + n], qs[0:96, 0:n], r=[QS], w=[("QTm", (h, gi))])
            ks = stg[cnt % 3]; KS = ("stg", cnt % 3)
            cnt += 1
            k.act("activation", out=ks[0:64, 0:n], in_=C.bank[bC][0:64, 0:n], func=AF.Copy, r=[("bank", bC)], w=[KS])
            k.dma(C.KTm[h, 0:64, g0:g0 + n], ks[0:64, 0:n], r=[KS], w=[("KTm", (h, gi, "n"))])
    for i in range(NT):
        s_ = i % 2
        tc = slice(i * 128, (i + 1) * 128)
        bk = 6 + s_
        for kc in range(2):
            k.pe("matmul", C.bank[bk][:, 0:512], lhsT=ckvnT[:, kc, tc], rhs=wukv[:, kc, 512:1024], start=(kc == 0), stop=(kc == 1),
                 r=["wukv", ("ckvnT", i)], w=[("bank", bk)])
        evac_v(k, C.bank[bk][:, 0:512], vst[s_], s_, ("bank", bk), C.Vm, i)


def attention(k, QT, KT, V, Dk, scale, OT, nm):
    C = k.C
    k.new_phase()
    qT = [k.psb("qT%d" % i, [Dk, 2, T], BF16) for i in range(2)]
    kT = [k.psb("kT%d" % i, [Dk, 2, T], BF16) for i in range(2)]
    vv = [k.psb("vv%d" % i, [128, 2, NT, 128], BF16) for i in range(2)]
    pb = [k.psb("pb%d" % i, [128, 2, 512], BF16) for i in range(2)]
    rr = [k.psb("rr%d" % i, [128, 512], F32) for i in range(2)]
    osb = [k.psb("osb%d" % i, [128, 512], F32) for i in range(2)]
    pb.append(k.psb("pb2", [128, 2, 512], BF16))
    ost = [k.psb("ost%d" % i, [128, 512], BF16) for i in range(2)]
    step = 0
    pend = []

    def drain(keep):
        while len(pend) > keep:
            f, post = pend.pop(0)
            f()
            if post is not None:
                post()

    for hp in range(4):
        s_ = hp % 2
        for j in range(2):
            h = 2 * hp + j
            k.dma(qT[s_][:, j, :], QT[h], r=["QTf", "QTm"], w=[("qT", s_)])
            k.dma(kT[s_][:, j, :], KT[h], r=["KTf", "KTm"], w=[("kT", s_)])
            k.dma(vv[s_][:, j, :, :], V[h], r=["V"], w=[("vv", s_)])
        for gi, (g0, n) in enumerate(AGROUPS):
            qb0 = g0 // 128
            nkb = qb0 + n // 128
            ob = 6

            def make_pv(pkb, plo, pnn, psl, last, s_=s_):
                def f():
                    for j in range(2):
                        k.pe("matmul", C.bank[ob + j][:, plo:plo + pnn], lhsT=vv[s_][:, j, pkb, :], rhs=pb[psl][:, j, 0:pnn],
                             start=(pkb == 0), stop=last, r=[("vv", s_), ("pb", psl)], w=[("bank", ob + j)])
                return f

            def make_post(hp=hp, gi=gi, g0=g0, n=n):
                def post():
                    slots = []
                    for j in range(2):
                        r_ = rr[j]; RR = ("rr", j)
                        if j == 0:
                            po, pz = slice(0, 64), slice(64, 128)
                        else:
                            po, pz = slice(64, 128), slice(0, 64)
                        k.dve("tensor_copy", out=osb[j][po, 0:n], in_=C.bank[ob + j][po, 0:n], r=[("bank", ob + j)], w=[("osb", j)])
                        k.dve("tensor_copy", out=r_[po, 0:n], in_=C.bank[ob + j][pz, 0:n], r=[("bank", ob + j)], w=[RR])
                        slots.append((j, r_, RR, po))
                    for (j, r_, RR, po) in slots:
                        h = 2 * hp + j
                        os_ = ost[j]; OS = ("ost", j)
                        k.dve("reciprocal", out=r_[po, 0:n], in_=r_[po, 0:n], r=[RR], w=[RR])
                        k.dve("tensor_tensor", out=os_[po, 0:n], in0=osb[j][po, 0:n], in1=r_[po, 0:n], op=ALU.mult, r=[("osb", j), RR], w=[OS])
                        k.dma(OT[po, hp, g0:g0 + n], os_[po, 0:n], r=[OS], w=[(nm, (h, gi))])
                return post

            for kb in range(nkb):
                lo = max(0, kb * 128 - g0)
                nn = n - lo
                sl_ = step % 3
                step += 1
                diag = kb >= qb0
                for j in range(2):
                    bk = 2 * sl_ + j
                    k.pe("matmul", C.bank[bk][:, 0:nn], lhsT=kT[s_][:, j, kb * 128:(kb + 1) * 128], rhs=qT[s_][:, j, g0 + lo:g0 + n],
                         start=True, stop=not (diag or kb == 0), r=[("kT", s_), ("qT", s_)], w=[("bank", bk)])
                    if kb == 0:
                        k.pe("matmul", C.bank[bk][:, 0:nn], lhsT=C.identb[:], rhs=C.pmask[:, 0:nn], start=False, stop=not diag,
                             r=["identb", "pmask"], w=[("bank", bk)])
                    if diag:
                        cm = C.cmask0 if kb == 0 else C.cmask
                        k.pe("matmul", C.bank[bk][:, 0:128], lhsT=C.identb[:], rhs=cm[:], start=False, stop=True,
                             r=["identb", "cmask", "cmask0"], w=[("bank", bk)])
                k.act("activation", out=pb[sl_][:, :, 0:nn], in_=C.psall[:, 2 * sl_:2 * sl_ + 2, 0:nn], func=AF.Exp, scale=scale,
                      r=[("bank", 2 * sl_), ("bank", 2 * sl_ + 1)], w=[("pb", sl_)])
                last = (kb == nkb - 1)
                pend.append((make_pv(kb, lo, nn, sl_, last), make_post() if last else None))
                drain(2)
    drain(0)


def phase3(k, l, Hin, hin_name, Hout, hout_name):
    C = k.C
    k.new_phase()
    alloc_ln(k)
    load_ln_params(k, C.ln1_g[l], C.ln1_b[l])
    win = C.w_in[l].rearrange("(kc p) c -> p kc c", p=128)
    wgf = k.psb("wgf", [128, KC, D], BF16); wgm = k.psb("wgm", [128, KC, D], BF16)
    wof = k.psb("wof", [128, 4, D], BF16); wom = k.psb("wom", [128, 4, D], BF16)
    wo = k.psb("wo", [128, KC, D], BF16)
    otf = [k.psb("otf%d" % i, [128, 4, 512], BF16) for i in range(2)]
    otm = [k.psb("otm%d" % i, [128, 4, 512], BF16) for i in range(2)]
    mT = k.psb("mT", [128, KC, 512], BF16)
    sg = [k.psb("sg%d" % i, [128, 512], BF16) for i in range(4)]
    tt = [k.psb("tt%d" % i, [128, 512], F32) for i in range(4)]
    k.dma(wgf[:], win[:, :, OGF:OGF + D], w=["wgf"], cast=True)
    k.dma(wgm[:], win[:, :, OGM:OGM + D], w=["wgm"], cast=True)
    k.dma(wof[:], C.fox_w_o[l].rearrange("(m p) c -> p m c", p=128), w=["wof"], cast=True)
    k.dma(wom[:], C.mla_w_o[l].rearrange("(m p) c -> p m c", p=128), w=["wom"], cast=True)
    k.dma(wo[:], C.w_out[l].rearrange("(kc p) c -> p kc c", p=128), w=["wo"], cast=True)
    cc = 0
    for gi, (g0, n) in enumerate(GROUPS):
        s_ = gi % 2
        tiles = list(range(g0 // 128, (g0 + n) // 128))
        hk = [("hT", i) for i in tiles]
        k.dma(otf[s_][:, :, 0:n], C.OTf[:, :, g0:g0 + n], r=["OTf"], w=[("otf", s_)])
        k.dma(otm[s_][:, :, 0:n], C.OTm[:, :, g0:g0 + n], r=["OTm"], w=[("otm", s_)])
        for c in range(KC):
            b0 = 4 * (cc % 2)
            q_ = 2 * (cc % 2)
            cc += 1
            cs_ = slice(c * 128, (c + 1) * 128)
            for kc in range(KC):
                k.pe("matmul", C.bank[b0][:, 0:n], lhsT=wgf[:, kc, cs_], rhs=C.hT[:, kc, g0:g0 + n], start=(kc == 0), stop=(kc == KC - 1),
                     r=["wgf"] + hk, w=[("bank", b0)])
            for kc in range(KC):
                k.pe("matmul", C.bank[b0 + 1][:, 0:n], lhsT=wgm[:, kc, cs_], rhs=C.hT[:, kc, g0:g0 + n], start=(kc == 0), stop=(kc == KC - 1),
                     r=["wgm"] + hk, w=[("bank", b0 + 1)])
            for m in range(4):
                k.pe("matmul", C.bank[b0 + 2][:, 0:n], lhsT=wof[:, m, cs_], rhs=otf[s_][:, m, 0:n], start=(m == 0), stop=(m == 3),
                     r=["wof", ("otf", s_)], w=[("bank", b0 + 2)])
            for m in range(4):
                k.pe("matmul", C.bank[b0 + 3][:, 0:n], lhsT=wom[:, m, cs_], rhs=otm[s_][:, m, 0:n], start=(m == 0), stop=(m == 3),
                     r=["wom", ("otm", s_)], w=[("bank", b0 + 3)])
            k.act("activation", out=sg[q_][:, 0:n], in_=C.bank[b0][:, 0:n], func=AF.Sigmoid, r=[("bank", b0)], w=[("sg", q_)])
            k.act("activation", out=sg[q_ + 1][:, 0:n], in_=C.bank[b0 + 1][:, 0:n], func=AF.Sigmoid, r=[("bank", b0 + 1)], w=[("sg", q_ + 1)])
            k.dve("tensor_tensor", out=tt[q_][:, 0:n], in0=C.bank[b0 + 2][:, 0:n], in1=sg[q_][:, 0:n], op=ALU.mult,
                  r=[("bank", b0 + 2), ("sg", q_)], w=[("tt", q_)])
            k.dve("tensor_tensor", out=tt[q_ + 1][:, 0:n], in0=C.bank[b0 + 3][:, 0:n], in1=sg[q_ + 1][:, 0:n], op=ALU.mult,
                  r=[("bank", b0 + 3), ("sg", q_ + 1)], w=[("tt", q_ + 1)])
            k.pool("tensor_tensor", out=mT[:, c, 0:n], in0=tt[q_][:, 0:n], in1=tt[q_ + 1][:, 0:n], op=ALU.add,
                   r=[("tt", q_), ("tt", q_ + 1)], w=[("mT", c)])
        sl = ln_slots(k, len(tiles))
        items = []
        for j, i in enumerate(tiles):
            ls = sl[j]
            k.dma(C.lnx[ls][:], Hin[i * 128:(i + 1) * 128, :], r=[(hin_name, i)], w=[("lnx", ls)])
            for half in range(2):
                bk = 2 * (j % 2) + half
                for kc in range(KC):
                    k.pe("matmul", C.bank[bk][:, 0:512], lhsT=mT[:, kc, j * 128:(j + 1) * 128], rhs=wo[:, kc, half * 512:(half + 1) * 512],
                         start=(kc == 0), stop=(kc == KC - 1), r=["mT", "wo"], w=[("bank", bk)])
                hs = slice(half * 512, (half + 1) * 512)
                k.dve("scalar_tensor_tensor", out=C.lnx[ls][:, hs], in0=C.lnx[ls][:, hs], scalar=ALPHA, in1=C.bank[bk][:, 0:512],
                      op0=ALU.mult, op1=ALU.add, r=[("lnx", ls), ("bank", bk)], w=[("lnx", ls)])
            items.append((ls, i, Hout[i * 128:(i + 1) * 128, :], (hout_name, i), False))
        ln_batch(k, items)
    ln_flush(k)


def phase4(k, l, Hin, hin_name, Hout, hout_name, last):
    C = k.C
    k.new_phase()
    alloc_ln(k)
    load_ln_params(k, C.ln2_g[l], C.ln2_b[l])
    rw = k.psb("rw", [128, KC, NE], BF16)
    rb = k.psb("rb", [128, NE], F32)
    comb = k.psb("comb", [128, NT, NE], F32)
    wg = [k.psb("wg%d" % i, [128, KC, DE], BF16) for i in range(2)]
    wu = [k.psb("wu%d" % i, [128, KC, DE], BF16) for i in range(2)]
    wd = [k.psb("wd%d" % i, [128, 2, D], BF16) for i in range(2)]
    NSG = 11
    yacc = k.psb("yacc", [128, NSG, D], F32)
    hid = [k.psb("hid%d" % i, [128, 2, 512], BF16) for i in range(2)]
    sl = [k.psb("sl%d" % i, [128, 512], F32) for i in range(2)]
    k.dma(rw[:], C.router_w.rearrange("(kc p) e -> p kc e", p=128), w=["rw"], cast=True)
    k.dma(rb[:], bcast_row(C.router_b, NE), w=["rb"])
    NR = NT * NE
    ra = [k.psb("ra%d" % i, [128, NR], F32) for i in range(5)]
    rsm = k.psb("rsm", [128, NT * 4 * 4], F32)
    lg = C.psall[:, 6:8, :].rearrange("p b c -> p (b c)")
    for i in range(NT):
        tc = slice(i * 128, (i + 1) * 128)
        for kc in range(KC):
            k.pe("matmul", lg[:, i * NE:(i + 1) * NE], lhsT=C.hT[:, kc, tc], rhs=rw[:, kc, :], start=(kc == 0), stop=(kc == KC - 1),
                 r=[("hT", i), "rw"], w=[("bank", 6), ("bank", 7)])
    sc, b_, eq, b2, ge = [t[:] for t in ra]
    G4 = NT * 4
    m1 = rsm[:, 0:G4]; m2 = rsm[:, G4:2 * G4]; gs = rsm[:, 2 * G4:3 * G4]; gsel = rsm[:, 3 * G4:4 * G4]
    gmax = k.psb("gmax", [128, NT], F32); den = k.psb("den", [128, NT], F32)
    v3 = lambda ap: ap.rearrange("p (g e) -> p g e", e=4)
    bc4 = lambda ap: ap.unsqueeze(2).broadcast_to([128, G4, 4])
    k.act("activation", out=sc, in_=lg[:, 0:NR], func=AF.Sigmoid, r=[("bank", 6), ("bank", 7)], w=["ra0"])
    k.dve("tensor_tensor", out=b_.rearrange("p (i e) -> p i e", e=NE), in0=sc.rearrange("p (i e) -> p i e", e=NE),
          in1=rb[:].unsqueeze(1).broadcast_to([128, NT, NE]), op=ALU.add, r=["ra0", "rb"], w=["ra1"])
    k.dve("tensor_reduce", out=m1, in_=v3(b_), axis=AX.X, op=ALU.max, r=["ra1"], w=["rsm"])
    k.dve("tensor_tensor", out=v3(eq), in0=v3(b_), in1=bc4(m1), op=ALU.is_equal, r=["ra1", "rsm"], w=["ra2"])
    k.dve("scalar_tensor_tensor", out=b2, in0=eq, scalar=-1e9, in1=b_, op0=ALU.mult, op1=ALU.add, r=["ra2", "ra1"], w=["ra3"])
    k.dve("tensor_reduce", out=m2, in_=v3(b2), axis=AX.X, op=ALU.max, r=["ra3"], w=["rsm"])
    k.dve("tensor_tensor", out=gs, in0=m1, in1=m2, op=ALU.add, r=["rsm"], w=["rsm"])
    k.dve("tensor_reduce", out=gmax[:], in_=gs.rearrange("p (i g) -> p i g", g=4), axis=AX.X, op=ALU.max, r=["rsm"], w=["gmax"])
    k.dve("tensor_tensor", out=gsel.rearrange("p (i g) -> p i g", g=4), in0=gs.rearrange("p (i g) -> p i g", g=4),
          in1=gmax[:].unsqueeze(2).broadcast_to([128, NT, 4]), op=ALU.is_equal, r=["rsm", "gmax"], w=["rsm"])
    k.dve("tensor_tensor", out=v3(ge), in0=v3(b_), in1=bc4(m2), op=ALU.is_ge, r=["ra1", "rsm"], w=["ra4"])
    k.dve("tensor_tensor", out=v3(ge), in0=v3(ge), in1=bc4(gsel), op=ALU.mult, r=["ra4", "rsm"], w=["ra4"])
    k.dve("tensor_tensor", out=eq, in0=ge, in1=sc, op=ALU.mult, r=["ra4", "ra0"], w=["ra2"])
    k.dve("tensor_reduce", out=den[:], in_=eq.rearrange("p (i e) -> p i e", e=NE), axis=AX.X, op=ALU.add, r=["ra2"], w=["den"])
    k.dve("reciprocal", out=den[:], in_=den[:], r=["den"], w=["den"])
    k.dve("tensor_tensor", out=comb[:], in0=eq.rearrange("p (i e) -> p i e", e=NE),
          in1=den[:].unsqueeze(2).broadcast_to([128, NT, NE]), op=ALU.mult, r=["ra2", "den"], w=["comb"])
    subs = []
    for t0 in range(0, NT, NSG):
        nt_ = min(NSG, NT - t0)
        for e in range(NE):
            for j0 in range(0, nt_, 4):
                subs.append((t0, nt_, e, j0, min(4, nt_ - j0)))
    wslot = {}
    wcnt = [0]

    def load_w(t0, e):
        if (t0, e) in wslot:
            return
        ws = wcnt[0] % 2
        wcnt[0] += 1
        wslot[(t0, e)] = ws
        k.dma(wg[ws][:], C.w_gate[l, e].rearrange("(kc p) f -> p kc f", p=128), w=[("wg", ws)], cast=True)
        k.dma(wu[ws][:], C.w_up[l, e].rearrange("(kc p) f -> p kc f", p=128), w=[("wu", ws)], cast=True)
        k.dma(wd[ws][:], C.w_down[l, e].rearrange("(kc p) f -> p kc f", p=128), w=[("wd", ws)], cast=True)

    def gu_ops(si):
        t0, nt_, e, j0, ntile = subs[si]
        ws = wslot[(t0, e)]
        n = ntile * 128
        c0 = (t0 + j0) * 128
        hk = [("hT", i) for i in range(t0 + j0, t0 + j0 + ntile)]
        q_ = si % 2
        ops = []
        for ch in range(2):
            for (wt_, wn, bkk) in ((wg, "wg", ch), (wu, "wu", 2 + ch)):
                for kc in range(KC):
                    ops.append(lambda wt_=wt_, wn=wn, bkk=bkk, kc=kc, ch=ch: k.pe(
                        "matmul", C.bank[bkk][:, 0:n], lhsT=wt_[ws][:, kc, ch * 128:(ch + 1) * 128], rhs=C.hT[:, kc, c0:c0 + n],
                        start=(kc == 0), stop=(kc == KC - 1), r=[(wn, ws)] + hk, w=[("bank", bkk)]))

        def post(ch):
            k.act("activation", out=sl[ch][:, 0:n], in_=C.bank[ch][:, 0:n], func=AF.Silu, r=[("bank", ch)], w=[("sl", ch)])
            k.dve("tensor_tensor", out=hid[q_][:, ch, 0:n], in0=C.bank[2 + ch][:, 0:n], in1=sl[ch][:, 0:n], op=ALU.mult,
                  r=[("bank", 2 + ch), ("sl", ch)], w=[("hid", (q_, ch))])
        return ops, post

    ycnt = [0]

    def down_ops(si):
        t0, nt_, e, j0, ntile = subs[si]
        ws = wslot[(t0, e)]
        q_ = si % 2
        ops = []
        for j in range(ntile):
            tl = j0 + j
            for half in range(2):
                def f(j=j, tl=tl, half=half):
                    bk = 4 + ycnt[0] % 4
                    ycnt[0] += 1
                    for ch in range(2):
                        k.pe("matmul", C.bank[bk][:, 0:512], lhsT=hid[q_][:, ch, j * 128:(j + 1) * 128], rhs=wd[ws][:, ch, half * 512:(half + 1) * 512],
                             start=(ch == 0), stop=(ch == 1), r=[("hid", (q_, ch)), ("wd", ws)], w=[("bank", bk)])
                    ya = yacc[:, tl, half * 512:(half + 1) * 512]
                    cw = comb[:, t0 + tl, e:e + 1]
                    if e == 0:
                        k.dve("tensor_scalar", out=ya, in0=C.bank[bk][:, 0:512], scalar1=cw, scalar2=None, op0=ALU.mult,
                              r=[("bank", bk), ("comb", t0 + tl)], w=[("yacc", tl)])
                    else:
                        k.dve("scalar_tensor_tensor", out=ya, in0=C.bank[bk][:, 0:512], scalar=cw, in1=ya, op0=ALU.mult, op1=ALU.add,
                              r=[("bank", bk), ("comb", t0 + tl), ("yacc", tl)], w=[("yacc", tl)])
                ops.append(f)
        return ops

    def ln_tail(t0, nt_):
        tls = [tl for tl in range(nt_) if not (last and t0 + tl == 0)]
        for b0 in range(0, len(tls), NLN):
            bt = tls[b0:b0 + NLN]
            sl_ = ln_slots(k, len(bt))
            items = []
            for ls, tl in zip(sl_, bt):
                i = t0 + tl
                k.dma(C.lnx[ls][:], Hin[i * 128:(i + 1) * 128, :], r=[(hin_name, i)], w=[("lnx", ls)])
                k.dve("scalar_tensor_tensor", out=C.lnx[ls][:], in0=C.lnx[ls][:], scalar=ALPHA, in1=yacc[:, tl, :],
                      op0=ALU.mult, op1=ALU.add, r=[("lnx", ls), ("yacc", tl)], w=[("lnx", ls)])
                if last:
                    items.append((ls, i, C.out[(i - 1) * 128:i * 128, :], ("out", i), True))
                else:
                    items.append((ls, i, Hout[i * 128:(i + 1) * 128, :], (hout_name, i), False))
            ln_batch(k, items)
        ln_flush(k)

    NS = len(subs)
    load_w(subs[0][0], subs[0][2])
    gops, gpost = gu_ops(0)
    for f in gops[0:16]:
        f()
    gpost(0)
    for f in gops[16:32]:
        f()
    gpost(1)
    for si in range(NS):
        t0, nt_, e, j0, ntile = subs[si]
        if j0 == 0:
            nxt = [x for x in subs[si + 1:] if (x[0], x[2]) != (t0, e)]
            if nxt:
                load_w(nxt[0][0], nxt[0][2])
        dops = down_ops(si)
        last_of_sg = (si + 1 == NS) or (subs[si + 1][0] != t0)
        if si + 1 < NS and not last_of_sg:
            gops, gpost = gu_ops(si + 1)
            gi_ = 0
            nd = len(dops)
            early = max(0, nd - 3)
            per = (32 + early - 1) // max(1, early) if early else 32
            di = 0
            while gi_ < 32:
                stop_at = min(32, gi_ + per)
                while gi_ < stop_at:
                    gops[gi_]()
                    gi_ += 1
                    if gi_ == 16:
                        gpost(0)
                if di < early:
                    dops[di]()
                    di += 1
            gpost(1)
            while di < nd:
                dops[di]()
                di += 1
        else:
            for f in dops:
                f()
            if last_of_sg:
                ln_tail(t0, nt_)
            if si + 1 < NS:
                gops, gpost = gu_ops(si + 1)
                for f in gops[0:16]:
                    f()
                gpost(0)
                for f in gops[16:32]:
                    f()
                gpost(1)


def build(upto=99, debug=False):
    k = K(upto, debug)
    nc = k.nc
    C = setup(k)
    if upto >= 1:
        phase1(k, 0, with_p0=True)
    else:
        phase0(k)
    if debug and upto == 1:
        for nm, ap in (("QTf", C.QTf), ("KTf", C.KTf), ("QTm", C.QTm), ("KTm", C.KTm), ("Vf", C.Vf), ("Vm", C.Vm)):
            o = k.dram_out("dbg_" + nm, list(ap.shape), BF16)
            k.dma(o, ap, r=[nm, "V"], w=["dbg_" + nm], final=True)
    if upto >= 2:
        attention(k, C.QTf, C.KTf, C.Vf, 68, 1.0, C.OTf, "OTf")
        attention(k, C.QTm, C.KTm, C.Vm, 96, 96 ** -0.5, C.OTm, "OTm")
    if debug and upto == 2:
        for nm, ap in (("OTf", C.OTf), ("OTm", C.OTm)):
            o = k.dram_out("dbg_" + nm, list(ap.shape), BF16)
            k.dma(o, ap, r=[nm], w=["dbg_" + nm], final=True)
    if upto >= 3:
        phase3(k, 0, C.H[0], "H0", C.H[1], "H1")
    if debug and upto == 3:
        o = k.dram_out("dbg_H1", [T, D], F32)
        k.dma(o, C.H[1], r=["H1"], w=["dbg_H1"], final=True)
    if upto >= 4:
        phase4(k, 0, C.H[1], "H1", C.H[0], "H0", False)
    if debug and upto == 4:
        o = k.dram_out("dbg_H2", [T, D], F32)
        k.dma(o, C.H[0], r=["H0"], w=["dbg_H2"], final=True)
    if upto >= 5:
        phase1(k, 1)
        attention(k, C.QTf, C.KTf, C.Vf, 68, 1.0, C.OTf, "OTf")
        attention(k, C.QTm, C.KTm, C.Vm, 96, 96 ** -0.5, C.OTm, "OTm")
        phase3(k, 1, C.H[0], "H0", C.H[1], "H1")
        phase4(k, 1, C.H[1], "H1", None, None, True)
    if debug and upto == 0:
        C.dbg_hT = k.dram_out("dbg_hT", [128, KC, T], BF16)
        k.dma(C.dbg_hT, C.hT[:], r=["hT"], w=["dbg_hT"], final=True)
        C.dbg_H = k.dram_out("dbg_H", [T, D], F32)
        k.dma(C.dbg_H, C.H[0], r=["H0"], w=["dbg_H"], final=True)
    finish(k)
    return k


def finish(k):
    nc = k.nc
    R = k.R
    sems = {e: nc.alloc_semaphore("sem_" + e) for e in R.ENGS}
    dsems = {}
    for e in ("sp", "pool"):
        for j in range(R.NQ):
            dsems[(e, j)] = nc.alloc_semaphore("dsem_%s_%d" % (e, j))
    R.replay(nc, sems, dsems)


def host_consts():
    c = {}
    c["c_ident"] = np.eye(128, dtype=np.float32)
    kk = np.arange(128)[:, None]; qq = np.arange(128)[None, :]
    c["c_cmask"] = np.where(kk > qq, NEG, 0.0).astype(np.float32)
    cm0 = np.where(kk > qq, NEG, 0.0).astype(np.float32)
    cm0[NPAD, :NPAD] = 0.0
    c["c_cmask0"] = cm0
    c["c_pmask"] = np.where(np.broadcast_to(kk, (128, 512)) < NPAD, NEG, 0.0).astype(np.float32)
    c["c_ut"] = (kk <= qq).astype(np.float32)
    c["c_padcol"] = (np.arange(128) >= NPAD).astype(np.float32)[:, None]
    inv = (10000.0 ** (-np.arange(16, dtype=np.float32) / 16)).astype(np.float32)
    pos = (np.arange(T) - NPAD).astype(np.float32)
    ang = pos[None, :] * inv[np.arange(128) % 16][:, None]
    cs = np.cos(ang).astype(np.float32)
    sn = np.sin(ang).astype(np.float32)
    sel_sin = ((np.arange(128) // 32) % 2 == 1)[:, None]
    c["c_cs"] = np.where(sel_sin, sn, cs).astype(np.float32)
    c["c_ones8"] = np.ones((8, T), np.float32)
    sel = np.zeros((48, NE, 128), np.float32)
    for e in range(NE):
        sel[e, e, :] = 1.0
        sel[32 + e, e, :] = 1.0
    c["c_sel"] = sel.reshape(48, NE * 128)
    return c


def make_in_maps(inputs):
    x = np.asarray(inputs["x"], np.float32)
    meta = np.asarray(inputs["meta_tokens"], np.float32)
    shared = {}
    for name in ["ln_in_g", "ln_in_b", "w_in", "fox_f_bias", "fox_w_o", "mla_q_norm", "mla_w_uq",
                 "mla_kv_norm", "mla_w_o", "w_out", "ln1_g", "ln1_b", "router_w", "router_b",
                 "w_gate", "w_up", "w_down", "ln2_g", "ln2_b"]:
        shared[name] = np.ascontiguousarray(np.asarray(inputs[name], np.float32))
    wkv = np.asarray(inputs["mla_w_ukv"], np.float32).reshape(DEPTH, 256, 8, 2, 64)
    shared["mla_w_ukv"] = np.ascontiguousarray(
        np.concatenate([wkv[:, :, :, 0, :].reshape(DEPTH, 256, 512), wkv[:, :, :, 1, :].reshape(DEPTH, 256, 512)], axis=2))
    shared.update(host_consts())
    maps = []
    for b in range(8):
        xp = np.zeros((T, D), np.float32)
        xp[NPAD:128] = meta
        xp[128:] = x[b]
        m = dict(shared)
        m["x"] = xp
        maps.append(m)
    return maps


_CACHE = {}


def kernel(**inputs):
    if "k" not in _CACHE:
        _CACHE["k"] = build()
    k = _CACHE["k"]
    maps = make_in_maps(inputs)
    maps = [{n: m[n] for n in k.inp} for m in maps]
    res = run_bass_kernel_spmd(k.nc, maps, core_ids=list(range(8)))
    out = np.stack([np.asarray(r["out"], np.float32) for r in res.results], axis=0)
    return out


def check_debug(r, R, relerr):
    f = lambda a: np.asarray(a).astype(np.float32)
    if "dbg_H" in r:
        print("H0 err", relerr(f(r["dbg_H"])[NPAD:], R["h0"]))
    if "dbg_hT" in r:
        hT = f(r["dbg_hT"]).transpose(2, 1, 0).reshape(T, D)
        print("hT err (vs h0)", relerr(hT[NPAD:], R["h0"]))
    L0 = R["L0"]
    if "dbg_H1" in r:
        print("H1 err", relerr(f(r["dbg_H1"])[NPAD:], L0["h1"]), np.isfinite(f(r["dbg_H1"])).all())
    if "dbg_H2" in r:
        print("H2 err", relerr(f(r["dbg_H2"])[NPAD:], L0["h2"]), np.isfinite(f(r["dbg_H2"])).all())
    if "out" in r and "dbg_H" not in r and "dbg_H1" not in r and "dbg_H2" not in r and "dbg_QTf" not in r and "dbg_OTf" not in r:
        print("OUT err", relerr(f(r["out"]), R["L1"]["h2"][NMETA:]), np.isfinite(f(r["out"])).all())
    for nm, rn in (("dbg_OTf", "ofox"), ("dbg_OTm", "omla")):
        if nm in r:
            o = f(r[nm])
            oh = np.stack([o[(hh % 2) * 64:(hh % 2) * 64 + 64, hh // 2, :] for hh in range(8)], 0)
            print(nm, "err", relerr(oh[:, :, NPAD:].transpose(0, 2, 1), L0[rn]), "finite", np.isfinite(o).all())
    if "dbg_QTf" in r:
        q = f(r["dbg_QTf"]); kk = f(r["dbg_KTf"])
        print("qf err", relerr(q[:, 0:64, NPAD:].transpose(0, 2, 1) * 8, L0["qf"]))
        print("kf err", relerr(kk[:, 0:64, NPAD:].transpose(0, 2, 1), L0["kf"]))
        print("q ones rows", q[:, 64:67].min(), q[:, 64:67].max(), "k ones row", kk[:, 67].min(), kk[:, 67].max())
        print("c row err", relerr(q[:, 67, NPAD:], L0["dec"]), np.abs(q[:, 67, :NPAD]).max())
        nc_ = kk[:, 64, NPAD:] + kk[:, 65, NPAD:] + kk[:, 66, NPAD:]
        print("-c split err", relerr(-nc_, L0["dec"]), np.abs(nc_ + L0["dec"]).max())
        v = f(r["dbg_Vf"])
        v = v.transpose(0, 2, 1, 3).reshape(8, T, 128)
        ve = np.concatenate([v[0::2, :, 0:64], v[1::2, :, 64:128]], 0)[[0, 4, 1, 5, 2, 6, 3, 7]]
        print("vf err", relerr(ve[:, NPAD:], L0["vf"]))
        on = np.concatenate([v[0::2, :, 64:128], v[1::2, :, 0:64]], 0)
        print("v ones", on.min(), on.max())
        q = f(r["dbg_QTm"]); kk = f(r["dbg_KTm"])
        print("qm err", relerr(q[:, :, NPAD:].transpose(0, 2, 1), L0["qm"]), relerr(q[:, 64:, NPAD:].transpose(0, 2, 1), L0["qm"][:, :, 64:]))
        print("km err", relerr(kk[:, :, NPAD:].transpose(0, 2, 1), L0["km"]), relerr(kk[:, 64:, NPAD:].transpose(0, 2, 1), L0["km"][:, :, 64:]))
        v = f(r["dbg_Vm"]).transpose(0, 2, 1, 3).reshape(8, T, 128)
        ve = np.concatenate([v[0::2, :, 0:64], v[1::2, :, 64:128]], 0)[[0, 4, 1, 5, 2, 6, 3, 7]]
        print("vm err", relerr(ve[:, NPAD:], L0["vm"]))
```

```python
import numpy as np
import concourse.bass as bass
import concourse.mybir as mybir
from concourse.bass_utils import run_bass_kernel_spmd

F32 = mybir.dt.float32
BF16 = mybir.dt.bfloat16
AF = mybir.ActivationFunctionType
ALU = mybir.AluOpType
AX = mybir.AxisListType

D = 1024
KC = 8
SEQ = 4096
NMETA = 16
NPAD = 112
T = SEQ + 128
NT = T // 128
DEPTH = 2
NE = 16
DE = 256
IN_COLS = 4264
ALPHA = (2 * DEPTH) ** 0.25
LN_EPS = 1e-5
RMS_EPS = 1e-6
NEG = -30000.0
GROUPS = [(g * 512, min(512, T - g * 512)) for g in range((T + 511) // 512)]
AGROUPS = [(0, 128)] + [(128 + 512 * g, 512) for g in range(8)]
OQ, OK_, OV, OF, OCQ, OCKV, OKR, OGF, OGM = 0, 512, 1024, 1536, 1544, 1928, 2184, 2216, 3240


class Op:
    __slots__ = ("eng", "meth", "args", "kw", "dma", "idx", "marked", "waits", "dsem", "dval", "ticket")

    def __init__(self, eng, meth, args, kw, dma):
        self.eng, self.meth, self.args, self.kw, self.dma = eng, meth, args, kw, dma
        self.marked = False
        self.waits = []
        self.dsem = None
        self.dval = 0


class Rec:
    ENGS = ("pe", "act", "dve", "pool", "sp")
    NQ = 12

    def __init__(self):
        self.ops = {e: [] for e in self.ENGS}
        self.state = {}
        self.seen = {e: {p: -1 for p in self.ENGS} for e in self.ENGS}
        self.seen_dma = {e: {} for e in self.ENGS}
        self.ndma = {e: 0 for e in self.ENGS}
        self.out_dmas = []
        self.pending = {}

    def _st(self, key):
        buf, sub = key if isinstance(key, tuple) else (key, None)
        d = self.state.setdefault(buf, {})
        return buf, sub, d

    def _deps(self, reads, writes):
        deps = []
        for key in reads:
            buf, sub, d = self._st(key)
            subs = list(d.keys()) if sub is None else [s for s in (sub, None) if s in d]
            for s in subs:
                if d[s][0] is not None:
                    deps.append(d[s][0])
        for key in writes:
            buf, sub, d = self._st(key)
            subs = list(d.keys()) if sub is None else [s for s in (sub, None) if s in d]
            for s in subs:
                if d[s][0] is not None:
                    deps.append(d[s][0])
                deps.extend(d[s][1])
        return deps

    def _update(self, op, reads, writes):
        for key in reads:
            buf, sub, d = self._st(key)
            if sub is None:
                for s in list(d.keys()):
                    d[s][1].append(op)
                d.setdefault(None, [None, []])
                if op not in d[None][1]:
                    d[None][1].append(op)
            else:
                d.setdefault(sub, [None, []])[1].append(op)
        for key in writes:
            buf, sub, d = self._st(key)
            if sub is None:
                d.clear()
                d[None] = [op, []]
            else:
                d[sub] = [op, []]

    def op(self, eng, meth, *args, r=(), w=(), dma=False, final=False, **kw):
        o = Op(eng, meth, args, kw, dma)
        lst = self.ops[eng]
        o.idx = len(lst)
        if self.pending.get(eng):
            o.waits.extend(self.pending[eng])
            self.pending[eng] = []
        for dpo in self._deps(r, w):
            if dpo is o:
                continue
            if dpo.dma:
                sd = self.seen_dma[eng]
                if sd.get(dpo.dsem, 0) >= dpo.dval:
                    continue
                sd[dpo.dsem] = dpo.dval
                o.waits.append(dpo)
            else:
                if dpo.eng == eng and eng in ("pe", "sp"):
                    continue
                if self.seen[eng][dpo.eng] >= dpo.idx:
                    continue
                self.seen[eng][dpo.eng] = dpo.idx
                dpo.marked = True
                o.waits.append(dpo)
        if dma:
            j = self.ndma[eng]
            self.ndma[eng] = j + 1
            o.dsem = (eng, j % self.NQ)
            o.dval = 16 * (j // self.NQ + 1)
            prev = 16 * (j // self.NQ)
            if prev > 0 and self.seen_dma[eng].get(o.dsem, 0) < prev:
                self.seen_dma[eng][o.dsem] = prev
                o.waits.append(("dma", o.dsem, prev))
            if final:
                self.out_dmas.append(o)
        lst.append(o)
        self._update(o, r, w)
        return o

    def barrier(self):
        lastop = {}
        for e in self.ENGS:
            for o in reversed(self.ops[e]):
                if not o.dma:
                    lastop[e] = o
                    break
        dmas = {}
        for e in self.ENGS:
            for o in self.ops[e]:
                if o.dma:
                    dmas[o.dsem] = max(dmas.get(o.dsem, 0), o.dval)
        for e in self.ENGS:
            pw = self.pending.setdefault(e, [])
            for p, o in lastop.items():
                if p == e and e in ("pe", "sp"):
                    continue
                if self.seen[e][p] >= o.idx:
                    continue
                self.seen[e][p] = o.idx
                o.marked = True
                pw.append(o)
            for ds, v in dmas.items():
                if self.seen_dma[e].get(ds, 0) < v:
                    self.seen_dma[e][ds] = v
                    pw.append(("dma", ds, v))
        self.state = {}

    def replay(self, nc, sems, dsems):
        engs = {"pe": "tensor", "act": "scalar", "dve": "vector", "pool": "gpsimd", "sp": "sync"}
        for e in self.ENGS:
            t = 0
            for o in self.ops[e]:
                if o.marked:
                    t += 1
                o.ticket = t
        with nc.Block() as block:
            for e in self.ENGS:
                ops = self.ops[e]
                last = (e == "sp")
                outs = self.out_dmas

                def body(eng, ops=ops, e=e, last=last):
                    for o in ops:
                        for wdep in o.waits:
                            if isinstance(wdep, tuple):
                                eng.wait_ge(dsems[wdep[1]], wdep[2])
                            elif wdep.dma:
                                eng.wait_ge(dsems[wdep.dsem], wdep.dval)
                            else:
                                eng.wait_ge(sems[wdep.eng], wdep.ticket)
                        ins = getattr(eng, o.meth)(*o.args, **o.kw)
                        if o.dma:
                            ins.then_inc(dsems[o.dsem], 16)
                        elif o.marked:
                            ins.then_inc(sems[o.eng], 1)
                    if last:
                        done = {}
                        for o in outs:
                            done[o.dsem] = max(done.get(o.dsem, 0), o.dval)
                        for s, v in done.items():
                            eng.wait_ge(dsems[s], v)

                getattr(block, engs[e])(body)


class K:
    def __init__(self, upto=99, debug=False):
        self.upto = upto
        self.debug = debug
        self.nc = nc = bass.Bass("TRN2", target_bir_lowering=False)
        self.R = Rec()
        self.sb_off = 17536
        self.ph_off = None
        self.nalloc = 0
        self.inp = {}
        self.tcount = 0

    def dram_in(self, name, shape, dt=F32):
        h = self.nc.dram_tensor(name, list(shape), dt, kind="ExternalInput")
        self.inp[name] = h
        return h.ap()

    def dram_out(self, name, shape, dt=F32):
        return self.nc.dram_tensor(name, list(shape), dt, kind="ExternalOutput").ap()

    def dram(self, name, shape, dt):
        return self.nc.dram_tensor(name, list(shape), dt).ap()

    def sb(self, name, shape, dt):
        return self._at(name, shape, dt, "persist")

    def _at(self, name, shape, dt, region):
        esz = {F32: 4, BF16: 2}[dt]
        nbytes = int(np.prod(shape[1:])) * esz
        nbytes = (nbytes + 63) // 64 * 64
        if region == "persist":
            off = self.sb_off
            self.sb_off += nbytes
            assert self.ph_off is None, "persistent alloc after phase alloc"
        else:
            if self.ph_off is None:
                self.ph_off = self.sb_off
            off = self.ph_off
            self.ph_off += nbytes
        assert off + nbytes <= 228000, ("SBUF overflow", name, off + nbytes)
        self.nalloc += 1
        return self.nc.alloc_sbuf_tensor_at("%s_%d" % (name, self.nalloc), list(shape), dt, offset=off)

    def psb(self, name, shape, dt):
        return self._at(name, shape, dt, "phase")

    def new_phase(self):
        self.R.barrier()
        self.ph_off = self.sb_off

    def ps(self, name, shape, dt=F32):
        return self.nc.alloc_psum_tensor(name, list(shape), dt)

    def pe(self, meth, *a, **k):
        return self.R.op("pe", meth, *a, **k)

    def act(self, meth, *a, **k):
        return self.R.op("act", meth, *a, **k)

    def dve(self, meth, *a, **k):
        return self.R.op("dve", meth, *a, **k)

    def pool(self, meth, *a, **k):
        return self.R.op("pool", meth, *a, **k)

    def dma(self, out, in_, r=(), w=(), cast=False, final=False):
        eng = "pool" if cast else "sp"
        return self.R.op(eng, "dma_start", out=out, in_=in_, r=r, w=w, dma=True, final=final)


def bcast_row(ap_1d, n, parts=128):
    return bass.AP(ap_1d.tensor, ap_1d.offset, [[0, parts], [1, n]])


class Ctx:
    pass


def setup(k):
    nc = k.nc
    C = Ctx()
    k.C = C
    C.x = k.dram_in("x", [T, D])
    C.ln_in_g = k.dram_in("ln_in_g", [D]); C.ln_in_b = k.dram_in("ln_in_b", [D])
    C.w_in = k.dram_in("w_in", [DEPTH, D, IN_COLS])
    C.fox_f_bias = k.dram_in("fox_f_bias", [DEPTH, 8])
    C.fox_w_o = k.dram_in("fox_w_o", [DEPTH, 512, D])
    C.mla_q_norm = k.dram_in("mla_q_norm", [DEPTH, 384])
    C.mla_w_uq = k.dram_in("mla_w_uq", [DEPTH, 384, 768])
    C.mla_kv_norm = k.dram_in("mla_kv_norm", [DEPTH, 256])
    C.mla_w_ukv = k.dram_in("mla_w_ukv", [DEPTH, 256, 1024])
    C.mla_w_o = k.dram_in("mla_w_o", [DEPTH, 512, D])
    C.w_out = k.dram_in("w_out", [DEPTH, D, D])
    C.ln1_g = k.dram_in("ln1_g", [DEPTH, D]); C.ln1_b = k.dram_in("ln1_b", [DEPTH, D])
    C.router_w = k.dram_in("router_w", [D, NE]); C.router_b = k.dram_in("router_b", [NE])
    C.w_gate = k.dram_in("w_gate", [DEPTH, NE, D, DE])
    C.w_up = k.dram_in("w_up", [DEPTH, NE, D, DE])
    C.w_down = k.dram_in("w_down", [DEPTH, NE, DE, D])
    C.ln2_g = k.dram_in("ln2_g", [DEPTH, D]); C.ln2_b = k.dram_in("ln2_b", [DEPTH, D])
    C.c_ident = k.dram_in("c_ident", [128, 128])
    C.c_cmask = k.dram_in("c_cmask", [128, 128])
    C.c_pmask = k.dram_in("c_pmask", [128, 512])
    C.c_cmask0 = k.dram_in("c_cmask0", [128, 128])
    C.c_ut = k.dram_in("c_ut", [128, 128])
    C.c_padcol = k.dram_in("c_padcol", [128, 1])
    C.c_sel = k.dram_in("c_sel", [48, NE * 128])
    C.c_ones8 = k.dram_in("c_ones8", [8, T])
    C.c_cs = k.dram_in("c_cs", [128, T])
    C.out = k.dram_out("out", [SEQ, D])
    C.H = [k.dram("Hs%d" % i, [T, D], F32) for i in range(2)]
    C.QTf = k.dram("QTf", [8, 68, T], BF16); C.KTf = k.dram("KTf", [8, 68, T], BF16)
    C.QTm = k.dram("QTm", [8, 96, T], BF16); C.KTm = k.dram("KTm", [8, 96, T], BF16)
    C.Vf = k.dram("Vf", [8, 128, NT, 128], BF16); C.Vm = k.dram("Vm", [8, 128, NT, 128], BF16)
    C.OTf = k.dram("OTf", [128, 4, T], BF16); C.OTm = k.dram("OTm", [128, 4, T], BF16)
    C.psall = k.ps("psall", [128, 8, 512], F32)
    C.bank = [C.psall[:, i, :] for i in range(8)]
    C.hT = k.sb("hT", [128, KC, T], BF16)
    C.identb = k.sb("identb", [128, 128], BF16)
    C.identf = k.sb("identf", [128, 128], F32)
    C.padcol = k.sb("padcol", [128, 1], F32)
    C.onesf = k.sb("onesf", [128, 128], F32)
    C.utf = k.sb("utf", [128, 128], F32)
    C.cmask = k.sb("cmask", [128, 128], BF16)
    C.pmask = k.sb("pmask", [128, 512], BF16)
    C.cmask0 = k.sb("cmask0", [128, 128], BF16)
    C.ln_n = 0
    C.nhalf = k.sb("nhalf", [128, 2], F32)
    k.pool("memset", C.nhalf[:], -0.5, w=["nhalf"])
    k.dma(C.utf[:], C.c_ut, w=["utf"])
    k.dma(C.cmask[:], C.c_cmask, w=["cmask"], cast=True)
    k.dma(C.pmask[:], C.c_pmask, w=["pmask"], cast=True)
    k.dma(C.cmask0[:], C.c_cmask0, w=["cmask0"], cast=True)
    k.pool("memset", C.onesf[:], 1.0, w=["onesf"])
    for row in (64, 65, 66):
        k.dma(C.QTf[:, row, :], C.c_ones8, w=[("QTf", "ones%d" % row)], cast=True)
    k.dma(C.KTf[:, 67, :], C.c_ones8, w=[("KTf", "ones")], cast=True)
    k.dma(C.identb[:], C.c_ident, w=["identb"], cast=True)
    k.dma(C.identf[:], C.c_ident, w=["identf"])
    k.dma(C.padcol[:], C.c_padcol, w=["padcol"])
    return C


NLN = 4


def alloc_ln(k):
    C = k.C
    C.lnx = [k.psb("lnx%d" % i, [128, D], F32) for i in range(NLN)]
    C.hb = [k.psb("hb%d" % i, [128, D], BF16) for i in range(NLN)]
    C.lst = [k.psb("lst%d" % i, [128, 12], F32) for i in range(NLN)]
    C.lmv = [k.psb("lmv%d" % i, [128, 8], F32) for i in range(NLN)]
    C.gbc = k.psb("gbc", [128, D], F32); C.bbc = k.psb("bbc", [128, D], F32)
    C.ln_pending = []


def load_ln_params(k, g1d, b1d):
    C = k.C
    k.dma(C.gbc[:], bcast_row(g1d, D), w=["gbc"])
    k.dma(C.bbc[:], bcast_row(b1d, D), w=["bbc"])


def ln_slots(k, n):
    C = k.C
    ln_flush(k, 0)
    return list(range(n))


def ln_flush(k, keep=0):
    C = k.C
    while len(C.ln_pending) > keep:
        C.ln_pending.pop(0)()


def ln_batch(k, items):
    for _ in ln_batch_gen(k, items):
        pass


def ln_batch_gen(k, items):
    C = k.C
    def names(s):
        return C.lnx[s], C.lst[s], C.lmv[s], C.hb[s], ("lnx", s), ("lst", s), ("lmv", s), ("hb", s)
    half = (len(items) + 1) // 2
    for part in (items[:half], items[half:]):
        for (s, i, out_ap, outkey, final) in part:
            x, st, mv, hb, X, ST, MV, HB = names(s)
            k.dve("bn_stats", out=st[:, 0:6], in_=x[:, 0:512], r=[X], w=[ST])
            k.dve("bn_stats", out=st[:, 6:12], in_=x[:, 512:1024], r=[X], w=[ST])
            k.dve("bn_aggr", out=mv[:, 0:2], in_=st[:, 0:12], r=[ST], w=[MV])
        yield
    for (s, i, out_ap, outkey, final) in items:
        x, st, mv, hb, X, ST, MV, HB = names(s)
        k.pool("tensor_scalar", out=mv[:, 2:3], in0=mv[:, 1:2], scalar1=1.0, scalar2=LN_EPS, op0=ALU.mult, op1=ALU.add, r=[MV], w=[MV])
        k.pool("tensor_tensor", out=mv[:, 4:5], in0=mv[:, 2:3], in1=C.nhalf[:, 0:1], op=ALU.pow, r=[MV, "nhalf"], w=[MV])
    yield
    for (s, i, out_ap, outkey, final) in items:
        x, st, mv, hb, X, ST, MV, HB = names(s)
        k.dve("scalar_tensor_tensor", out=mv[:, 5:6], in0=mv[:, 0:1], scalar=-1.0, in1=mv[:, 4:5],
              op0=ALU.mult, op1=ALU.mult, r=[MV], w=[MV])
    for (s, i, out_ap, outkey, final) in items:
        x, st, mv, hb, X, ST, MV, HB = names(s)
        k.act("activation", out=x[:], in_=x[:], func=AF.Identity, scale=mv[:, 4:5], bias=mv[:, 5:6], r=[X, MV], w=[X])
    yield
    for (s, i, out_ap, outkey, final) in items:
        x, st, mv, hb, X, ST, MV, HB = names(s)
        k.dve("tensor_tensor", out=x[:], in0=x[:], in1=C.gbc[:], op=ALU.mult, r=[X, "gbc"], w=[X])
    yield
    for (s, i, out_ap, outkey, final) in items:
        x, st, mv, hb, X, ST, MV, HB = names(s)
        k.pool("tensor_tensor", out=x[:], in0=x[:], in1=C.bbc[:], op=ALU.add, r=[X, "bbc"], w=[X])
    yield
    for (s, i, out_ap, outkey, final) in items:
        x, st, mv, hb, X, ST, MV, HB = names(s)
        k.act("activation", out=hb[:], in_=x[:], func=AF.Copy, r=[X], w=[HB])
        k.dma(out_ap, x[:], r=[X], w=[outkey], final=final)
        C.ln_n += 1

        def deferred(i=i, hb=hb, HB=HB):
            bk = 6 + (i % 2)
            pst = C.bank[bk][:].bitcast(BF16)
            for kc in range(KC):
                k.pe("transpose", out=pst[:, kc * 128:(kc + 1) * 128], in_=hb[:, kc * 128:(kc + 1) * 128],
                     identity=C.identb[:], r=[HB, "identb"], w=[("bank", bk)])
            k.act("activation", out=C.hT[:, :, i * 128:(i + 1) * 128],
                  in_=pst.rearrange("p (k t) -> p k t", k=KC), func=AF.Copy,
                  r=[("bank", bk)], w=[("hT", i)])
        C.ln_pending.append(deferred)
    yield


def phase0(k):
    C = k.C
    k.new_phase()
    alloc_ln(k)
    load_ln_params(k, C.ln_in_g, C.ln_in_b)
    for i0 in range(0, NT, NLN):
        tiles = list(range(i0, min(NT, i0 + NLN)))
        sl = ln_slots(k, len(tiles))
        items = []
        for s, i in zip(sl, tiles):
            k.dma(C.lnx[s][:], C.x[i * 128:(i + 1) * 128, :], w=[("lnx", s)])
            items.append((s, i, C.H[0][i * 128:(i + 1) * 128, :], ("H0", i), False))
        ln_batch(k, items)
    ln_flush(k)


def evac_v(k, src_bank_ap, vst, s, bankkey, dst_dram, i):
    C = k.C
    sv = src_bank_ap.rearrange("p (m two d) -> p m two d", two=2, d=64)
    k.act("activation", out=vst[:, :, 0, 0:64], in_=sv[:, :, 0, :], func=AF.Copy, r=[bankkey], w=[("vst", s)])
    k.dve("tensor_copy", out=vst[:, :, 1, 64:128], in_=sv[:, :, 1, :], r=[bankkey], w=[("vst", s)])
    k.dma(dst_dram[:, :, i, :].rearrange("h p d -> p h d"), vst[:].rearrange("p m two d -> p (m two) d"),
          r=[("vst", s)], w=[("V", i)])


def phase1(k, l, with_p0=False):
    C = k.C
    k.new_phase()
    win = C.w_in[l].rearrange("(kc p) c -> p kc c", p=128)
    wv = k.psb("wv", [128, KC, 512], BF16)
    wcqf = k.psb("wcqf", [128, KC, 392], BF16)
    wckv = k.psb("wckv", [128, KC, 256], BF16)
    wt = [k.psb("wt%d" % i, [128, KC, 128], BF16) for i in range(2)]
    wkr = k.psb("wkr", [128, KC, 32], BF16); wkrR = k.psb("wkrR", [128, KC, 32], BF16)
    vst = [k.psb("vst%d" % i, [128, 4, 2, 128], BF16) for i in range(2)]
    cqnT = k.psb("cqnT", [128, 3, T], BF16); ckvnT = k.psb("ckvnT", [128, 2, T], BF16)
    augT = k.psb("augT", [32, T], BF16)
    fbias = k.psb("fbias", [128, 8], F32)
    sm = [k.psb("sm%d" % i, [128, 64], F32) for i in range(2)]
    junk = k.psb("junk", [128, 384], BF16)
    cqn = [k.psb("cqn%d" % i, [128, 640], BF16) for i in range(2)]
    stg = [k.psb("stg%d" % i, [128, 512], BF16) for i in range(3)]
    rt = [k.psb("rt%d" % i, [128, 512], F32) for i in range(2)]
    gq = k.psb("gq", [128, 8], F32)
    z_all = k.psb("z_all", [128, NT, 8], F32)
    lf_all = k.psb("lf_all", [128, NT, 8], F32)
    carry = k.psb("carry", [128, NT, 8], F32)
    aug_all = k.psb("aug_all", [128, NT, 32], F32)
    hb_all = k.psb("hb_all", [128, NT, 8], BF16)
    ov_off = k.ph_off
    cs = k.psb("cs", [128, T], F32)
    wuq = k.psb("wuq", [128, 3, 768], BF16); wuqR = k.psb("wuqR", [128, 3, 768], BF16)
    wukv = k.psb("wukv", [128, 2, 1024], BF16)
    stgw = k.psb("stgw", [128, 1024], F32)
    ALIAS = ["lnx", "hb", "lst", "lmv", "gbc", "bbc"] if with_p0 else []

    k.dma(wv[:], win[:, :, OV:OV + 512], w=["wv"], cast=True)
    k.dma(wcqf[:], win[:, :, OF:OF + 392], w=["wcqf"], cast=True)
    k.dma(wckv[:], win[:, :, OCKV:OCKV + 256], w=["wckv"], cast=True)
    k.dma(wkr[:], win[:, :, OKR:OKR + 32], w=["wkr"], cast=True)
    k.dma(fbias[:], bcast_row(C.fox_f_bias[l], 8), w=["fbias"])
    for kc in range(3):
        k.dma(gq[:, kc:kc + 1], C.mla_q_norm[l][kc * 128:(kc + 1) * 128].rearrange("(p o) -> p o", o=1), w=["gq"])
    for kc in range(2):
        k.dma(gq[:, 4 + kc:5 + kc], C.mla_kv_norm[l][kc * 128:(kc + 1) * 128].rearrange("(p o) -> p o", o=1), w=["gq"])
    for s_ in range(2):
        k.pool("memset", vst[s_][:, :, 0, 64:128], 1.0, w=[("vst", s_)])
        k.pool("memset", vst[s_][:, :, 1, 0:64], 1.0, w=[("vst", s_)])
    k.dve("tensor_scalar", out=wkrR[:, :, 0:16], in0=wkr[:, :, 16:32], scalar1=-1.0, scalar2=None, op0=ALU.mult, r=["wkr"], w=["wkrR"])
    k.dve("tensor_copy", out=wkrR[:, :, 16:32], in_=wkr[:, :, 0:16], r=["wkr"], w=["wkrR"])

    def mm_tile(i):
        s_ = i % 2
        tc = slice(i * 128, (i + 1) * 128)
        b0 = 3 * s_
        for (bk, w_, wn, ncol) in ((b0, wv, "wv", 512), (b0 + 1, wcqf, "wcqf", 392), (b0 + 2, wckv, "wckv", 256)):
            for kc in range(KC):
                k.pe("matmul", C.bank[bk][:, 0:ncol], lhsT=C.hT[:, kc, tc], rhs=w_[:, kc, :], start=(kc == 0), stop=(kc == KC - 1),
                     r=[("hT", i), wn], w=[("bank", bk)])

    def chain_tile(i):
        s_ = i % 2
        tc = slice(i * 128, (i + 1) * 128)
        b0 = 3 * s_
        bv, ba, bb = C.bank[b0], C.bank[b0 + 1], C.bank[b0 + 2]
        m = sm[s_]; M = ("sm", s_)
        k.act("activation", out=junk[:, 0:384], in_=ba[:, 8:392], func=AF.Square, scale=384 ** -0.5, accum_out=m[:, 0:1], r=[("bank", b0 + 1)], w=[M, "junk"])
        k.act("activation", out=junk[:, 0:256], in_=bb[:, 0:256], func=AF.Square, scale=256 ** -0.5, accum_out=m[:, 1:2], r=[("bank", b0 + 2)], w=[M, "junk"])
        evac_v(k, bv[:, 0:512], vst[s_], s_, ("bank", b0), C.Vf, i)
        k.dve("tensor_tensor", out=z_all[:, i, :], in0=ba[:, 0:8], in1=fbias[:], op=ALU.add, r=[("bank", b0 + 1), "fbias"], w=[("z_all", i)])
        k.pool("tensor_scalar", out=m[:, 2:4], in0=m[:, 0:2], scalar1=1.0, scalar2=RMS_EPS, op0=ALU.mult, op1=ALU.add, r=[M], w=[M])
        k.pool("tensor_tensor", out=m[:, 6:8], in0=m[:, 2:4], in1=C.nhalf[:, 0:2], op=ALU.pow, r=[M, "nhalf"], w=[M])
        cq = cqn[s_]; CQ = ("cqn", s_)
        k.dve("tensor_scalar", out=cq[:, 0:384], in0=ba[:, 8:392], scalar1=m[:, 6:7], scalar2=None, op0=ALU.mult, r=[M, ("bank", b0 + 1)], w=[CQ])
        k.dve("tensor_scalar", out=cq[:, 384:640], in0=bb[:, 0:256], scalar1=m[:, 7:8], scalar2=None, op0=ALU.mult, r=[M, ("bank", b0 + 2)], w=[CQ])
        pst = C.bank[6 + s_][:].bitcast(BF16)
        for j in range(5):
            k.pe("transpose", out=pst[:, j * 128:(j + 1) * 128], in_=cq[:, j * 128:(j + 1) * 128], identity=C.identb[:],
                 r=[CQ, "identb"], w=[("bank", 6 + s_)])
        k.act("activation", out=cqnT[:, :, tc], in_=pst[:, 0:384].rearrange("p (k t) -> p k t", k=3), func=AF.Copy,
              r=[("bank", 6 + s_)], w=[("cqnT", i)])
        k.act("activation", out=ckvnT[:, :, tc], in_=pst[:, 384:640].rearrange("p (k t) -> p k t", k=2), func=AF.Copy,
              r=[("bank", 6 + s_)], w=[("ckvnT", i)])

    pend_chain = []

    def stepA_push(i):
        mm_tile(i)
        if pend_chain:
            chain_tile(pend_chain.pop(0))
        pend_chain.append(i)

    if with_p0:
        end_off = k.ph_off
        k.ph_off = ov_off
        alloc_ln(k)
        assert k.ph_off <= end_off, "LN overlay too large"
        k.ph_off = end_off
        load_ln_params(k, C.ln_in_g, C.ln_in_b)
        prev_tiles = []
        for i0 in range(0, NT, NLN):
            tiles = list(range(i0, min(NT, i0 + NLN)))
            sl = ln_slots(k, len(tiles))
            for i in prev_tiles:
                stepA_push(i)
            items = []
            for s__, i in zip(sl, tiles):
                k.dma(C.lnx[s__][:], C.x[i * 128:(i + 1) * 128, :], w=[("lnx", s__)])
                items.append((s__, i, C.H[0][i * 128:(i + 1) * 128, :], ("H0", i), False))
            ln_batch(k, items)
            prev_tiles = tiles
        ln_flush(k)
        for i in prev_tiles:
            stepA_push(i)
    else:
        for i in range(NT):
            stepA_push(i)
    chain_tile(pend_chain.pop(0))

    NF = NT * 8
    zf = z_all[:].rearrange("p i h -> p (i h)")
    lff = lf_all[:].rearrange("p i h -> p (i h)")
    k.act("activation", out=lff, in_=zf, func=AF.Exp, scale=-1.0, r=["z_all"], w=["lf_all"])
    k.act("activation", out=lff, in_=lff, func=AF.Ln, bias=1.0, r=["lf_all"], w=["lf_all"])
    k.dve("tensor_scalar", out=lff, in0=lff, scalar1=-1.0, scalar2=None, op0=ALU.mult, r=["lf_all"], w=["lf_all"])
    k.dve("tensor_scalar", out=lf_all[:, 0, :], in0=lf_all[:, 0, :], scalar1=C.padcol[:, 0:1], scalar2=None, op0=ALU.mult, r=["lf_all", "padcol"], w=["lf_all"])
    k.pe("matmul", C.bank[0][:, 0:NF], lhsT=C.utf[:], rhs=lff, start=True, stop=True, r=["lf_all", "utf"], w=[("bank", 0)])
    k.pe("matmul", C.bank[1][:, 0:NF], lhsT=C.onesf[:], rhs=lff, start=True, stop=True, r=["lf_all", "onesf"], w=[("bank", 1)])
    tot = C.bank[1][:, 0:NF].rearrange("p (i h) -> p i h", h=8)
    k.dve("memset", carry[:, 0, :], 0.0, w=["carry"])
    for i in range(1, NT):
        k.dve("tensor_tensor", out=carry[:, i, :], in0=carry[:, i - 1, :], in1=tot[:, i - 1, :], op=ALU.add, r=["carry", ("bank", 1)], w=["carry"])
    au = aug_all
    c3 = C.bank[0][:, 0:NF].rearrange("p (i h) -> p i h", h=8)
    k.dve("tensor_tensor", out=au[:, :, 0:8], in0=c3, in1=carry[:], op=ALU.add, r=[("bank", 0), "carry"], w=["aug_all"])
    k.dve("tensor_scalar", out=lf_all[:], in0=au[:, :, 0:8], scalar1=-1.0, scalar2=None, op0=ALU.mult, r=["aug_all"], w=["lf_all"])
    k.dve("tensor_copy", out=hb_all[:], in_=lf_all[:], r=["lf_all"], w=["hb_all"])
    k.dve("tensor_copy", out=au[:, :, 8:16], in_=hb_all[:], r=["hb_all"], w=["aug_all"])
    k.dve("tensor_tensor", out=lf_all[:], in0=lf_all[:], in1=au[:, :, 8:16], op=ALU.subtract, r=["lf_all", "aug_all"], w=["lf_all"])
    k.dve("tensor_copy", out=hb_all[:], in_=lf_all[:], r=["lf_all"], w=["hb_all"])
    k.dve("tensor_copy", out=au[:, :, 16:24], in_=hb_all[:], r=["hb_all"], w=["aug_all"])
    k.dve("tensor_tensor", out=au[:, :, 24:32], in0=lf_all[:], in1=au[:, :, 16:24], op=ALU.subtract, r=["lf_all", "aug_all"], w=["aug_all"])
    for i0 in range(0, NT, 4):
        n4 = min(4, NT - i0)
        bk = 2 + (i0 // 4) % 2
        for j in range(n4):
            k.pe("transpose", out=C.bank[bk][0:32, j * 128:(j + 1) * 128], in_=au[:, i0 + j, :], identity=C.identf[:],
                 r=["aug_all", "identf"], w=[("bank", bk)])
        k.dve("tensor_copy", out=augT[:, i0 * 128:(i0 + n4) * 128], in_=C.bank[bk][0:32, 0:n4 * 128], r=[("bank", bk)], w=["augT"])
    k.dma(C.QTf[:, 67, :], augT[0:8, :], r=["augT"], w=[("QTf", "c")])
    k.dma(C.KTf[:, 64, :], augT[8:16, :], r=["augT"], w=[("KTf", "hi")])
    k.dma(C.KTf[:, 65, :], augT[16:24, :], r=["augT"], w=[("KTf", "mid")])
    k.dma(C.KTf[:, 66, :], augT[24:32, :], r=["augT"], w=[("KTf", "lo")])

    k.dma(cs[:], C.c_cs, w=["cs"] + ALIAS)
    cnt = 0
    for c in range(8):
        s_ = c % 2
        k.dma(wt[s_][:], win[:, :, c * 128:(c + 1) * 128], w=[("wt", s_)], cast=True)
        dst = C.QTf if c < 4 else C.KTf
        dn = "QTf" if c < 4 else "KTf"
        h0 = 2 * (c % 4)
        for gi, (g0, n) in enumerate(GROUPS):
            bk = cnt % 6
            st_ = stg[cnt % 3]; SG = ("stg", cnt % 3)
            cnt += 1
            for kc in range(KC):
                k.pe("matmul", C.bank[bk][:, 0:n], lhsT=wt[s_][:, kc, :], rhs=C.hT[:, kc, g0:g0 + n], start=(kc == 0), stop=(kc == KC - 1),
                     r=[("wt", s_), "hT"], w=[("bank", bk)])
            if c < 4:
                k.act("activation", out=st_[:, 0:n], in_=C.bank[bk][:, 0:n], func=AF.Copy, scale=0.125, r=[("bank", bk)], w=[SG])
            else:
                k.dve("tensor_copy", out=st_[:, 0:n], in_=C.bank[bk][:, 0:n], r=[("bank", bk)], w=[SG])
            k.dma(dst[h0, 0:64, g0:g0 + n], st_[0:64, 0:n], r=[SG], w=[(dn, (h0, gi))])
            k.dma(dst[h0 + 1, 0:64, g0:g0 + n], st_[64:128, 0:n], r=[SG], w=[(dn, (h0 + 1, gi))])
    for gi, (g0, n) in enumerate(GROUPS):
        bA, bB = 6, 7
        for kc in range(KC):
            k.pe("matmul", C.bank[bA][0:32, 0:n], lhsT=wkr[:, kc, :], rhs=C.hT[:, kc, g0:g0 + n], start=(kc == 0), stop=(kc == KC - 1),
                 r=["wkr", "hT"], w=[("bank", bA)])
        for kc in range(KC):
            k.pe("matmul", C.bank[bB][0:32, 0:n], lhsT=wkrR[:, kc, :], rhs=C.hT[:, kc, g0:g0 + n], start=(kc == 0), stop=(kc == KC - 1),
                 r=["wkrR", "hT"], w=[("bank", bB)])
        st_ = stg[gi % 3]; SG = ("stg", gi % 3)
        k.dve("tensor_tensor", out=rt[0][0:32, 0:n], in0=C.bank[bA][0:32, 0:n], in1=cs[0:32, g0:g0 + n], op=ALU.mult, r=[("bank", bA), "cs"], w=[("rt", 0)])
        k.dve("tensor_tensor", out=rt[1][0:32, 0:n], in0=C.bank[bB][0:32, 0:n], in1=cs[32:64, g0:g0 + n], op=ALU.mult, r=[("bank", bB), "cs"], w=[("rt", 1)])
        k.dve("tensor_tensor", out=st_[0:32, 0:n], in0=rt[0][0:32, 0:n], in1=rt[1][0:32, 0:n], op=ALU.add, r=[("rt", 0), ("rt", 1)], w=[SG])
        for h in range(8):
            k.dma(C.KTm[h, 64:96, g0:g0 + n], st_[0:32, 0:n], r=[SG], w=[("KTm", (h, gi, "r"))])

    for kc in range(3):
        k.dma(stgw[:, 0:768], C.mla_w_uq[l][kc * 128:(kc + 1) * 128, :], w=["stgw"] + ALIAS)
        k.dve("tensor_scalar", out=wuq[:, kc, :], in0=stgw[:, 0:768], scalar1=gq[:, kc:kc + 1], scalar2=None, op0=ALU.mult, r=["stgw", "gq"], w=["wuq"] + ALIAS)
        k.dve("tensor_copy", out=wuqR[:, kc, :], in_=wuq[:, kc, :], r=["wuq"], w=["wuqR"] + ALIAS)
        wv_ = wuq[:, kc, :].rearrange("p (h d) -> p h d", d=96)
        wr_ = wuqR[:, kc, :].rearrange("p (h d) -> p h d", d=96)
        k.dve("tensor_scalar", out=wr_[:, :, 64:80], in0=wv_[:, :, 80:96], scalar1=-1.0, scalar2=None, op0=ALU.mult, r=["wuq"], w=["wuqR"])
        k.dve("tensor_copy", out=wr_[:, :, 80:96], in_=wv_[:, :, 64:80], r=["wuq"], w=["wuqR"])
    for kc in range(2):
        k.dma(stgw[:, 0:1024], C.mla_w_ukv[l][kc * 128:(kc + 1) * 128, :], w=["stgw"])
        k.dve("tensor_scalar", out=wukv[:, kc, :], in0=stgw[:, 0:1024], scalar1=gq[:, 4 + kc:5 + kc], scalar2=None, op0=ALU.mult, r=["stgw", "gq"], w=["wukv"] + ALIAS)
    cnt = 0
    for h in range(8):
        for gi, (g0, n) in enumerate(GROUPS):
            bA, bB, bC = 3 * (cnt % 2), 3 * (cnt % 2) + 1, 3 * (cnt % 2) + 2
            qs = stg[cnt % 3]; QS = ("stg", cnt % 3)
            cnt += 1
            for kc in range(3):
                k.pe("matmul", C.bank[bA][0:96, 0:n], lhsT=wuq[:, kc, h * 96:(h + 1) * 96], rhs=cqnT[:, kc, g0:g0 + n], start=(kc == 0), stop=(kc == 2),
                     r=["wuq", "cqnT"], w=[("bank", bA)])
            for kc in range(3):
                k.pe("matmul", C.bank[bB][0:96, 0:n], lhsT=wuqR[:, kc, h * 96:(h + 1) * 96], rhs=cqnT[:, kc, g0:g0 + n], start=(kc == 0), stop=(kc == 2),
                     r=["wuqR", "cqnT"], w=[("bank", bB)])
            for kc in range(2):
                k.pe("matmul", C.bank[bC][0:64, 0:n], lhsT=wukv[:, kc, h * 64:(h + 1) * 64], rhs=ckvnT[:, kc, g0:g0 + n], start=(kc == 0), stop=(kc == 1),
                     r=["wukv", "ckvnT"], w=[("bank", bC)])
            k.act("activation", out=qs[0:64, 0:n], in_=C.bank[bA][0:64, 0:n], func=AF.Copy, r=[("bank", bA)], w=[QS])
            k.dve("tensor_tensor", out=rt[0][64:96, 0:n], in0=C.bank[bA][64:96, 0:n], in1=cs[64:96, g0:g0 + n], op=ALU.mult, r=[("bank", bA), "cs"], w=[("rt", 0)])
            k.dve("tensor_tensor", out=rt[1][64:96, 0:n], in0=C.bank[bB][64:96, 0:n], in1=cs[96:128, g0:g0 + n], op=ALU.mult, r=[("bank", bB), "cs"], w=[("rt", 1)])
            k.dve("tensor_tensor", out=qs[64:96, 0:n], in0=rt[0][64:96, 0:n], in1=rt[1][64:96, 0:n], op=ALU.add, r=[("rt", 0), ("rt", 1)], w=[QS])
            k.dma(C.QTm[h, :, g0:g0 + n], qs[0:96, 0:n], r=[QS], w=[("QTm", (h, gi))])
            ks = stg[cnt % 3]; KS = ("stg", cnt % 3)
            cnt += 1
            k.act("activation", out=ks[0:64, 0:n], in_=C.bank[bC][0:64, 0:n], func=AF.Copy, r=[("bank", bC)], w=[KS])
            k.dma(C.KTm[h, 0:64, g0:g0 + n], ks[0:64, 0:n], r=[KS], w=[("KTm", (h, gi, "n"))])
    for i in range(NT):
        s_ = i % 2
        tc = slice(i * 128, (i + 1) * 128)
        bk = 6 + s_
        for kc in range(2):
            k.pe("matmul", C.bank[bk][:, 0:512], lhsT=ckvnT[:, kc, tc], rhs=wukv[:, kc, 512:1024], start=(kc == 0), stop=(kc == 1),
                 r=["wukv", ("ckvnT", i)], w=[("bank", bk)])
        evac_v(k, C.bank[bk][:, 0:512], vst[s_], s_, ("bank", bk), C.Vm, i)


def attention(k, QT, KT, V, Dk, scale, OT, nm):
    C = k.C
    k.new_phase()
    qT = [k.psb("qT%d" % i, [Dk, 2, T], BF16) for i in range(2)]
    kT = [k.psb("kT%d" % i, [Dk, 2, T], BF16) for i in range(2)]
    vv = [k.psb("vv%d" % i, [128, 2, NT, 128], BF16) for i in range(2)]
    pb = [k.psb("pb%d" % i, [128, 2, 512], BF16) for i in range(2)]
    rr = [k.psb("rr%d" % i, [128, 512], F32) for i in range(2)]
    osb = [k.psb("osb%d" % i, [128, 512], F32) for i in range(2)]
    pb.append(k.psb("pb2", [128, 2, 512], BF16))
    ost = [k.psb("ost%d" % i, [128, 512], BF16) for i in range(2)]
    step = 0
    pend = []

    def drain(keep):
        while len(pend) > keep:
            f, post = pend.pop(0)
            f()
            if post is not None:
                post()

    def load_pair(hp):
        s_ = hp % 2
        for j in range(2):
            h = 2 * hp + j
            k.dma(qT[s_][:, j, :], QT[h], r=["QTf", "QTm"], w=[("qT", s_)])
            k.dma(kT[s_][:, j, :], KT[h], r=["KTf", "KTm"], w=[("kT", s_)])
            k.dma(vv[s_][:, j, :, :], V[h], r=["V"], w=[("vv", s_)])

    load_pair(0)
    for hp in range(4):
        s_ = hp % 2
        for gi, (g0, n) in enumerate(AGROUPS):
            if gi == 2 and hp + 1 < 4:
                load_pair(hp + 1)
            qb0 = g0 // 128
            nkb = qb0 + n // 128
            ob = 6

            def make_pv(pkb, plo, pnn, psl, last, s_=s_):
                def f():
                    for j in range(2):
                        k.pe("matmul", C.bank[ob + j][:, plo:plo + pnn], lhsT=vv[s_][:, j, pkb, :], rhs=pb[psl][:, j, 0:pnn],
                             start=(pkb == 0), stop=last, r=[("vv", s_), ("pb", psl)], w=[("bank", ob + j)])
                return f

            def make_post(hp=hp, gi=gi, g0=g0, n=n):
                def post():
                    slots = []
                    for j in range(2):
                        r_ = rr[j]; RR = ("rr", j)
                        if j == 0:
                            po, pz = slice(0, 64), slice(64, 128)
                        else:
                            po, pz = slice(64, 128), slice(0, 64)
                        k.dve("tensor_copy", out=osb[j][po, 0:n], in_=C.bank[ob + j][po, 0:n], r=[("bank", ob + j)], w=[("osb", j)])
                        k.dve("tensor_copy", out=r_[po, 0:n], in_=C.bank[ob + j][pz, 0:n], r=[("bank", ob + j)], w=[RR])
                        slots.append((j, r_, RR, po))
                    for (j, r_, RR, po) in slots:
                        h = 2 * hp + j
                        os_ = ost[j]; OS = ("ost", j)
                        k.dve("reciprocal", out=r_[po, 0:n], in_=r_[po, 0:n], r=[RR], w=[RR])
                        k.dve("tensor_tensor", out=os_[po, 0:n], in0=osb[j][po, 0:n], in1=r_[po, 0:n], op=ALU.mult, r=[("osb", j), RR], w=[OS])
                        k.dma(OT[po, hp, g0:g0 + n], os_[po, 0:n], r=[OS], w=[(nm, (h, gi))])
                return post

            for kb in range(nkb):
                lo = max(0, kb * 128 - g0)
                nn = n - lo
                sl_ = step % 3
                step += 1
                diag = kb >= qb0
                for j in range(2):
                    bk = 2 * sl_ + j
                    k.pe("matmul", C.bank[bk][:, 0:nn], lhsT=kT[s_][:, j, kb * 128:(kb + 1) * 128], rhs=qT[s_][:, j, g0 + lo:g0 + n],
                         start=True, stop=not (diag or kb == 0), r=[("kT", s_), ("qT", s_)], w=[("bank", bk)])
                    if kb == 0:
                        k.pe("matmul", C.bank[bk][:, 0:nn], lhsT=C.identb[:], rhs=C.pmask[:, 0:nn], start=False, stop=not diag,
                             r=["identb", "pmask"], w=[("bank", bk)])
                    if diag:
                        cm = C.cmask0 if kb == 0 else C.cmask
                        k.pe("matmul", C.bank[bk][:, 0:128], lhsT=C.identb[:], rhs=cm[:], start=False, stop=True,
                             r=["identb", "cmask", "cmask0"], w=[("bank", bk)])
                k.act("activation", out=pb[sl_][:, :, 0:nn], in_=C.psall[:, 2 * sl_:2 * sl_ + 2, 0:nn], func=AF.Exp, scale=scale,
                      r=[("bank", 2 * sl_), ("bank", 2 * sl_ + 1)], w=[("pb", sl_)])
                last = (kb == nkb - 1)
                pend.append((make_pv(kb, lo, nn, sl_, last), make_post() if last else None))
                drain(2)
    drain(0)


def phase3(k, l, Hin, hin_name, Hout, hout_name):
    C = k.C
    k.new_phase()
    alloc_ln(k)
    load_ln_params(k, C.ln1_g[l], C.ln1_b[l])
    win = C.w_in[l].rearrange("(kc p) c -> p kc c", p=128)
    wgf = k.psb("wgf", [128, KC, D], BF16); wgm = k.psb("wgm", [128, KC, D], BF16)
    wof = k.psb("wof", [128, 4, D], BF16); wom = k.psb("wom", [128, 4, D], BF16)
    wo = k.psb("wo", [128, KC, D], BF16)
    otf = [k.psb("otf%d" % i, [128, 4, 512], BF16) for i in range(2)]
    otm = [k.psb("otm%d" % i, [128, 4, 512], BF16) for i in range(2)]
    mT = k.psb("mT", [128, KC, 512], BF16)
    sg = [k.psb("sg%d" % i, [128, 512], BF16) for i in range(4)]
    tt = [k.psb("tt%d" % i, [128, 512], F32) for i in range(4)]
    k.dma(wgf[:], win[:, :, OGF:OGF + D], w=["wgf"], cast=True)
    k.dma(wgm[:], win[:, :, OGM:OGM + D], w=["wgm"], cast=True)
    k.dma(wof[:], C.fox_w_o[l].rearrange("(m p) c -> p m c", p=128), w=["wof"], cast=True)
    k.dma(wom[:], C.mla_w_o[l].rearrange("(m p) c -> p m c", p=128), w=["wom"], cast=True)
    k.dma(wo[:], C.w_out[l].rearrange("(kc p) c -> p kc c", p=128), w=["wo"], cast=True)
    cc = 0
    lngen = [None]

    def load_ot(gi_):
        g0_, n_ = GROUPS[gi_]
        k.dma(otf[gi_ % 2][:, :, 0:n_], C.OTf[:, :, g0_:g0_ + n_], r=["OTf"], w=[("otf", gi_ % 2)])
        k.dma(otm[gi_ % 2][:, :, 0:n_], C.OTm[:, :, g0_:g0_ + n_], r=["OTm"], w=[("otm", gi_ % 2)])

    for gi, (g0, n) in enumerate(GROUPS):
        s_ = gi % 2
        tiles = list(range(g0 // 128, (g0 + n) // 128))
        hk = [("hT", i) for i in tiles]
        if gi == 0:
            load_ot(0)
        if gi + 1 < len(GROUPS):
            load_ot(gi + 1)
        for c in range(KC):
            b0 = 4 * (cc % 2)
            q_ = 2 * (cc % 2)
            cc += 1
            cs_ = slice(c * 128, (c + 1) * 128)
            for kc in range(KC):
                k.pe("matmul", C.bank[b0][:, 0:n], lhsT=wgf[:, kc, cs_], rhs=C.hT[:, kc, g0:g0 + n], start=(kc == 0), stop=(kc == KC - 1),
                     r=["wgf"] + hk, w=[("bank", b0)])
            for kc in range(KC):
                k.pe("matmul", C.bank[b0 + 1][:, 0:n], lhsT=wgm[:, kc, cs_], rhs=C.hT[:, kc, g0:g0 + n], start=(kc == 0), stop=(kc == KC - 1),
                     r=["wgm"] + hk, w=[("bank", b0 + 1)])
            for m in range(4):
                k.pe("matmul", C.bank[b0 + 2][:, 0:n], lhsT=wof[:, m, cs_], rhs=otf[s_][:, m, 0:n], start=(m == 0), stop=(m == 3),
                     r=["wof", ("otf", s_)], w=[("bank", b0 + 2)])
            for m in range(4):
                k.pe("matmul", C.bank[b0 + 3][:, 0:n], lhsT=wom[:, m, cs_], rhs=otm[s_][:, m, 0:n], start=(m == 0), stop=(m == 3),
                     r=["wom", ("otm", s_)], w=[("bank", b0 + 3)])
            k.act("activation", out=sg[q_][:, 0:n], in_=C.bank[b0][:, 0:n], func=AF.Sigmoid, r=[("bank", b0)], w=[("sg", q_)])
            k.act("activation", out=sg[q_ + 1][:, 0:n], in_=C.bank[b0 + 1][:, 0:n], func=AF.Sigmoid, r=[("bank", b0 + 1)], w=[("sg", q_ + 1)])
            k.dve("tensor_tensor", out=tt[q_][:, 0:n], in0=C.bank[b0 + 2][:, 0:n], in1=sg[q_][:, 0:n], op=ALU.mult,
                  r=[("bank", b0 + 2), ("sg", q_)], w=[("tt", q_)])
            k.dve("tensor_tensor", out=tt[q_ + 1][:, 0:n], in0=C.bank[b0 + 3][:, 0:n], in1=sg[q_ + 1][:, 0:n], op=ALU.mult,
                  r=[("bank", b0 + 3), ("sg", q_ + 1)], w=[("tt", q_ + 1)])
            k.pool("tensor_tensor", out=mT[:, c, 0:n], in0=tt[q_][:, 0:n], in1=tt[q_ + 1][:, 0:n], op=ALU.add,
                   r=[("tt", q_), ("tt", q_ + 1)], w=[("mT", c)])
            if lngen[0] is not None:
                if next(lngen[0], "done") == "done":
                    lngen[0] = None
        if lngen[0] is not None:
            for _ in lngen[0]:
                pass
            lngen[0] = None
        sl = ln_slots(k, len(tiles))
        items = []
        for j, i in enumerate(tiles):
            ls = sl[j]
            k.dma(C.lnx[ls][:], Hin[i * 128:(i + 1) * 128, :], r=[(hin_name, i)], w=[("lnx", ls)])
            for half in range(2):
                bk = 2 * (j % 2) + half
                for kc in range(KC):
                    k.pe("matmul", C.bank[bk][:, 0:512], lhsT=mT[:, kc, j * 128:(j + 1) * 128], rhs=wo[:, kc, half * 512:(half + 1) * 512],
                         start=(kc == 0), stop=(kc == KC - 1), r=["mT", "wo"], w=[("bank", bk)])
                hs = slice(half * 512, (half + 1) * 512)
                k.dve("scalar_tensor_tensor", out=C.lnx[ls][:, hs], in0=C.lnx[ls][:, hs], scalar=ALPHA, in1=C.bank[bk][:, 0:512],
                      op0=ALU.mult, op1=ALU.add, r=[("lnx", ls), ("bank", bk)], w=[("lnx", ls)])
            items.append((ls, i, Hout[i * 128:(i + 1) * 128, :], (hout_name, i), False))
        lngen[0] = ln_batch_gen(k, items)
    for _ in lngen[0]:
        pass
    ln_flush(k)


def phase4(k, l, Hin, hin_name, Hout, hout_name, last):
    C = k.C
    k.new_phase()
    alloc_ln(k)
    load_ln_params(k, C.ln2_g[l], C.ln2_b[l])
    rw = k.psb("rw", [128, KC, NE], BF16)
    rb = k.psb("rb", [128, NE], F32)
    comb = k.psb("comb", [128, NT, NE], F32)
    wg = [k.psb("wg%d" % i, [128, KC, DE], BF16) for i in range(2)]
    wu = [k.psb("wu%d" % i, [128, KC, DE], BF16) for i in range(2)]
    wd = [k.psb("wd%d" % i, [128, 2, D], BF16) for i in range(2)]
    NSG = 11
    yacc = k.psb("yacc", [128, NSG, D], F32)
    hid = [k.psb("hid%d" % i, [128, 2, 512], BF16) for i in range(2)]
    sl = [k.psb("sl%d" % i, [128, 512], F32) for i in range(2)]
    k.dma(rw[:], C.router_w.rearrange("(kc p) e -> p kc e", p=128), w=["rw"], cast=True)
    k.dma(rb[:], bcast_row(C.router_b, NE), w=["rb"])
    NR = NT * NE
    ra = [k.psb("ra%d" % i, [128, NR], F32) for i in range(5)]
    rsm = k.psb("rsm", [128, NT * 4 * 4], F32)
    lg = C.psall[:, 6:8, :].rearrange("p b c -> p (b c)")
    for i in range(NT):
        tc = slice(i * 128, (i + 1) * 128)
        for kc in range(KC):
            k.pe("matmul", lg[:, i * NE:(i + 1) * NE], lhsT=C.hT[:, kc, tc], rhs=rw[:, kc, :], start=(kc == 0), stop=(kc == KC - 1),
                 r=[("hT", i), "rw"], w=[("bank", 6), ("bank", 7)])
    sc, b_, eq, b2, ge = [t[:] for t in ra]
    G4 = NT * 4
    m1 = rsm[:, 0:G4]; m2 = rsm[:, G4:2 * G4]; gs = rsm[:, 2 * G4:3 * G4]; gsel = rsm[:, 3 * G4:4 * G4]
    gmax = k.psb("gmax", [128, NT], F32); den = k.psb("den", [128, NT], F32)
    v3 = lambda ap: ap.rearrange("p (g e) -> p g e", e=4)
    bc4 = lambda ap: ap.unsqueeze(2).broadcast_to([128, G4, 4])
    k.act("activation", out=sc, in_=lg[:, 0:NR], func=AF.Sigmoid, r=[("bank", 6), ("bank", 7)], w=["ra0"])
    k.dve("tensor_tensor", out=b_.rearrange("p (i e) -> p i e", e=NE), in0=sc.rearrange("p (i e) -> p i e", e=NE),
          in1=rb[:].unsqueeze(1).broadcast_to([128, NT, NE]), op=ALU.add, r=["ra0", "rb"], w=["ra1"])
    k.dve("tensor_reduce", out=m1, in_=v3(b_), axis=AX.X, op=ALU.max, r=["ra1"], w=["rsm"])
    k.dve("tensor_tensor", out=v3(eq), in0=v3(b_), in1=bc4(m1), op=ALU.is_equal, r=["ra1", "rsm"], w=["ra2"])
    k.dve("scalar_tensor_tensor", out=b2, in0=eq, scalar=-1e9, in1=b_, op0=ALU.mult, op1=ALU.add, r=["ra2", "ra1"], w=["ra3"])
    k.dve("tensor_reduce", out=m2, in_=v3(b2), axis=AX.X, op=ALU.max, r=["ra3"], w=["rsm"])
    k.dve("tensor_tensor", out=gs, in0=m1, in1=m2, op=ALU.add, r=["rsm"], w=["rsm"])
    k.dve("tensor_reduce", out=gmax[:], in_=gs.rearrange("p (i g) -> p i g", g=4), axis=AX.X, op=ALU.max, r=["rsm"], w=["gmax"])
    k.dve("tensor_tensor", out=gsel.rearrange("p (i g) -> p i g", g=4), in0=gs.rearrange("p (i g) -> p i g", g=4),
          in1=gmax[:].unsqueeze(2).broadcast_to([128, NT, 4]), op=ALU.is_equal, r=["rsm", "gmax"], w=["rsm"])
    k.dve("tensor_tensor", out=v3(ge), in0=v3(b_), in1=bc4(m2), op=ALU.is_ge, r=["ra1", "rsm"], w=["ra4"])
    k.dve("tensor_tensor", out=v3(ge), in0=v3(ge), in1=bc4(gsel), op=ALU.mult, r=["ra4", "rsm"], w=["ra4"])
    k.dve("tensor_tensor", out=eq, in0=ge, in1=sc, op=ALU.mult, r=["ra4", "ra0"], w=["ra2"])
    k.dve("tensor_reduce", out=den[:], in_=eq.rearrange("p (i e) -> p i e", e=NE), axis=AX.X, op=ALU.add, r=["ra2"], w=["den"])
    k.dve("reciprocal", out=den[:], in_=den[:], r=["den"], w=["den"])
    k.dve("tensor_tensor", out=comb[:], in0=eq.rearrange("p (i e) -> p i e", e=NE),
          in1=den[:].unsqueeze(2).broadcast_to([128, NT, NE]), op=ALU.mult, r=["ra2", "den"], w=["comb"])
    subs = []
    for t0 in range(0, NT, NSG):
        nt_ = min(NSG, NT - t0)
        for e in range(NE):
            for j0 in range(0, nt_, 4):
                subs.append((t0, nt_, e, j0, min(4, nt_ - j0)))
    wslot = {}
    wcnt = [0]

    def load_w(t0, e):
        if (t0, e) in wslot:
            return
        ws = wcnt[0] % 2
        wcnt[0] += 1
        wslot[(t0, e)] = ws
        k.dma(wg[ws][:], C.w_gate[l, e].rearrange("(kc p) f -> p kc f", p=128), w=[("wg", ws)], cast=True)
        k.dma(wu[ws][:], C.w_up[l, e].rearrange("(kc p) f -> p kc f", p=128), w=[("wu", ws)], cast=True)
        k.dma(wd[ws][:], C.w_down[l, e].rearrange("(kc p) f -> p kc f", p=128), w=[("wd", ws)], cast=True)

    def gu_ops(si):
        t0, nt_, e, j0, ntile = subs[si]
        ws = wslot[(t0, e)]
        n = ntile * 128
        c0 = (t0 + j0) * 128
        hk = [("hT", i) for i in range(t0 + j0, t0 + j0 + ntile)]
        q_ = si % 2
        ops = []
        for ch in range(2):
            for (wt_, wn, bkk) in ((wg, "wg", ch), (wu, "wu", 2 + ch)):
                for kc in range(KC):
                    ops.append(lambda wt_=wt_, wn=wn, bkk=bkk, kc=kc, ch=ch: k.pe(
                        "matmul", C.bank[bkk][:, 0:n], lhsT=wt_[ws][:, kc, ch * 128:(ch + 1) * 128], rhs=C.hT[:, kc, c0:c0 + n],
                        start=(kc == 0), stop=(kc == KC - 1), r=[(wn, ws)] + hk, w=[("bank", bkk)]))

        def post(ch):
            k.act("activation", out=sl[ch][:, 0:n], in_=C.bank[ch][:, 0:n], func=AF.Silu, r=[("bank", ch)], w=[("sl", ch)])
            k.dve("tensor_tensor", out=hid[q_][:, ch, 0:n], in0=C.bank[2 + ch][:, 0:n], in1=sl[ch][:, 0:n], op=ALU.mult,
                  r=[("bank", 2 + ch), ("sl", ch)], w=[("hid", (q_, ch))])
        return ops, post

    ycnt = [0]

    def down_ops(si):
        t0, nt_, e, j0, ntile = subs[si]
        ws = wslot[(t0, e)]
        q_ = si % 2
        ops = []
        for j in range(ntile):
            tl = j0 + j
            for half in range(2):
                def f(j=j, tl=tl, half=half):
                    bk = 4 + ycnt[0] % 4
                    ycnt[0] += 1
                    for ch in range(2):
                        k.pe("matmul", C.bank[bk][:, 0:512], lhsT=hid[q_][:, ch, j * 128:(j + 1) * 128], rhs=wd[ws][:, ch, half * 512:(half + 1) * 512],
                             start=(ch == 0), stop=(ch == 1), r=[("hid", (q_, ch)), ("wd", ws)], w=[("bank", bk)])
                    ya = yacc[:, tl, half * 512:(half + 1) * 512]
                    cw = comb[:, t0 + tl, e:e + 1]
                    if e == 0:
                        k.dve("tensor_scalar", out=ya, in0=C.bank[bk][:, 0:512], scalar1=cw, scalar2=None, op0=ALU.mult,
                              r=[("bank", bk), ("comb", t0 + tl)], w=[("yacc", tl)])
                    else:
                        k.dve("scalar_tensor_tensor", out=ya, in0=C.bank[bk][:, 0:512], scalar=cw, in1=ya, op0=ALU.mult, op1=ALU.add,
                              r=[("bank", bk), ("comb", t0 + tl), ("yacc", tl)], w=[("yacc", tl)])
                ops.append(f)
        return ops

    def ln_tail(t0, nt_):
        tls = [tl for tl in range(nt_) if not (last and t0 + tl == 0)]
        for b0 in range(0, len(tls), NLN):
            bt = tls[b0:b0 + NLN]
            sl_ = ln_slots(k, len(bt))
            items = []
            for ls, tl in zip(sl_, bt):
                i = t0 + tl
                k.dma(C.lnx[ls][:], Hin[i * 128:(i + 1) * 128, :], r=[(hin_name, i)], w=[("lnx", ls)])
                k.dve("scalar_tensor_tensor", out=C.lnx[ls][:], in0=C.lnx[ls][:], scalar=ALPHA, in1=yacc[:, tl, :],
                      op0=ALU.mult, op1=ALU.add, r=[("lnx", ls), ("yacc", tl)], w=[("lnx", ls)])
                if last:
                    items.append((ls, i, C.out[(i - 1) * 128:i * 128, :], ("out", i), True))
                else:
                    items.append((ls, i, Hout[i * 128:(i + 1) * 128, :], (hout_name, i), False))
            ln_batch(k, items)
        ln_flush(k)

    NS = len(subs)
    load_w(subs[0][0], subs[0][2])
    gops, gpost = gu_ops(0)
    for f in gops[0:16]:
        f()
    gpost(0)
    for f in gops[16:32]:
        f()
    gpost(1)
    for si in range(NS):
        t0, nt_, e, j0, ntile = subs[si]
        if j0 == 0:
            nxt = [x for x in subs[si + 1:] if (x[0], x[2]) != (t0, e)]
            if nxt:
                load_w(nxt[0][0], nxt[0][2])
        dops = down_ops(si)
        last_of_sg = (si + 1 == NS) or (subs[si + 1][0] != t0)
        if si + 1 < NS and not last_of_sg:
            gops, gpost = gu_ops(si + 1)
            gi_ = 0
            nd = len(dops)
            early = max(0, nd - 3)
            per = (32 + early - 1) // max(1, early) if early else 32
            di = 0
            while gi_ < 32:
                stop_at = min(32, gi_ + per)
                while gi_ < stop_at:
                    gops[gi_]()
                    gi_ += 1
                    if gi_ == 16:
                        gpost(0)
                if di < early:
                    dops[di]()
                    di += 1
            gpost(1)
            while di < nd:
                dops[di]()
                di += 1
        else:
            for f in dops:
                f()
            if last_of_sg:
                ln_tail(t0, nt_)
            if si + 1 < NS:
                gops, gpost = gu_ops(si + 1)
                for f in gops[0:16]:
                    f()
                gpost(0)
                for f in gops[16:32]:
                    f()
                gpost(1)


def build(upto=99, debug=False):
    k = K(upto, debug)
    nc = k.nc
    C = setup(k)
    if upto >= 1:
        phase1(k, 0, with_p0=True)
    else:
        phase0(k)
    if debug and upto == 1:
        for nm, ap in (("QTf", C.QTf), ("KTf", C.KTf), ("QTm", C.QTm), ("KTm", C.KTm), ("Vf", C.Vf), ("Vm", C.Vm)):
            o = k.dram_out("dbg_" + nm, list(ap.shape), BF16)
            k.dma(o, ap, r=[nm, "V"], w=["dbg_" + nm], final=True)
    if upto >= 2:
        attention(k, C.QTf, C.KTf, C.Vf, 68, 1.0, C.OTf, "OTf")
        attention(k, C.QTm, C.KTm, C.Vm, 96, 96 ** -0.5, C.OTm, "OTm")
    if debug and upto == 2:
        for nm, ap in (("OTf", C.OTf), ("OTm", C.OTm)):
            o = k.dram_out("dbg_" + nm, list(ap.shape), BF16)
            k.dma(o, ap, r=[nm], w=["dbg_" + nm], final=True)
    if upto >= 3:
        phase3(k, 0, C.H[0], "H0", C.H[1], "H1")
    if debug and upto == 3:
        o = k.dram_out("dbg_H1", [T, D], F32)
        k.dma(o, C.H[1], r=["H1"], w=["dbg_H1"], final=True)
    if upto >= 4:
        phase4(k, 0, C.H[1], "H1", C.H[0], "H0", False)
    if debug and upto == 4:
        o = k.dram_out("dbg_H2", [T, D], F32)
        k.dma(o, C.H[0], r=["H0"], w=["dbg_H2"], final=True)
    if upto >= 5:
        phase1(k, 1)
        attention(k, C.QTf, C.KTf, C.Vf, 68, 1.0, C.OTf, "OTf")
        attention(k, C.QTm, C.KTm, C.Vm, 96, 96 ** -0.5, C.OTm, "OTm")
        phase3(k, 1, C.H[0], "H0", C.H[1], "H1")
        phase4(k, 1, C.H[1], "H1", None, None, True)
    if debug and upto == 0:
        C.dbg_hT = k.dram_out("dbg_hT", [128, KC, T], BF16)
        k.dma(C.dbg_hT, C.hT[:], r=["hT"], w=["dbg_hT"], final=True)
        C.dbg_H = k.dram_out("dbg_H", [T, D], F32)
        k.dma(C.dbg_H, C.H[0], r=["H0"], w=["dbg_H"], final=True)
    finish(k)
    return k


def finish(k):
    nc = k.nc
    R = k.R
    sems = {e: nc.alloc_semaphore("sem_" + e) for e in R.ENGS}
    dsems = {}
    for e in ("sp", "pool"):
        for j in range(R.NQ):
            dsems[(e, j)] = nc.alloc_semaphore("dsem_%s_%d" % (e, j))
    R.replay(nc, sems, dsems)


def host_consts():
    c = {}
    c["c_ident"] = np.eye(128, dtype=np.float32)
    kk = np.arange(128)[:, None]; qq = np.arange(128)[None, :]
    c["c_cmask"] = np.where(kk > qq, NEG, 0.0).astype(np.float32)
    cm0 = np.where(kk > qq, NEG, 0.0).astype(np.float32)
    cm0[NPAD, :NPAD] = 0.0
    c["c_cmask0"] = cm0
    c["c_pmask"] = np.where(np.broadcast_to(kk, (128, 512)) < NPAD, NEG, 0.0).astype(np.float32)
    c["c_ut"] = (kk <= qq).astype(np.float32)
    c["c_padcol"] = (np.arange(128) >= NPAD).astype(np.float32)[:, None]
    inv = (10000.0 ** (-np.arange(16, dtype=np.float32) / 16)).astype(np.float32)
    pos = (np.arange(T) - NPAD).astype(np.float32)
    ang = pos[None, :] * inv[np.arange(128) % 16][:, None]
    cs = np.cos(ang).astype(np.float32)
    sn = np.sin(ang).astype(np.float32)
    sel_sin = ((np.arange(128) // 32) % 2 == 1)[:, None]
    c["c_cs"] = np.where(sel_sin, sn, cs).astype(np.float32)
    c["c_ones8"] = np.ones((8, T), np.float32)
    sel = np.zeros((48, NE, 128), np.float32)
    for e in range(NE):
        sel[e, e, :] = 1.0
        sel[32 + e, e, :] = 1.0
    c["c_sel"] = sel.reshape(48, NE * 128)
    return c


def make_in_maps(inputs):
    x = np.asarray(inputs["x"], np.float32)
    meta = np.asarray(inputs["meta_tokens"], np.float32)
    shared = {}
    for name in ["ln_in_g", "ln_in_b", "w_in", "fox_f_bias", "fox_w_o", "mla_q_norm", "mla_w_uq",
                 "mla_kv_norm", "mla_w_o", "w_out", "ln1_g", "ln1_b", "router_w", "router_b",
                 "w_gate", "w_up", "w_down", "ln2_g", "ln2_b"]:
        shared[name] = np.ascontiguousarray(np.asarray(inputs[name], np.float32))
    wkv = np.asarray(inputs["mla_w_ukv"], np.float32).reshape(DEPTH, 256, 8, 2, 64)
    shared["mla_w_ukv"] = np.ascontiguousarray(
        np.concatenate([wkv[:, :, :, 0, :].reshape(DEPTH, 256, 512), wkv[:, :, :, 1, :].reshape(DEPTH, 256, 512)], axis=2))
    shared.update(host_consts())
    maps = []
    for b in range(8):
        xp = np.zeros((T, D), np.float32)
        xp[NPAD:128] = meta
        xp[128:] = x[b]
        m = dict(shared)
        m["x"] = xp
        maps.append(m)
    return maps


_CACHE = {}


def kernel(**inputs):
    if "k" not in _CACHE:
        _CACHE["k"] = build()
    k = _CACHE["k"]
    maps = make_in_maps(inputs)
    maps = [{n: m[n] for n in k.inp} for m in maps]
    res = run_bass_kernel_spmd(k.nc, maps, core_ids=list(range(8)))
    out = np.stack([np.asarray(r["out"], np.float32) for r in res.results], axis=0)
    return out


def check_debug(r, R, relerr):
    f = lambda a: np.asarray(a).astype(np.float32)
    if "dbg_H" in r:
        print("H0 err", relerr(f(r["dbg_H"])[NPAD:], R["h0"]))
    if "dbg_hT" in r:
        hT = f(r["dbg_hT"]).transpose(2, 1, 0).reshape(T, D)
        print("hT err (vs h0)", relerr(hT[NPAD:], R["h0"]))
    L0 = R["L0"]
    if "dbg_H1" in r:
        print("H1 err", relerr(f(r["dbg_H1"])[NPAD:], L0["h1"]), np.isfinite(f(r["dbg_H1"])).all())
    if "dbg_H2" in r:
        print("H2 err", relerr(f(r["dbg_H2"])[NPAD:], L0["h2"]), np.isfinite(f(r["dbg_H2"])).all())
    if "out" in r and "dbg_H" not in r and "dbg_H1" not in r and "dbg_H2" not in r and "dbg_QTf" not in r and "dbg_OTf" not in r:
        print("OUT err", relerr(f(r["out"]), R["L1"]["h2"][NMETA:]), np.isfinite(f(r["out"])).all())
    for nm, rn in (("dbg_OTf", "ofox"), ("dbg_OTm", "omla")):
        if nm in r:
            o = f(r[nm])
            oh = np.stack([o[(hh % 2) * 64:(hh % 2) * 64 + 64, hh // 2, :] for hh in range(8)], 0)
            print(nm, "err", relerr(oh[:, :, NPAD:].transpose(0, 2, 1), L0[rn]), "finite", np.isfinite(o).all())
    if "dbg_QTf" in r:
        q = f(r["dbg_QTf"]); kk = f(r["dbg_KTf"])
        print("qf err", relerr(q[:, 0:64, NPAD:].transpose(0, 2, 1) * 8, L0["qf"]))
        print("kf err", relerr(kk[:, 0:64, NPAD:].transpose(0, 2, 1), L0["kf"]))
        print("q ones rows", q[:, 64:67].min(), q[:, 64:67].max(), "k ones row", kk[:, 67].min(), kk[:, 67].max())
        print("c row err", relerr(q[:, 67, NPAD:], L0["dec"]), np.abs(q[:, 67, :NPAD]).max())
        nc_ = kk[:, 64, NPAD:] + kk[:, 65, NPAD:] + kk[:, 66, NPAD:]
        print("-c split err", relerr(-nc_, L0["dec"]), np.abs(nc_ + L0["dec"]).max())
        v = f(r["dbg_Vf"])
        v = v.transpose(0, 2, 1, 3).reshape(8, T, 128)
        ve = np.concatenate([v[0::2, :, 0:64], v[1::2, :, 64:128]], 0)[[0, 4, 1, 5, 2, 6, 3, 7]]
        print("vf err", relerr(ve[:, NPAD:], L0["vf"]))
        on = np.concatenate([v[0::2, :, 64:128], v[1::2, :, 0:64]], 0)
        print("v ones", on.min(), on.max())
        q = f(r["dbg_QTm"]); kk = f(r["dbg_KTm"])
        print("qm err", relerr(q[:, :, NPAD:].transpose(0, 2, 1), L0["qm"]), relerr(q[:, 64:, NPAD:].transpose(0, 2, 1), L0["qm"][:, :, 64:]))
        print("km err", relerr(kk[:, :, NPAD:].transpose(0, 2, 1), L0["km"]), relerr(kk[:, 64:, NPAD:].transpose(0, 2, 1), L0["km"][:, :, 64:]))
        v = f(r["dbg_Vm"]).transpose(0, 2, 1, 3).reshape(8, T, 128)
        ve = np.concatenate([v[0::2, :, 0:64], v[1::2, :, 64:128]], 0)[[0, 4, 1, 5, 2, 6, 3, 7]]
        print("vm err", relerr(ve[:, NPAD:], L0["vm"]))
```

```python
import numpy as np
import concourse.bass as bass
import concourse.mybir as mybir
from concourse.bass_utils import run_bass_kernel_spmd

F32 = mybir.dt.float32
BF16 = mybir.dt.bfloat16
AF = mybir.ActivationFunctionType
ALU = mybir.AluOpType
AX = mybir.AxisListType

D = 1024
KC = 8
SEQ = 4096
NMETA = 16
NPAD = 112
T = SEQ + 128
NT = T // 128
DEPTH = 2
NE = 16
DE = 256
IN_COLS = 4264
ALPHA = (2 * DEPTH) ** 0.25
LN_EPS = 1e-5
RMS_EPS = 1e-6
NEG = -30000.0
GROUPS = [(g * 512, min(512, T - g * 512)) for g in range((T + 511) // 512)]
AGROUPS = [(0, 128)] + [(128 + 512 * g, 512) for g in range(8)]
OQ, OK_, OV, OF, OCQ, OCKV, OKR, OGF, OGM = 0, 512, 1024, 1536, 1544, 1928, 2184, 2216, 3240


class Op:
    __slots__ = ("eng", "meth", "args", "kw", "dma", "idx", "marked", "waits", "dsem", "dval", "ticket")

    def __init__(self, eng, meth, args, kw, dma):
        self.eng, self.meth, self.args, self.kw, self.dma = eng, meth, args, kw, dma
        self.marked = False
        self.waits = []
        self.dsem = None
        self.dval = 0


class Rec:
    ENGS = ("pe", "act", "dve", "pool", "sp")
    NQ = 12

    def __init__(self):
        self.ops = {e: [] for e in self.ENGS}
        self.state = {}
        self.seen = {e: {p: -1 for p in self.ENGS} for e in self.ENGS}
        self.seen_dma = {e: {} for e in self.ENGS}
        self.ndma = {e: 0 for e in self.ENGS}
        self.out_dmas = []
        self.pending = {}

    def _st(self, key):
        buf, sub = key if isinstance(key, tuple) else (key, None)
        d = self.state.setdefault(buf, {})
        return buf, sub, d

    def _deps(self, reads, writes):
        deps = []
        for key in reads:
            buf, sub, d = self._st(key)
            subs = list(d.keys()) if sub is None else [s for s in (sub, None) if s in d]
            for s in subs:
                if d[s][0] is not None:
                    deps.append(d[s][0])
        for key in writes:
            buf, sub, d = self._st(key)
            subs = list(d.keys()) if sub is None else [s for s in (sub, None) if s in d]
            for s in subs:
                if d[s][0] is not None:
                    deps.append(d[s][0])
                deps.extend(d[s][1])
        return deps

    def _update(self, op, reads, writes):
        for key in reads:
            buf, sub, d = self._st(key)
            if sub is None:
                for s in list(d.keys()):
                    d[s][1].append(op)
                d.setdefault(None, [None, []])
                if op not in d[None][1]:
                    d[None][1].append(op)
            else:
                d.setdefault(sub, [None, []])[1].append(op)
        for key in writes:
            buf, sub, d = self._st(key)
            if sub is None:
                d.clear()
                d[None] = [op, []]
            else:
                d[sub] = [op, []]

    def op(self, eng, meth, *args, r=(), w=(), dma=False, final=False, **kw):
        o = Op(eng, meth, args, kw, dma)
        lst = self.ops[eng]
        o.idx = len(lst)
        if self.pending.get(eng):
            o.waits.extend(self.pending[eng])
            self.pending[eng] = []
        for dpo in self._deps(r, w):
            if dpo is o:
                continue
            if dpo.dma:
                sd = self.seen_dma[eng]
                if sd.get(dpo.dsem, 0) >= dpo.dval:
                    continue
                sd[dpo.dsem] = dpo.dval
                o.waits.append(dpo)
            else:
                if dpo.eng == eng and eng in ("pe", "sp"):
                    continue
                if self.seen[eng][dpo.eng] >= dpo.idx:
                    continue
                self.seen[eng][dpo.eng] = dpo.idx
                dpo.marked = True
                o.waits.append(dpo)
        if dma:
            j = self.ndma[eng]
            self.ndma[eng] = j + 1
            o.dsem = (eng, j % self.NQ)
            o.dval = 16 * (j // self.NQ + 1)
            prev = 16 * (j // self.NQ)
            if prev > 0 and self.seen_dma[eng].get(o.dsem, 0) < prev:
                self.seen_dma[eng][o.dsem] = prev
                o.waits.append(("dma", o.dsem, prev))
            if final:
                self.out_dmas.append(o)
        lst.append(o)
        self._update(o, r, w)
        return o

    def barrier(self):
        lastop = {}
        for e in self.ENGS:
            for o in reversed(self.ops[e]):
                if not o.dma:
                    lastop[e] = o
                    break
        dmas = {}
        for e in self.ENGS:
            for o in self.ops[e]:
                if o.dma:
                    dmas[o.dsem] = max(dmas.get(o.dsem, 0), o.dval)
        for e in self.ENGS:
            pw = self.pending.setdefault(e, [])
            for p, o in lastop.items():
                if p == e and e in ("pe", "sp"):
                    continue
                if self.seen[e][p] >= o.idx:
                    continue
                self.seen[e][p] = o.idx
                o.marked = True
                pw.append(o)
            for ds, v in dmas.items():
                if self.seen_dma[e].get(ds, 0) < v:
                    self.seen_dma[e][ds] = v
                    pw.append(("dma", ds, v))
        self.state = {}

    def replay(self, nc, sems, dsems):
        engs = {"pe": "tensor", "act": "scalar", "dve": "vector", "pool": "gpsimd", "sp": "sync"}
        for e in self.ENGS:
            t = 0
            for o in self.ops[e]:
                if o.marked:
                    t += 1
                o.ticket = t
        with nc.Block() as block:
            for e in self.ENGS:
                ops = self.ops[e]
                last = (e == "sp")
                outs = self.out_dmas

                def body(eng, ops=ops, e=e, last=last):
                    for o in ops:
                        for wdep in o.waits:
                            if isinstance(wdep, tuple):
                                eng.wait_ge(dsems[wdep[1]], wdep[2])
                            elif wdep.dma:
                                eng.wait_ge(dsems[wdep.dsem], wdep.dval)
                            else:
                                eng.wait_ge(sems[wdep.eng], wdep.ticket)
                        ins = getattr(eng, o.meth)(*o.args, **o.kw)
                        if o.dma:
                            ins.then_inc(dsems[o.dsem], 16)
                        elif o.marked:
                            ins.then_inc(sems[o.eng], 1)
                    if last:
                        done = {}
                        for o in outs:
                            done[o.dsem] = max(done.get(o.dsem, 0), o.dval)
                        for s, v in done.items():
                            eng.wait_ge(dsems[s], v)

                getattr(block, engs[e])(body)


class K:
    def __init__(self, upto=99, debug=False):
        self.upto = upto
        self.debug = debug
        self.nc = nc = bass.Bass("TRN2", target_bir_lowering=False)
        self.R = Rec()
        self.sb_off = 17536
        self.ph_off = None
        self.nalloc = 0
        self.inp = {}
        self.tcount = 0

    def dram_in(self, name, shape, dt=F32):
        h = self.nc.dram_tensor(name, list(shape), dt, kind="ExternalInput")
        self.inp[name] = h
        return h.ap()

    def dram_out(self, name, shape, dt=F32):
        return self.nc.dram_tensor(name, list(shape), dt, kind="ExternalOutput").ap()

    def dram(self, name, shape, dt):
        return self.nc.dram_tensor(name, list(shape), dt).ap()

    def sb(self, name, shape, dt):
        return self._at(name, shape, dt, "persist")

    def _at(self, name, shape, dt, region):
        esz = {F32: 4, BF16: 2}[dt]
        nbytes = int(np.prod(shape[1:])) * esz
        nbytes = (nbytes + 63) // 64 * 64
        if region == "persist":
            off = self.sb_off
            self.sb_off += nbytes
            assert self.ph_off is None, "persistent alloc after phase alloc"
        else:
            if self.ph_off is None:
                self.ph_off = self.sb_off
            off = self.ph_off
            self.ph_off += nbytes
        assert off + nbytes <= 228000, ("SBUF overflow", name, off + nbytes)
        self.nalloc += 1
        return self.nc.alloc_sbuf_tensor_at("%s_%d" % (name, self.nalloc), list(shape), dt, offset=off)

    def psb(self, name, shape, dt):
        return self._at(name, shape, dt, "phase")

    def new_phase(self):
        self.R.barrier()
        self.ph_off = self.sb_off

    def ps(self, name, shape, dt=F32):
        return self.nc.alloc_psum_tensor(name, list(shape), dt)

    def pe(self, meth, *a, **k):
        return self.R.op("pe", meth, *a, **k)

    def act(self, meth, *a, **k):
        return self.R.op("act", meth, *a, **k)

    def dve(self, meth, *a, **k):
        return self.R.op("dve", meth, *a, **k)

    def pool(self, meth, *a, **k):
        return self.R.op("pool", meth, *a, **k)

    def dma(self, out, in_, r=(), w=(), cast=False, final=False):
        eng = "pool" if cast else "sp"
        return self.R.op(eng, "dma_start", out=out, in_=in_, r=r, w=w, dma=True, final=final)


def bcast_row(ap_1d, n, parts=128):
    return bass.AP(ap_1d.tensor, ap_1d.offset, [[0, parts], [1, n]])


class Ctx:
    pass


def setup(k):
    nc = k.nc
    C = Ctx()
    k.C = C
    C.x = k.dram_in("x", [T, D])
    C.ln_in_g = k.dram_in("ln_in_g", [D]); C.ln_in_b = k.dram_in("ln_in_b", [D])
    C.w_in = k.dram_in("w_in", [DEPTH, D, IN_COLS])
    C.fox_f_bias = k.dram_in("fox_f_bias", [DEPTH, 8])
    C.fox_w_o = k.dram_in("fox_w_o", [DEPTH, 512, D])
    C.mla_q_norm = k.dram_in("mla_q_norm", [DEPTH, 384])
    C.mla_w_uq = k.dram_in("mla_w_uq", [DEPTH, 384, 768])
    C.mla_kv_norm = k.dram_in("mla_kv_norm", [DEPTH, 256])
    C.mla_w_ukv = k.dram_in("mla_w_ukv", [DEPTH, 256, 1024])
    C.mla_w_o = k.dram_in("mla_w_o", [DEPTH, 512, D])
    C.w_out = k.dram_in("w_out", [DEPTH, D, D])
    C.ln1_g = k.dram_in("ln1_g", [DEPTH, D]); C.ln1_b = k.dram_in("ln1_b", [DEPTH, D])
    C.router_w = k.dram_in("router_w", [D, NE]); C.router_b = k.dram_in("router_b", [NE])
    C.w_gate = k.dram_in("w_gate", [DEPTH, NE, D, DE])
    C.w_up = k.dram_in("w_up", [DEPTH, NE, D, DE])
    C.w_down = k.dram_in("w_down", [DEPTH, NE, DE, D])
    C.ln2_g = k.dram_in("ln2_g", [DEPTH, D]); C.ln2_b = k.dram_in("ln2_b", [DEPTH, D])
    C.c_ident = k.dram_in("c_ident", [128, 128])
    C.c_cmask = k.dram_in("c_cmask", [128, 128])
    C.c_pmask = k.dram_in("c_pmask", [128, 512])
    C.c_cmask0 = k.dram_in("c_cmask0", [128, 128])
    C.c_ut = k.dram_in("c_ut", [128, 128])
    C.c_padcol = k.dram_in("c_padcol", [128, 1])
    C.c_sel = k.dram_in("c_sel", [48, NE * 128])
    C.c_ones8 = k.dram_in("c_ones8", [8, T])
    C.c_cs = k.dram_in("c_cs", [128, T])
    C.out = k.dram_out("out", [SEQ, D])
    C.H = [k.dram("Hs%d" % i, [T, D], F32) for i in range(2)]
    C.Rs = k.dram("Rs", [T, D], F32)
    C.QTf = k.dram("QTf", [8, 68, T], BF16); C.KTf = k.dram("KTf", [8, 68, T], BF16)
    C.QTm = k.dram("QTm", [8, 96, T], BF16); C.KTm = k.dram("KTm", [8, 96, T], BF16)
    C.Vf = k.dram("Vf", [8, 128, NT, 128], BF16); C.Vm = k.dram("Vm", [8, 128, NT, 128], BF16)
    C.OTf = k.dram("OTf", [128, 4, T], BF16); C.OTm = k.dram("OTm", [128, 4, T], BF16)
    C.psall = k.ps("psall", [128, 8, 512], F32)
    C.bank = [C.psall[:, i, :] for i in range(8)]
    C.hT = k.sb("hT", [128, KC, T], BF16)
    C.identb = k.sb("identb", [128, 128], BF16)
    C.identf = k.sb("identf", [128, 128], F32)
    C.padcol = k.sb("padcol", [128, 1], F32)
    C.onesf = k.sb("onesf", [128, 128], F32)
    C.utf = k.sb("utf", [128, 128], F32)
    C.cmask = k.sb("cmask", [128, 128], BF16)
    C.pmask = k.sb("pmask", [128, 512], BF16)
    C.cmask0 = k.sb("cmask0", [128, 128], BF16)
    C.ln_n = 0
    C.nhalf = k.sb("nhalf", [128, 2], F32)
    k.pool("memset", C.nhalf[:], -0.5, w=["nhalf"])
    k.dma(C.utf[:], C.c_ut, w=["utf"])
    k.dma(C.cmask[:], C.c_cmask, w=["cmask"], cast=True)
    k.dma(C.pmask[:], C.c_pmask, w=["pmask"], cast=True)
    k.dma(C.cmask0[:], C.c_cmask0, w=["cmask0"], cast=True)
    k.pool("memset", C.onesf[:], 1.0, w=["onesf"])
    for row in (64, 65, 66):
        k.dma(C.QTf[:, row, :], C.c_ones8, w=[("QTf", "ones%d" % row)], cast=True)
    k.dma(C.KTf[:, 67, :], C.c_ones8, w=[("KTf", "ones")], cast=True)
    k.dma(C.identb[:], C.c_ident, w=["identb"], cast=True)
    k.dma(C.identf[:], C.c_ident, w=["identf"])
    k.dma(C.padcol[:], C.c_padcol, w=["padcol"])
    return C


NLN = 4


def alloc_ln(k):
    C = k.C
    C.lnx = [k.psb("lnx%d" % i, [128, D], F32) for i in range(NLN)]
    C.hb = [k.psb("hb%d" % i, [128, D], BF16) for i in range(NLN)]
    C.lst = [k.psb("lst%d" % i, [128, 12], F32) for i in range(NLN)]
    C.lmv = [k.psb("lmv%d" % i, [128, 8], F32) for i in range(NLN)]
    C.gbc = k.psb("gbc", [128, D], F32); C.bbc = k.psb("bbc", [128, D], F32)
    C.ln_pending = []


def load_ln_params(k, g1d, b1d):
    C = k.C
    k.dma(C.gbc[:], bcast_row(g1d, D), w=["gbc"])
    k.dma(C.bbc[:], bcast_row(b1d, D), w=["bbc"])


def ln_slots(k, n):
    C = k.C
    ln_flush(k, 0)
    return list(range(n))


def ln_flush(k, keep=0):
    C = k.C
    while len(C.ln_pending) > keep:
        C.ln_pending.pop(0)()


def ln_batch(k, items):
    for _ in ln_batch_gen(k, items):
        pass


def ln_batch_gen(k, items):
    C = k.C
    def names(s):
        return C.lnx[s], C.lst[s], C.lmv[s], C.hb[s], ("lnx", s), ("lst", s), ("lmv", s), ("hb", s)
    half = (len(items) + 1) // 2
    for part in (items[:half], items[half:]):
        for (s, i, out_ap, outkey, final) in part:
            x, st, mv, hb, X, ST, MV, HB = names(s)
            k.dve("bn_stats", out=st[:, 0:6], in_=x[:, 0:512], r=[X], w=[ST])
            k.dve("bn_stats", out=st[:, 6:12], in_=x[:, 512:1024], r=[X], w=[ST])
            k.dve("bn_aggr", out=mv[:, 0:2], in_=st[:, 0:12], r=[ST], w=[MV])
        yield
    for (s, i, out_ap, outkey, final) in items:
        x, st, mv, hb, X, ST, MV, HB = names(s)
        k.pool("tensor_scalar", out=mv[:, 2:3], in0=mv[:, 1:2], scalar1=1.0, scalar2=LN_EPS, op0=ALU.mult, op1=ALU.add, r=[MV], w=[MV])
        k.pool("tensor_tensor", out=mv[:, 4:5], in0=mv[:, 2:3], in1=C.nhalf[:, 0:1], op=ALU.pow, r=[MV, "nhalf"], w=[MV])
    yield
    for (s, i, out_ap, outkey, final) in items:
        x, st, mv, hb, X, ST, MV, HB = names(s)
        k.dve("scalar_tensor_tensor", out=mv[:, 5:6], in0=mv[:, 0:1], scalar=-1.0, in1=mv[:, 4:5],
              op0=ALU.mult, op1=ALU.mult, r=[MV], w=[MV])
    for (s, i, out_ap, outkey, final) in items:
        x, st, mv, hb, X, ST, MV, HB = names(s)
        k.act("activation", out=x[:], in_=x[:], func=AF.Identity, scale=mv[:, 4:5], bias=mv[:, 5:6], r=[X, MV], w=[X])
    yield
    for (s, i, out_ap, outkey, final) in items:
        x, st, mv, hb, X, ST, MV, HB = names(s)
        k.dve("tensor_tensor", out=x[:], in0=x[:], in1=C.gbc[:], op=ALU.mult, r=[X, "gbc"], w=[X])
    yield
    for (s, i, out_ap, outkey, final) in items:
        x, st, mv, hb, X, ST, MV, HB = names(s)
        k.pool("tensor_tensor", out=x[:], in0=x[:], in1=C.bbc[:], op=ALU.add, r=[X, "bbc"], w=[X])
    yield
    for (s, i, out_ap, outkey, final) in items:
        x, st, mv, hb, X, ST, MV, HB = names(s)
        k.act("activation", out=hb[:], in_=x[:], func=AF.Copy, r=[X], w=[HB])
        k.dma(out_ap, x[:], r=[X], w=[outkey], final=final)
        C.ln_n += 1

        def deferred(i=i, hb=hb, HB=HB):
            bk = 6 + (i % 2)
            pst = C.bank[bk][:].bitcast(BF16)
            for kc in range(KC):
                k.pe("transpose", out=pst[:, kc * 128:(kc + 1) * 128], in_=hb[:, kc * 128:(kc + 1) * 128],
                     identity=C.identb[:], r=[HB, "identb"], w=[("bank", bk)])
            k.act("activation", out=C.hT[:, :, i * 128:(i + 1) * 128],
                  in_=pst.rearrange("p (k t) -> p k t", k=KC), func=AF.Copy,
                  r=[("bank", bk)], w=[("hT", i)])
        C.ln_pending.append(deferred)
    yield


def phase0(k):
    C = k.C
    k.new_phase()
    alloc_ln(k)
    load_ln_params(k, C.ln_in_g, C.ln_in_b)
    for i0 in range(0, NT, NLN):
        tiles = list(range(i0, min(NT, i0 + NLN)))
        sl = ln_slots(k, len(tiles))
        items = []
        for s, i in zip(sl, tiles):
            k.dma(C.lnx[s][:], C.x[i * 128:(i + 1) * 128, :], w=[("lnx", s)])
            items.append((s, i, C.H[0][i * 128:(i + 1) * 128, :], ("H0", i), False))
        ln_batch(k, items)
    ln_flush(k)


def evac_v(k, src_bank_ap, vst, s, bankkey, dst_dram, i):
    C = k.C
    sv = src_bank_ap.rearrange("p (m two d) -> p m two d", two=2, d=64)
    k.act("activation", out=vst[:, :, 0, 0:64], in_=sv[:, :, 0, :], func=AF.Copy, r=[bankkey], w=[("vst", s)])
    k.dve("tensor_copy", out=vst[:, :, 1, 64:128], in_=sv[:, :, 1, :], r=[bankkey], w=[("vst", s)])
    k.dma(dst_dram[:, :, i, :].rearrange("h p d -> p h d"), vst[:].rearrange("p m two d -> p (m two) d"),
          r=[("vst", s)], w=[("V", i)])


def phase1(k, l, with_p0=False):
    C = k.C
    k.new_phase()
    win = C.w_in[l].rearrange("(kc p) c -> p kc c", p=128)
    wv = k.psb("wv", [128, KC, 512], BF16)
    wcqf = k.psb("wcqf", [128, KC, 392], BF16)
    wckv = k.psb("wckv", [128, KC, 256], BF16)
    wt = [k.psb("wt%d" % i, [128, KC, 128], BF16) for i in range(2)]
    wkr = k.psb("wkr", [128, KC, 32], BF16); wkrR = k.psb("wkrR", [128, KC, 32], BF16)
    vst = [k.psb("vst%d" % i, [128, 4, 2, 128], BF16) for i in range(2)]
    cqnT = k.psb("cqnT", [128, 3, T], BF16); ckvnT = k.psb("ckvnT", [128, 2, T], BF16)
    augT = k.psb("augT", [32, T], BF16)
    fbias = k.psb("fbias", [128, 8], F32)
    sm = [k.psb("sm%d" % i, [128, 64], F32) for i in range(2)]
    junk = k.psb("junk", [128, 384], BF16)
    cqn = [k.psb("cqn%d" % i, [128, 640], BF16) for i in range(2)]
    stg = [k.psb("stg%d" % i, [128, 512], BF16) for i in range(3)]
    rt = [k.psb("rt%d" % i, [128, 512], F32) for i in range(2)]
    gq = k.psb("gq", [128, 8], F32)
    z_all = k.psb("z_all", [128, NT, 8], F32)
    lf_all = k.psb("lf_all", [128, NT, 8], F32)
    carry = k.psb("carry", [128, NT, 8], F32)
    aug_all = k.psb("aug_all", [128, NT, 32], F32)
    hb_all = k.psb("hb_all", [128, NT, 8], BF16)
    ov_off = k.ph_off
    cs = k.psb("cs", [128, T], F32)
    wuq = k.psb("wuq", [128, 3, 768], BF16); wuqR = k.psb("wuqR", [128, 3, 768], BF16)
    wukv = k.psb("wukv", [128, 2, 1024], BF16)
    stgw = k.psb("stgw", [128, 1024], F32)
    ALIAS = ["lnx", "hb", "lst", "lmv", "gbc", "bbc"] if with_p0 else []

    k.dma(wv[:], win[:, :, OV:OV + 512], w=["wv"], cast=True)
    k.dma(wcqf[:], win[:, :, OF:OF + 392], w=["wcqf"], cast=True)
    k.dma(wckv[:], win[:, :, OCKV:OCKV + 256], w=["wckv"], cast=True)
    k.dma(wkr[:], win[:, :, OKR:OKR + 32], w=["wkr"], cast=True)
    k.dma(fbias[:], bcast_row(C.fox_f_bias[l], 8), w=["fbias"])
    for kc in range(3):
        k.dma(gq[:, kc:kc + 1], C.mla_q_norm[l][kc * 128:(kc + 1) * 128].rearrange("(p o) -> p o", o=1), w=["gq"])
    for kc in range(2):
        k.dma(gq[:, 4 + kc:5 + kc], C.mla_kv_norm[l][kc * 128:(kc + 1) * 128].rearrange("(p o) -> p o", o=1), w=["gq"])
    for s_ in range(2):
        k.pool("memset", vst[s_][:, :, 0, 64:128], 1.0, w=[("vst", s_)])
        k.pool("memset", vst[s_][:, :, 1, 0:64], 1.0, w=[("vst", s_)])
    k.dve("tensor_scalar", out=wkrR[:, :, 0:16], in0=wkr[:, :, 16:32], scalar1=-1.0, scalar2=None, op0=ALU.mult, r=["wkr"], w=["wkrR"])
    k.dve("tensor_copy", out=wkrR[:, :, 16:32], in_=wkr[:, :, 0:16], r=["wkr"], w=["wkrR"])

    def mm_tile(i):
        s_ = i % 2
        tc = slice(i * 128, (i + 1) * 128)
        b0 = 3 * s_
        for (bk, w_, wn, ncol) in ((b0, wv, "wv", 512), (b0 + 1, wcqf, "wcqf", 392), (b0 + 2, wckv, "wckv", 256)):
            for kc in range(KC):
                k.pe("matmul", C.bank[bk][:, 0:ncol], lhsT=C.hT[:, kc, tc], rhs=w_[:, kc, :], start=(kc == 0), stop=(kc == KC - 1),
                     r=[("hT", i), wn], w=[("bank", bk)])

    def chain_tile(i):
        s_ = i % 2
        tc = slice(i * 128, (i + 1) * 128)
        b0 = 3 * s_
        bv, ba, bb = C.bank[b0], C.bank[b0 + 1], C.bank[b0 + 2]
        m = sm[s_]; M = ("sm", s_)
        k.act("activation", out=junk[:, 0:384], in_=ba[:, 8:392], func=AF.Square, scale=384 ** -0.5, accum_out=m[:, 0:1], r=[("bank", b0 + 1)], w=[M, "junk"])
        k.act("activation", out=junk[:, 0:256], in_=bb[:, 0:256], func=AF.Square, scale=256 ** -0.5, accum_out=m[:, 1:2], r=[("bank", b0 + 2)], w=[M, "junk"])
        evac_v(k, bv[:, 0:512], vst[s_], s_, ("bank", b0), C.Vf, i)
        k.dve("tensor_tensor", out=z_all[:, i, :], in0=ba[:, 0:8], in1=fbias[:], op=ALU.add, r=[("bank", b0 + 1), "fbias"], w=[("z_all", i)])
        k.pool("tensor_scalar", out=m[:, 2:4], in0=m[:, 0:2], scalar1=1.0, scalar2=RMS_EPS, op0=ALU.mult, op1=ALU.add, r=[M], w=[M])
        k.pool("tensor_tensor", out=m[:, 6:8], in0=m[:, 2:4], in1=C.nhalf[:, 0:2], op=ALU.pow, r=[M, "nhalf"], w=[M])
        cq = cqn[s_]; CQ = ("cqn", s_)
        k.dve("tensor_scalar", out=cq[:, 0:384], in0=ba[:, 8:392], scalar1=m[:, 6:7], scalar2=None, op0=ALU.mult, r=[M, ("bank", b0 + 1)], w=[CQ])
        k.dve("tensor_scalar", out=cq[:, 384:640], in0=bb[:, 0:256], scalar1=m[:, 7:8], scalar2=None, op0=ALU.mult, r=[M, ("bank", b0 + 2)], w=[CQ])
        pst = C.bank[6 + s_][:].bitcast(BF16)
        for j in range(5):
            k.pe("transpose", out=pst[:, j * 128:(j + 1) * 128], in_=cq[:, j * 128:(j + 1) * 128], identity=C.identb[:],
                 r=[CQ, "identb"], w=[("bank", 6 + s_)])
        k.act("activation", out=cqnT[:, :, tc], in_=pst[:, 0:384].rearrange("p (k t) -> p k t", k=3), func=AF.Copy,
              r=[("bank", 6 + s_)], w=[("cqnT", i)])
        k.act("activation", out=ckvnT[:, :, tc], in_=pst[:, 384:640].rearrange("p (k t) -> p k t", k=2), func=AF.Copy,
              r=[("bank", 6 + s_)], w=[("ckvnT", i)])

    pend_chain = []

    def stepA_push(i):
        mm_tile(i)
        if pend_chain:
            chain_tile(pend_chain.pop(0))
        pend_chain.append(i)

    if with_p0:
        end_off = k.ph_off
        k.ph_off = ov_off
        alloc_ln(k)
        assert k.ph_off <= end_off, "LN overlay too large"
        k.ph_off = end_off
        load_ln_params(k, C.ln_in_g, C.ln_in_b)
        prev_tiles = []
        for i0 in range(0, NT, NLN):
            tiles = list(range(i0, min(NT, i0 + NLN)))
            sl = ln_slots(k, len(tiles))
            items = []
            for s__, i in zip(sl, tiles):
                k.dma(C.lnx[s__][:], C.x[i * 128:(i + 1) * 128, :], w=[("lnx", s__)])
                items.append((s__, i, C.H[0][i * 128:(i + 1) * 128, :], ("H0", i), False))
            gen = ln_batch_gen(k, items)
            for i in prev_tiles:
                stepA_push(i)
                next(gen, None)
                next(gen, None)
            for _ in gen:
                pass
            prev_tiles = tiles
        ln_flush(k)
        for i in prev_tiles:
            stepA_push(i)
    else:
        for i in range(NT):
            stepA_push(i)
    chain_tile(pend_chain.pop(0))

    NF = NT * 8
    zf = z_all[:].rearrange("p i h -> p (i h)")
    lff = lf_all[:].rearrange("p i h -> p (i h)")
    k.act("activation", out=lff, in_=zf, func=AF.Exp, scale=-1.0, r=["z_all"], w=["lf_all"])
    k.act("activation", out=lff, in_=lff, func=AF.Ln, bias=1.0, r=["lf_all"], w=["lf_all"])
    k.dve("tensor_scalar", out=lff, in0=lff, scalar1=-1.0, scalar2=None, op0=ALU.mult, r=["lf_all"], w=["lf_all"])
    k.dve("tensor_scalar", out=lf_all[:, 0, :], in0=lf_all[:, 0, :], scalar1=C.padcol[:, 0:1], scalar2=None, op0=ALU.mult, r=["lf_all", "padcol"], w=["lf_all"])
    k.pe("matmul", C.bank[0][:, 0:NF], lhsT=C.utf[:], rhs=lff, start=True, stop=True, r=["lf_all", "utf"], w=[("bank", 0)])
    k.pe("matmul", C.bank[1][:, 0:NF], lhsT=C.onesf[:], rhs=lff, start=True, stop=True, r=["lf_all", "onesf"], w=[("bank", 1)])
    tot = C.bank[1][:, 0:NF].rearrange("p (i h) -> p i h", h=8)
    k.dve("memset", carry[:, 0, :], 0.0, w=["carry"])
    for i in range(1, NT):
        k.dve("tensor_tensor", out=carry[:, i, :], in0=carry[:, i - 1, :], in1=tot[:, i - 1, :], op=ALU.add, r=["carry", ("bank", 1)], w=["carry"])
    au = aug_all
    c3 = C.bank[0][:, 0:NF].rearrange("p (i h) -> p i h", h=8)
    k.dve("tensor_tensor", out=au[:, :, 0:8], in0=c3, in1=carry[:], op=ALU.add, r=[("bank", 0), "carry"], w=["aug_all"])
    k.dve("tensor_scalar", out=lf_all[:], in0=au[:, :, 0:8], scalar1=-1.0, scalar2=None, op0=ALU.mult, r=["aug_all"], w=["lf_all"])
    k.dve("tensor_copy", out=hb_all[:], in_=lf_all[:], r=["lf_all"], w=["hb_all"])
    k.dve("tensor_copy", out=au[:, :, 8:16], in_=hb_all[:], r=["hb_all"], w=["aug_all"])
    k.dve("tensor_tensor", out=lf_all[:], in0=lf_all[:], in1=au[:, :, 8:16], op=ALU.subtract, r=["lf_all", "aug_all"], w=["lf_all"])
    k.dve("tensor_copy", out=hb_all[:], in_=lf_all[:], r=["lf_all"], w=["hb_all"])
    k.dve("tensor_copy", out=au[:, :, 16:24], in_=hb_all[:], r=["hb_all"], w=["aug_all"])
    k.dve("tensor_tensor", out=au[:, :, 24:32], in0=lf_all[:], in1=au[:, :, 16:24], op=ALU.subtract, r=["lf_all", "aug_all"], w=["aug_all"])
    for i0 in range(0, NT, 4):
        n4 = min(4, NT - i0)
        bk = 2 + (i0 // 4) % 2
        for j in range(n4):
            k.pe("transpose", out=C.bank[bk][0:32, j * 128:(j + 1) * 128], in_=au[:, i0 + j, :], identity=C.identf[:],
                 r=["aug_all", "identf"], w=[("bank", bk)])
        k.dve("tensor_copy", out=augT[:, i0 * 128:(i0 + n4) * 128], in_=C.bank[bk][0:32, 0:n4 * 128], r=[("bank", bk)], w=["augT"])
    k.dma(C.QTf[:, 67, :], augT[0:8, :], r=["augT"], w=[("QTf", "c")])
    k.dma(C.KTf[:, 64, :], augT[8:16, :], r=["augT"], w=[("KTf", "hi")])
    k.dma(C.KTf[:, 65, :], augT[16:24, :], r=["augT"], w=[("KTf", "mid")])
    k.dma(C.KTf[:, 66, :], augT[24:32, :], r=["augT"], w=[("KTf", "lo")])

    k.dma(cs[:], C.c_cs, w=["cs"] + ALIAS)
    cnt = 0
    for c in range(8):
        s_ = c % 2
        k.dma(wt[s_][:], win[:, :, c * 128:(c + 1) * 128], w=[("wt", s_)], cast=True)
        dst = C.QTf if c < 4 else C.KTf
        dn = "QTf" if c < 4 else "KTf"
        h0 = 2 * (c % 4)
        for gi, (g0, n) in enumerate(GROUPS):
            bk = cnt % 6
            st_ = stg[cnt % 3]; SG = ("stg", cnt % 3)
            cnt += 1
            for kc in range(KC):
                k.pe("matmul", C.bank[bk][:, 0:n], lhsT=wt[s_][:, kc, :], rhs=C.hT[:, kc, g0:g0 + n], start=(kc == 0), stop=(kc == KC - 1),
                     r=[("wt", s_), "hT"], w=[("bank", bk)])
            if c < 4:
                k.act("activation", out=st_[:, 0:n], in_=C.bank[bk][:, 0:n], func=AF.Copy, scale=0.125, r=[("bank", bk)], w=[SG])
            else:
                k.dve("tensor_copy", out=st_[:, 0:n], in_=C.bank[bk][:, 0:n], r=[("bank", bk)], w=[SG])
            k.dma(dst[h0, 0:64, g0:g0 + n], st_[0:64, 0:n], r=[SG], w=[(dn, (h0, gi))])
            k.dma(dst[h0 + 1, 0:64, g0:g0 + n], st_[64:128, 0:n], r=[SG], w=[(dn, (h0 + 1, gi))])
    for gi, (g0, n) in enumerate(GROUPS):
        bA, bB = 6, 7
        for kc in range(KC):
            k.pe("matmul", C.bank[bA][0:32, 0:n], lhsT=wkr[:, kc, :], rhs=C.hT[:, kc, g0:g0 + n], start=(kc == 0), stop=(kc == KC - 1),
                 r=["wkr", "hT"], w=[("bank", bA)])
        for kc in range(KC):
            k.pe("matmul", C.bank[bB][0:32, 0:n], lhsT=wkrR[:, kc, :], rhs=C.hT[:, kc, g0:g0 + n], start=(kc == 0), stop=(kc == KC - 1),
                 r=["wkrR", "hT"], w=[("bank", bB)])
        st_ = stg[gi % 3]; SG = ("stg", gi % 3)
        k.dve("tensor_tensor", out=rt[0][0:32, 0:n], in0=C.bank[bA][0:32, 0:n], in1=cs[0:32, g0:g0 + n], op=ALU.mult, r=[("bank", bA), "cs"], w=[("rt", 0)])
        k.dve("tensor_tensor", out=rt[1][0:32, 0:n], in0=C.bank[bB][0:32, 0:n], in1=cs[32:64, g0:g0 + n], op=ALU.mult, r=[("bank", bB), "cs"], w=[("rt", 1)])
        k.dve("tensor_tensor", out=st_[0:32, 0:n], in0=rt[0][0:32, 0:n], in1=rt[1][0:32, 0:n], op=ALU.add, r=[("rt", 0), ("rt", 1)], w=[SG])
        for h in range(8):
            k.dma(C.KTm[h, 64:96, g0:g0 + n], st_[0:32, 0:n], r=[SG], w=[("KTm", (h, gi, "r"))])

    for kc in range(3):
        k.dma(stgw[:, 0:768], C.mla_w_uq[l][kc * 128:(kc + 1) * 128, :], w=["stgw"] + ALIAS)
        k.dve("tensor_scalar", out=wuq[:, kc, :], in0=stgw[:, 0:768], scalar1=gq[:, kc:kc + 1], scalar2=None, op0=ALU.mult, r=["stgw", "gq"], w=["wuq"] + ALIAS)
        k.dve("tensor_copy", out=wuqR[:, kc, :], in_=wuq[:, kc, :], r=["wuq"], w=["wuqR"] + ALIAS)
        wv_ = wuq[:, kc, :].rearrange("p (h d) -> p h d", d=96)
        wr_ = wuqR[:, kc, :].rearrange("p (h d) -> p h d", d=96)
        k.dve("tensor_scalar", out=wr_[:, :, 64:80], in0=wv_[:, :, 80:96], scalar1=-1.0, scalar2=None, op0=ALU.mult, r=["wuq"], w=["wuqR"])
        k.dve("tensor_copy", out=wr_[:, :, 80:96], in_=wv_[:, :, 64:80], r=["wuq"], w=["wuqR"])
    for kc in range(2):
        k.dma(stgw[:, 0:1024], C.mla_w_ukv[l][kc * 128:(kc + 1) * 128, :], w=["stgw"])
        k.dve("tensor_scalar", out=wukv[:, kc, :], in0=stgw[:, 0:1024], scalar1=gq[:, 4 + kc:5 + kc], scalar2=None, op0=ALU.mult, r=["stgw", "gq"], w=["wukv"] + ALIAS)
    cnt = 0
    for h in range(8):
        for gi, (g0, n) in enumerate(GROUPS):
            bA, bB, bC = 3 * (cnt % 2), 3 * (cnt % 2) + 1, 3 * (cnt % 2) + 2
            qs = stg[cnt % 3]; QS = ("stg", cnt % 3)
            cnt += 1
            for kc in range(3):
                k.pe("matmul", C.bank[bA][0:96, 0:n], lhsT=wuq[:, kc, h * 96:(h + 1) * 96], rhs=cqnT[:, kc, g0:g0 + n], start=(kc == 0), stop=(kc == 2),
                     r=["wuq", "cqnT"], w=[("bank", bA)])
            for kc in range(3):
                k.pe("matmul", C.bank[bB][0:96, 0:n], lhsT=wuqR[:, kc, h * 96:(h + 1) * 96], rhs=cqnT[:, kc, g0:g0 + n], start=(kc == 0), stop=(kc == 2),
                     r=["wuqR", "cqnT"], w=[("bank", bB)])
            for kc in range(2):
                k.pe("matmul", C.bank[bC][0:64, 0:n], lhsT=wukv[:, kc, h * 64:(h + 1) * 64], rhs=ckvnT[:, kc, g0:g0 + n], start=(kc == 0), stop=(kc == 1),
                     r=["wukv", "ckvnT"], w=[("bank", bC)])
            k.act("activation", out=qs[0:64, 0:n], in_=C.bank[bA][0:64, 0:n], func=AF.Copy, r=[("bank", bA)], w=[QS])
            k.dve("tensor_tensor", out=rt[0][64:96, 0:n], in0=C.bank[bA][64:96, 0:n], in1=cs[64:96, g0:g0 + n], op=ALU.mult, r=[("bank", bA), "cs"], w=[("rt", 0)])
            k.dve("tensor_tensor", out=rt[1][64:96, 0:n], in0=C.bank[bB][64:96, 0:n], in1=cs[96:128, g0:g0 + n], op=ALU.mult, r=[("bank", bB), "cs"], w=[("rt", 1)])
            k.dve("tensor_tensor", out=qs[64:96, 0:n], in0=rt[0][64:96, 0:n], in1=rt[1][64:96, 0:n], op=ALU.add, r=[("rt", 0), ("rt", 1)], w=[QS])
            k.dma(C.QTm[h, :, g0:g0 + n], qs[0:96, 0:n], r=[QS], w=[("QTm", (h, gi))])
            ks = stg[cnt % 3]; KS = ("stg", cnt % 3)
            cnt += 1
            k.act("activation", out=ks[0:64, 0:n], in_=C.bank[bC][0:64, 0:n], func=AF.Copy, r=[("bank", bC)], w=[KS])
            k.dma(C.KTm[h, 0:64, g0:g0 + n], ks[0:64, 0:n], r=[KS], w=[("KTm", (h, gi, "n"))])
    for i in range(NT):
        s_ = i % 2
        tc = slice(i * 128, (i + 1) * 128)
        bk = 6 + s_
        for kc in range(2):
            k.pe("matmul", C.bank[bk][:, 0:512], lhsT=ckvnT[:, kc, tc], rhs=wukv[:, kc, 512:1024], start=(kc == 0), stop=(kc == 1),
                 r=["wukv", ("ckvnT", i)], w=[("bank", bk)])
        evac_v(k, C.bank[bk][:, 0:512], vst[s_], s_, ("bank", bk), C.Vm, i)


def attention(k, calls):
    C = k.C
    k.new_phase()
    DKM = max(c[3] for c in calls)
    qT = [k.psb("qT%d" % i, [DKM, 2, T], BF16) for i in range(2)]
    kT = [k.psb("kT%d" % i, [DKM, 2, T], BF16) for i in range(2)]
    vv = [k.psb("vv%d" % i, [128, 2, NT, 128], BF16) for i in range(2)]
    pb = [k.psb("pb%d" % i, [128, 2, 512], BF16) for i in range(2)]
    rr = [k.psb("rr%d" % i, [128, 512], F32) for i in range(2)]
    osb = [k.psb("osb%d" % i, [128, 512], F32) for i in range(2)]
    pb.append(k.psb("pb2", [128, 2, 512], BF16))
    ost = [k.psb("ost%d" % i, [128, 512], BF16) for i in range(2)]
    step = 0
    pend = []

    def drain(keep):
        while len(pend) > keep:
            f, post = pend.pop(0)
            f()
            if post is not None:
                post()

    NHP = 4 * len(calls)

    def load_pair(hpg):
        QT, KT, V, Dk, scale, OT, nm = calls[hpg // 4]
        s_ = hpg % 2
        for j in range(2):
            h = 2 * (hpg % 4) + j
            k.dma(qT[s_][0:Dk, j, :], QT[h], r=["QTf", "QTm"], w=[("qT", s_)])
            k.dma(kT[s_][0:Dk, j, :], KT[h], r=["KTf", "KTm"], w=[("kT", s_)])
            k.dma(vv[s_][:, j, :, :], V[h], r=["V"], w=[("vv", s_)])

    load_pair(0)
    for hpg in range(NHP):
        QT, KT, V, Dk, scale, OT, nm = calls[hpg // 4]
        hp = hpg % 4
        s_ = hpg % 2
        for gi, (g0, n) in enumerate(AGROUPS):
            if gi == 2 and hpg + 1 < NHP:
                load_pair(hpg + 1)
            qb0 = g0 // 128
            nkb = qb0 + n // 128
            ob = 6

            def make_pv(pkb, plo, pnn, psl, last, s_=s_):
                def f():
                    for j in range(2):
                        k.pe("matmul", C.bank[ob + j][:, plo:plo + pnn], lhsT=vv[s_][:, j, pkb, :], rhs=pb[psl][:, j, 0:pnn],
                             start=(pkb == 0), stop=last, r=[("vv", s_), ("pb", psl)], w=[("bank", ob + j)])
                return f

            def make_post(hp=hp, gi=gi, g0=g0, n=n, OT=OT, nm=nm):
                def post():
                    slots = []
                    for j in range(2):
                        r_ = rr[j]; RR = ("rr", j)
                        if j == 0:
                            po, pz = slice(0, 64), slice(64, 128)
                        else:
                            po, pz = slice(64, 128), slice(0, 64)
                        k.dve("tensor_copy", out=osb[j][po, 0:n], in_=C.bank[ob + j][po, 0:n], r=[("bank", ob + j)], w=[("osb", j)])
                        k.dve("tensor_copy", out=r_[po, 0:n], in_=C.bank[ob + j][pz, 0:n], r=[("bank", ob + j)], w=[RR])
                        slots.append((j, r_, RR, po))
                    for (j, r_, RR, po) in slots:
                        h = 2 * hp + j
                        os_ = ost[j]; OS = ("ost", j)
                        k.dve("reciprocal", out=r_[po, 0:n], in_=r_[po, 0:n], r=[RR], w=[RR])
                        k.dve("tensor_tensor", out=os_[po, 0:n], in0=osb[j][po, 0:n], in1=r_[po, 0:n], op=ALU.mult, r=[("osb", j), RR], w=[OS])
                        k.dma(OT[po, hp, g0:g0 + n], os_[po, 0:n], r=[OS], w=[(nm, (h, gi))])
                return post

            for kb in range(nkb):
                lo = max(0, kb * 128 - g0)
                nn = n - lo
                sl_ = step % 3
                step += 1
                diag = kb >= qb0
                for j in range(2):
                    bk = 2 * sl_ + j
                    k.pe("matmul", C.bank[bk][:, 0:nn], lhsT=kT[s_][0:Dk, j, kb * 128:(kb + 1) * 128], rhs=qT[s_][0:Dk, j, g0 + lo:g0 + n],
                         start=True, stop=not (diag or kb == 0), r=[("kT", s_), ("qT", s_)], w=[("bank", bk)])
                    if kb == 0:
                        k.pe("matmul", C.bank[bk][:, 0:nn], lhsT=C.identb[:], rhs=C.pmask[:, 0:nn], start=False, stop=not diag,
                             r=["identb", "pmask"], w=[("bank", bk)])
                    if diag:
                        cm = C.cmask0 if kb == 0 else C.cmask
                        k.pe("matmul", C.bank[bk][:, 0:128], lhsT=C.identb[:], rhs=cm[:], start=False, stop=True,
                             r=["identb", "cmask", "cmask0"], w=[("bank", bk)])
                k.act("activation", out=pb[sl_][:, :, 0:nn], in_=C.psall[:, 2 * sl_:2 * sl_ + 2, 0:nn], func=AF.Exp, scale=scale,
                      r=[("bank", 2 * sl_), ("bank", 2 * sl_ + 1)], w=[("pb", sl_)])
                last = (kb == nkb - 1)
                pend.append((make_pv(kb, lo, nn, sl_, last), make_post() if last else None))
                drain(2)
    drain(0)


def phase3(k, l, Hin, hin_name, Hout, hout_name):
    C = k.C
    k.new_phase()
    alloc_ln(k)
    load_ln_params(k, C.ln1_g[l], C.ln1_b[l])
    win = C.w_in[l].rearrange("(kc p) c -> p kc c", p=128)
    wgf = k.psb("wgf", [128, KC, D], BF16); wgm = k.psb("wgm", [128, KC, D], BF16)
    wof = k.psb("wof", [128, 4, D], BF16); wom = k.psb("wom", [128, 4, D], BF16)
    wo = k.psb("wo", [128, KC, D], BF16)
    otf = [k.psb("otf%d" % i, [128, 4, 512], BF16) for i in range(2)]
    otm = [k.psb("otm%d" % i, [128, 4, 512], BF16) for i in range(2)]
    mT = k.psb("mT", [128, KC, 512], BF16)
    sg = [k.psb("sg%d" % i, [128, 512], BF16) for i in range(4)]
    tt = [k.psb("tt%d" % i, [128, 512], F32) for i in range(4)]
    k.dma(wgf[:], win[:, :, OGF:OGF + D], w=["wgf"], cast=True)
    k.dma(wgm[:], win[:, :, OGM:OGM + D], w=["wgm"], cast=True)
    k.dma(wof[:], C.fox_w_o[l].rearrange("(m p) c -> p m c", p=128), w=["wof"], cast=True)
    k.dma(wom[:], C.mla_w_o[l].rearrange("(m p) c -> p m c", p=128), w=["wom"], cast=True)
    k.dma(wo[:], C.w_out[l].rearrange("(kc p) c -> p kc c", p=128), w=["wo"], cast=True)
    cc = 0
    lngen = [None]

    def load_ot(gi_):
        g0_, n_ = GROUPS[gi_]
        k.dma(otf[gi_ % 2][:, :, 0:n_], C.OTf[:, :, g0_:g0_ + n_], r=["OTf"], w=[("otf", gi_ % 2)])
        k.dma(otm[gi_ % 2][:, :, 0:n_], C.OTm[:, :, g0_:g0_ + n_], r=["OTm"], w=[("otm", gi_ % 2)])

    for gi, (g0, n) in enumerate(GROUPS):
        s_ = gi % 2
        tiles = list(range(g0 // 128, (g0 + n) // 128))
        hk = [("hT", i) for i in tiles]
        if gi == 0:
            load_ot(0)
        if gi + 1 < len(GROUPS):
            load_ot(gi + 1)
        for c in range(KC):
            b0 = 4 * (cc % 2)
            q_ = 2 * (cc % 2)
            cc += 1
            cs_ = slice(c * 128, (c + 1) * 128)
            for kc in range(KC):
                k.pe("matmul", C.bank[b0][:, 0:n], lhsT=wgf[:, kc, cs_], rhs=C.hT[:, kc, g0:g0 + n], start=(kc == 0), stop=(kc == KC - 1),
                     r=["wgf"] + hk, w=[("bank", b0)])
            for kc in range(KC):
                k.pe("matmul", C.bank[b0 + 1][:, 0:n], lhsT=wgm[:, kc, cs_], rhs=C.hT[:, kc, g0:g0 + n], start=(kc == 0), stop=(kc == KC - 1),
                     r=["wgm"] + hk, w=[("bank", b0 + 1)])
            for m in range(4):
                k.pe("matmul", C.bank[b0 + 2][:, 0:n], lhsT=wof[:, m, cs_], rhs=otf[s_][:, m, 0:n], start=(m == 0), stop=(m == 3),
                     r=["wof", ("otf", s_)], w=[("bank", b0 + 2)])
            for m in range(4):
                k.pe("matmul", C.bank[b0 + 3][:, 0:n], lhsT=wom[:, m, cs_], rhs=otm[s_][:, m, 0:n], start=(m == 0), stop=(m == 3),
                     r=["wom", ("otm", s_)], w=[("bank", b0 + 3)])
            k.act("activation", out=sg[q_][:, 0:n], in_=C.bank[b0][:, 0:n], func=AF.Sigmoid, r=[("bank", b0)], w=[("sg", q_)])
            k.act("activation", out=sg[q_ + 1][:, 0:n], in_=C.bank[b0 + 1][:, 0:n], func=AF.Sigmoid, r=[("bank", b0 + 1)], w=[("sg", q_ + 1)])
            k.dve("tensor_tensor", out=tt[q_][:, 0:n], in0=C.bank[b0 + 2][:, 0:n], in1=sg[q_][:, 0:n], op=ALU.mult,
                  r=[("bank", b0 + 2), ("sg", q_)], w=[("tt", q_)])
            k.dve("tensor_tensor", out=tt[q_ + 1][:, 0:n], in0=C.bank[b0 + 3][:, 0:n], in1=sg[q_ + 1][:, 0:n], op=ALU.mult,
                  r=[("bank", b0 + 3), ("sg", q_ + 1)], w=[("tt", q_ + 1)])
            k.pool("tensor_tensor", out=mT[:, c, 0:n], in0=tt[q_][:, 0:n], in1=tt[q_ + 1][:, 0:n], op=ALU.add,
                   r=[("tt", q_), ("tt", q_ + 1)], w=[("mT", c)])
            if lngen[0] is not None:
                if next(lngen[0], "done") == "done":
                    lngen[0] = None
        if lngen[0] is not None:
            for _ in lngen[0]:
                pass
            lngen[0] = None
        sl = ln_slots(k, len(tiles))
        items = []
        for j, i in enumerate(tiles):
            ls = sl[j]
            k.dma(C.lnx[ls][:], Hin[i * 128:(i + 1) * 128, :], r=[(hin_name, i)], w=[("lnx", ls)])
            for half in range(2):
                bk = 2 * (j % 2) + half
                for kc in range(KC):
                    k.pe("matmul", C.bank[bk][:, 0:512], lhsT=mT[:, kc, j * 128:(j + 1) * 128], rhs=wo[:, kc, half * 512:(half + 1) * 512],
                         start=(kc == 0), stop=(kc == KC - 1), r=["mT", "wo"], w=[("bank", bk)])
                hs = slice(half * 512, (half + 1) * 512)
                k.dve("scalar_tensor_tensor", out=C.lnx[ls][:, hs], in0=C.lnx[ls][:, hs], scalar=ALPHA, in1=C.bank[bk][:, 0:512],
                      op0=ALU.mult, op1=ALU.add, r=[("lnx", ls), ("bank", bk)], w=[("lnx", ls)])
            items.append((ls, i, Hout[i * 128:(i + 1) * 128, :], (hout_name, i), False))
        lngen[0] = ln_batch_gen(k, items)
    for _ in lngen[0]:
        pass
    ln_flush(k)


def phase4(k, l, Hin, hin_name, Hout, hout_name, last):
    C = k.C
    k.new_phase()
    alloc_ln(k)
    load_ln_params(k, C.ln2_g[l], C.ln2_b[l])
    rw = k.psb("rw", [128, KC, NE], BF16)
    rb = k.psb("rb", [128, NE], F32)
    comb = k.psb("comb", [128, NT, NE], F32)
    wg = [k.psb("wg%d" % i, [128, KC, DE], BF16) for i in range(2)]
    wu = [k.psb("wu%d" % i, [128, KC, DE], BF16) for i in range(2)]
    wd = [k.psb("wd%d" % i, [128, 2, D], BF16) for i in range(2)]
    NSG = 11
    yacc = k.psb("yacc", [128, NSG, D], F32)
    hid = [k.psb("hid%d" % i, [128, 2, 512], BF16) for i in range(2)]
    sl = [k.psb("sl%d" % i, [128, 512], F32) for i in range(2)]
    k.dma(rw[:], C.router_w.rearrange("(kc p) e -> p kc e", p=128), w=["rw"], cast=True)
    k.dma(rb[:], bcast_row(C.router_b, NE), w=["rb"])
    NR = NT * NE
    ra = [k.psb("ra%d" % i, [128, NR], F32) for i in range(5)]
    rsm = k.psb("rsm", [128, NT * 4 * 4], F32)
    lg = C.psall[:, 6:8, :].rearrange("p b c -> p (b c)")
    for i in range(NT):
        tc = slice(i * 128, (i + 1) * 128)
        for kc in range(KC):
            k.pe("matmul", lg[:, i * NE:(i + 1) * NE], lhsT=C.hT[:, kc, tc], rhs=rw[:, kc, :], start=(kc == 0), stop=(kc == KC - 1),
                 r=[("hT", i), "rw"], w=[("bank", 6), ("bank", 7)])
    sc, b_, eq, b2, ge = [t[:] for t in ra]
    G4 = NT * 4
    m1 = rsm[:, 0:G4]; m2 = rsm[:, G4:2 * G4]; gs = rsm[:, 2 * G4:3 * G4]; gsel = rsm[:, 3 * G4:4 * G4]
    gmax = k.psb("gmax", [128, NT], F32); den = k.psb("den", [128, NT], F32)
    v3 = lambda ap: ap.rearrange("p (g e) -> p g e", e=4)
    bc4 = lambda ap: ap.unsqueeze(2).broadcast_to([128, G4, 4])
    k.act("activation", out=sc, in_=lg[:, 0:NR], func=AF.Sigmoid, r=[("bank", 6), ("bank", 7)], w=["ra0"])
    k.dve("tensor_tensor", out=b_.rearrange("p (i e) -> p i e", e=NE), in0=sc.rearrange("p (i e) -> p i e", e=NE),
          in1=rb[:].unsqueeze(1).broadcast_to([128, NT, NE]), op=ALU.add, r=["ra0", "rb"], w=["ra1"])
    k.dve("tensor_reduce", out=m1, in_=v3(b_), axis=AX.X, op=ALU.max, r=["ra1"], w=["rsm"])
    k.dve("tensor_tensor", out=v3(eq), in0=v3(b_), in1=bc4(m1), op=ALU.is_equal, r=["ra1", "rsm"], w=["ra2"])
    k.dve("scalar_tensor_tensor", out=b2, in0=eq, scalar=-1e9, in1=b_, op0=ALU.mult, op1=ALU.add, r=["ra2", "ra1"], w=["ra3"])
    k.dve("tensor_reduce", out=m2, in_=v3(b2), axis=AX.X, op=ALU.max, r=["ra3"], w=["rsm"])
    k.dve("tensor_tensor", out=gs, in0=m1, in1=m2, op=ALU.add, r=["rsm"], w=["rsm"])
    k.dve("tensor_reduce", out=gmax[:], in_=gs.rearrange("p (i g) -> p i g", g=4), axis=AX.X, op=ALU.max, r=["rsm"], w=["gmax"])
    k.dve("tensor_tensor", out=gsel.rearrange("p (i g) -> p i g", g=4), in0=gs.rearrange("p (i g) -> p i g", g=4),
          in1=gmax[:].unsqueeze(2).broadcast_to([128, NT, 4]), op=ALU.is_equal, r=["rsm", "gmax"], w=["rsm"])
    k.dve("tensor_tensor", out=v3(ge), in0=v3(b_), in1=bc4(m2), op=ALU.is_ge, r=["ra1", "rsm"], w=["ra4"])
    k.dve("tensor_tensor", out=v3(ge), in0=v3(ge), in1=bc4(gsel), op=ALU.mult, r=["ra4", "rsm"], w=["ra4"])
    k.dve("tensor_tensor", out=eq, in0=ge, in1=sc, op=ALU.mult, r=["ra4", "ra0"], w=["ra2"])
    k.dve("tensor_reduce", out=den[:], in_=eq.rearrange("p (i e) -> p i e", e=NE), axis=AX.X, op=ALU.add, r=["ra2"], w=["den"])
    k.dve("reciprocal", out=den[:], in_=den[:], r=["den"], w=["den"])
    k.dve("tensor_tensor", out=comb[:], in0=eq.rearrange("p (i e) -> p i e", e=NE),
          in1=den[:].unsqueeze(2).broadcast_to([128, NT, NE]), op=ALU.mult, r=["ra2", "den"], w=["comb"])
    subs = []
    for t0 in range(0, NT, NSG):
        nt_ = min(NSG, NT - t0)
        for e in range(NE):
            for j0 in range(0, nt_, 4):
                subs.append((t0, nt_, e, j0, min(4, nt_ - j0)))
    wslot = {}
    wcnt = [0]

    def load_w(t0, e):
        if (t0, e) in wslot:
            return
        ws = wcnt[0] % 2
        wcnt[0] += 1
        wslot[(t0, e)] = ws
        k.dma(wg[ws][:], C.w_gate[l, e].rearrange("(kc p) f -> p kc f", p=128), w=[("wg", ws)], cast=True)
        k.dma(wu[ws][:], C.w_up[l, e].rearrange("(kc p) f -> p kc f", p=128), w=[("wu", ws)], cast=True)
        k.dma(wd[ws][:], C.w_down[l, e].rearrange("(kc p) f -> p kc f", p=128), w=[("wd", ws)], cast=True)

    def gu_ops(si):
        t0, nt_, e, j0, ntile = subs[si]
        ws = wslot[(t0, e)]
        n = ntile * 128
        c0 = (t0 + j0) * 128
        hk = [("hT", i) for i in range(t0 + j0, t0 + j0 + ntile)]
        q_ = si % 2
        ops = []
        for ch in range(2):
            for (wt_, wn, bkk) in ((wg, "wg", ch), (wu, "wu", 2 + ch)):
                for kc in range(KC):
                    ops.append(lambda wt_=wt_, wn=wn, bkk=bkk, kc=kc, ch=ch: k.pe(
                        "matmul", C.bank[bkk][:, 0:n], lhsT=wt_[ws][:, kc, ch * 128:(ch + 1) * 128], rhs=C.hT[:, kc, c0:c0 + n],
                        start=(kc == 0), stop=(kc == KC - 1), r=[(wn, ws)] + hk, w=[("bank", bkk)]))

        def post(ch):
            k.act("activation", out=sl[ch][:, 0:n], in_=C.bank[ch][:, 0:n], func=AF.Silu, r=[("bank", ch)], w=[("sl", ch)])
            k.dve("tensor_tensor", out=hid[q_][:, ch, 0:n], in0=C.bank[2 + ch][:, 0:n], in1=sl[ch][:, 0:n], op=ALU.mult,
                  r=[("bank", 2 + ch), ("sl", ch)], w=[("hid", (q_, ch))])
        return ops, post

    ycnt = [0]

    def down_ops(si):
        t0, nt_, e, j0, ntile = subs[si]
        ws = wslot[(t0, e)]
        q_ = si % 2
        ops = []
        for j in range(ntile):
            tl = j0 + j
            for half in range(2):
                def f(j=j, tl=tl, half=half):
                    bk = 4 + ycnt[0] % 4
                    ycnt[0] += 1
                    for ch in range(2):
                        k.pe("matmul", C.bank[bk][:, 0:512], lhsT=hid[q_][:, ch, j * 128:(j + 1) * 128], rhs=wd[ws][:, ch, half * 512:(half + 1) * 512],
                             start=(ch == 0), stop=(ch == 1), r=[("hid", (q_, ch)), ("wd", ws)], w=[("bank", bk)])
                    ya = yacc[:, tl, half * 512:(half + 1) * 512]
                    cw = comb[:, t0 + tl, e:e + 1]
                    if e == 0:
                        k.dve("tensor_scalar", out=ya, in0=C.bank[bk][:, 0:512], scalar1=cw, scalar2=None, op0=ALU.mult,
                              r=[("bank", bk), ("comb", t0 + tl)], w=[("yacc", tl)])
                    else:
                        k.dve("scalar_tensor_tensor", out=ya, in0=C.bank[bk][:, 0:512], scalar=cw, in1=ya, op0=ALU.mult, op1=ALU.add,
                              r=[("bank", bk), ("comb", t0 + tl), ("yacc", tl)], w=[("yacc", tl)])
                ops.append(f)
        return ops

    def ln_tail(t0, nt_):
        tls = [tl for tl in range(nt_) if not (last and t0 + tl == 0)]
        for b0 in range(0, len(tls), NLN):
            bt = tls[b0:b0 + NLN]
            sl_ = ln_slots(k, len(bt))
            items = []
            for ls, tl in zip(sl_, bt):
                i = t0 + tl
                k.dma(C.lnx[ls][:], Hin[i * 128:(i + 1) * 128, :], r=[(hin_name, i)], w=[("lnx", ls)])
                k.dve("scalar_tensor_tensor", out=C.lnx[ls][:], in0=C.lnx[ls][:], scalar=ALPHA, in1=yacc[:, tl, :],
                      op0=ALU.mult, op1=ALU.add, r=[("lnx", ls), ("yacc", tl)], w=[("lnx", ls)])
                if last:
                    items.append((ls, i, C.out[(i - 1) * 128:i * 128, :], ("out", i), True))
                else:
                    items.append((ls, i, Hout[i * 128:(i + 1) * 128, :], (hout_name, i), False))
            ln_batch(k, items)
        ln_flush(k)

    jobs = []

    def tail_async(t0, nt_):
        tls = [tl for tl in range(nt_) if not (last and t0 + tl == 0)]
        for tl in tls:
            i = t0 + tl
            k.dma(C.Rs[i * 128:(i + 1) * 128, :], yacc[:, tl, :], r=[("yacc", tl)], w=[("Rs", i)])
        for b0 in range(0, len(tls), 2):
            bt = tls[b0:b0 + 2]
            state = {}

            def start(bt=bt, state=state):
                ln_slots(k, 4)
                items = []
                for ls, tl in enumerate(bt):
                    i = t0 + tl
                    k.dma(C.lnx[ls][:], C.Rs[i * 128:(i + 1) * 128, :], r=[("Rs", i)], w=[("lnx", ls)])
                    k.dma(C.lnx[ls + 2][:], Hin[i * 128:(i + 1) * 128, :], r=[(hin_name, i)], w=[("lnx", ls + 2)])
                    k.dve("scalar_tensor_tensor", out=C.lnx[ls][:], in0=C.lnx[ls + 2][:], scalar=ALPHA, in1=C.lnx[ls][:],
                          op0=ALU.mult, op1=ALU.add, r=[("lnx", ls), ("lnx", ls + 2)], w=[("lnx", ls)])
                    if last:
                        items.append((ls, i, C.out[(i - 1) * 128:i * 128, :], ("out", i), True))
                    else:
                        items.append((ls, i, Hout[i * 128:(i + 1) * 128, :], (hout_name, i), False))
                state["gen"] = ln_batch_gen(k, items)
            jobs.append(start)
            for _ in range(7):
                jobs.append(lambda state=state: next(state["gen"], None))

    def job_step():
        if jobs:
            jobs.pop(0)()

    NS = len(subs)
    load_w(subs[0][0], subs[0][2])
    gops, gpost = gu_ops(0)
    for f in gops[0:16]:
        f()
    gpost(0)
    for f in gops[16:32]:
        f()
    gpost(1)
    for si in range(NS):
        t0, nt_, e, j0, ntile = subs[si]
        job_step()
        if j0 == 0:
            nxt = [x for x in subs[si + 1:] if (x[0], x[2]) != (t0, e)]
            if nxt:
                load_w(nxt[0][0], nxt[0][2])
        dops = down_ops(si)
        last_of_sg = (si + 1 == NS) or (subs[si + 1][0] != t0)
        if si + 1 < NS and not last_of_sg:
            gops, gpost = gu_ops(si + 1)
            gi_ = 0
            nd = len(dops)
            early = max(0, nd - 3)
            per = (32 + early - 1) // max(1, early) if early else 32
            di = 0
            while gi_ < 32:
                stop_at = min(32, gi_ + per)
                while gi_ < stop_at:
                    gops[gi_]()
                    gi_ += 1
                    if gi_ == 16:
                        gpost(0)
                if di < early:
                    dops[di]()
                    di += 1
            gpost(1)
            while di < nd:
                dops[di]()
                di += 1
        else:
            for f in dops:
                f()
            if last_of_sg:
                if si + 1 < NS:
                    tail_async(t0, nt_)
                else:
                    while jobs:
                        job_step()
                    ln_tail(t0, nt_)
            if si + 1 < NS:
                gops, gpost = gu_ops(si + 1)
                for f in gops[0:16]:
                    f()
                gpost(0)
                for f in gops[16:32]:
                    f()
                gpost(1)


def build(upto=99, debug=False):
    k = K(upto, debug)
    nc = k.nc
    C = setup(k)
    if upto >= 1:
        phase1(k, 0, with_p0=True)
    else:
        phase0(k)
    if debug and upto == 1:
        for nm, ap in (("QTf", C.QTf), ("KTf", C.KTf), ("QTm", C.QTm), ("KTm", C.KTm), ("Vf", C.Vf), ("Vm", C.Vm)):
            o = k.dram_out("dbg_" + nm, list(ap.shape), BF16)
            k.dma(o, ap, r=[nm, "V"], w=["dbg_" + nm], final=True)
    if upto >= 2:
        attention(k, [(C.QTf, C.KTf, C.Vf, 68, 1.0, C.OTf, "OTf"), (C.QTm, C.KTm, C.Vm, 96, 96 ** -0.5, C.OTm, "OTm")])
    if debug and upto == 2:
        for nm, ap in (("OTf", C.OTf), ("OTm", C.OTm)):
            o = k.dram_out("dbg_" + nm, list(ap.shape), BF16)
            k.dma(o, ap, r=[nm], w=["dbg_" + nm], final=True)
    if upto >= 3:
        phase3(k, 0, C.H[0], "H0", C.H[1], "H1")
    if debug and upto == 3:
        o = k.dram_out("dbg_H1", [T, D], F32)
        k.dma(o, C.H[1], r=["H1"], w=["dbg_H1"], final=True)
    if upto >= 4:
        phase4(k, 0, C.H[1], "H1", C.H[0], "H0", False)
    if debug and upto == 4:
        o = k.dram_out("dbg_H2", [T, D], F32)
        k.dma(o, C.H[0], r=["H0"], w=["dbg_H2"], final=True)
    if upto >= 5:
        phase1(k, 1)
        attention(k, [(C.QTf, C.KTf, C.Vf, 68, 1.0, C.OTf, "OTf"), (C.QTm, C.KTm, C.Vm, 96, 96 ** -0.5, C.OTm, "OTm")])
        phase3(k, 1, C.H[0], "H0", C.H[1], "H1")
        phase4(k, 1, C.H[1], "H1", None, None, True)
    if debug and upto == 0:
        C.dbg_hT = k.dram_out("dbg_hT", [128, KC, T], BF16)
        k.dma(C.dbg_hT, C.hT[:], r=["hT"], w=["dbg_hT"], final=True)
        C.dbg_H = k.dram_out("dbg_H", [T, D], F32)
        k.dma(C.dbg_H, C.H[0], r=["H0"], w=["dbg_H"], final=True)
    finish(k)
    return k


def finish(k):
    nc = k.nc
    R = k.R
    sems = {e: nc.alloc_semaphore("sem_" + e) for e in R.ENGS}
    dsems = {}
    for e in ("sp", "pool"):
        for j in range(R.NQ):
            dsems[(e, j)] = nc.alloc_semaphore("dsem_%s_%d" % (e, j))
    R.replay(nc, sems, dsems)


def host_consts():
    c = {}
    c["c_ident"] = np.eye(128, dtype=np.float32)
    kk = np.arange(128)[:, None]; qq = np.arange(128)[None, :]
    c["c_cmask"] = np.where(kk > qq, NEG, 0.0).astype(np.float32)
    cm0 = np.where(kk > qq, NEG, 0.0).astype(np.float32)
    cm0[NPAD, :NPAD] = 0.0
    c["c_cmask0"] = cm0
    c["c_pmask"] = np.where(np.broadcast_to(kk, (128, 512)) < NPAD, NEG, 0.0).astype(np.float32)
    c["c_ut"] = (kk <= qq).astype(np.float32)
    c["c_padcol"] = (np.arange(128) >= NPAD).astype(np.float32)[:, None]
    inv = (10000.0 ** (-np.arange(16, dtype=np.float32) / 16)).astype(np.float32)
    pos = (np.arange(T) - NPAD).astype(np.float32)
    ang = pos[None, :] * inv[np.arange(128) % 16][:, None]
    cs = np.cos(ang).astype(np.float32)
    sn = np.sin(ang).astype(np.float32)
    sel_sin = ((np.arange(128) // 32) % 2 == 1)[:, None]
    c["c_cs"] = np.where(sel_sin, sn, cs).astype(np.float32)
    c["c_ones8"] = np.ones((8, T), np.float32)
    sel = np.zeros((48, NE, 128), np.float32)
    for e in range(NE):
        sel[e, e, :] = 1.0
        sel[32 + e, e, :] = 1.0
    c["c_sel"] = sel.reshape(48, NE * 128)
    return c


def make_in_maps(inputs):
    x = np.asarray(inputs["x"], np.float32)
    meta = np.asarray(inputs["meta_tokens"], np.float32)
    shared = {}
    for name in ["ln_in_g", "ln_in_b", "w_in", "fox_f_bias", "fox_w_o", "mla_q_norm", "mla_w_uq",
                 "mla_kv_norm", "mla_w_o", "w_out", "ln1_g", "ln1_b", "router_w", "router_b",
                 "w_gate", "w_up", "w_down", "ln2_g", "ln2_b"]:
        shared[name] = np.ascontiguousarray(np.asarray(inputs[name], np.float32))
    wkv = np.asarray(inputs["mla_w_ukv"], np.float32).reshape(DEPTH, 256, 8, 2, 64)
    shared["mla_w_ukv"] = np.ascontiguousarray(
        np.concatenate([wkv[:, :, :, 0, :].reshape(DEPTH, 256, 512), wkv[:, :, :, 1, :].reshape(DEPTH, 256, 512)], axis=2))
    shared.update(host_consts())
    maps = []
    for b in range(8):
        xp = np.zeros((T, D), np.float32)
        xp[NPAD:128] = meta
        xp[128:] = x[b]
        m = dict(shared)
        m["x"] = xp
        maps.append(m)
    return maps


_CACHE = {}


def kernel(**inputs):
    if "k" not in _CACHE:
        _CACHE["k"] = build()
    k = _CACHE["k"]
    maps = make_in_maps(inputs)
    maps = [{n: m[n] for n in k.inp} for m in maps]
    res = run_bass_kernel_spmd(k.nc, maps, core_ids=list(range(8)))
    out = np.stack([np.asarray(r["out"], np.float32) for r in res.results], axis=0)
    return out


def check_debug(r, R, relerr):
    f = lambda a: np.asarray(a).astype(np.float32)
    if "dbg_H" in r:
        print("H0 err", relerr(f(r["dbg_H"])[NPAD:], R["h0"]))
    if "dbg_hT" in r:
        hT = f(r["dbg_hT"]).transpose(2, 1, 0).reshape(T, D)
        print("hT err (vs h0)", relerr(hT[NPAD:], R["h0"]))
    L0 = R["L0"]
    if "dbg_H1" in r:
        print("H1 err", relerr(f(r["dbg_H1"])[NPAD:], L0["h1"]), np.isfinite(f(r["dbg_H1"])).all())
    if "dbg_H2" in r:
        print("H2 err", relerr(f(r["dbg_H2"])[NPAD:], L0["h2"]), np.isfinite(f(r["dbg_H2"])).all())
    if "out" in r and "dbg_H" not in r and "dbg_H1" not in r and "dbg_H2" not in r and "dbg_QTf" not in r and "dbg_OTf" not in r:
        print("OUT err", relerr(f(r["out"]), R["L1"]["h2"][NMETA:]), np.isfinite(f(r["out"])).all())
    for nm, rn in (("dbg_OTf", "ofox"), ("dbg_OTm", "omla")):
        if nm in r:
            o = f(r[nm])
            oh = np.stack([o[(hh % 2) * 64:(hh % 2) * 64 + 64, hh // 2, :] for hh in range(8)], 0)
            print(nm, "err", relerr(oh[:, :, NPAD:].transpose(0, 2, 1), L0[rn]), "finite", np.isfinite(o).all())
    if "dbg_QTf" in r:
        q = f(r["dbg_QTf"]); kk = f(r["dbg_KTf"])
        print("qf err", relerr(q[:, 0:64, NPAD:].transpose(0, 2, 1) * 8, L0["qf"]))
        print("kf err", relerr(kk[:, 0:64, NPAD:].transpose(0, 2, 1), L0["kf"]))
        print("q ones rows", q[:, 64:67].min(), q[:, 64:67].max(), "k ones row", kk[:, 67].min(), kk[:, 67].max())
        print("c row err", relerr(q[:, 67, NPAD:], L0["dec"]), np.abs(q[:, 67, :NPAD]).max())
        nc_ = kk[:, 64, NPAD:] + kk[:, 65, NPAD:] + kk[:, 66, NPAD:]
        print("-c split err", relerr(-nc_, L0["dec"]), np.abs(nc_ + L0["dec"]).max())
        v = f(r["dbg_Vf"])
        v = v.transpose(0, 2, 1, 3).reshape(8, T, 128)
        ve = np.concatenate([v[0::2, :, 0:64], v[1::2, :, 64:128]], 0)[[0, 4, 1, 5, 2, 6, 3, 7]]
        print("vf err", relerr(ve[:, NPAD:], L0["vf"]))
        on = np.concatenate([v[0::2, :, 64:128], v[1::2, :, 0:64]], 0)
        print("v ones", on.min(), on.max())
        q = f(r["dbg_QTm"]); kk = f(r["dbg_KTm"])
        print("qm err", relerr(q[:, :, NPAD:].transpose(0, 2, 1), L0["qm"]), relerr(q[:, 64:, NPAD:].transpose(0, 2, 1), L0["qm"][:, :, 64:]))
        print("km err", relerr(kk[:, :, NPAD:].transpose(0, 2, 1), L0["km"]), relerr(kk[:, 64:, NPAD:].transpose(0, 2, 1), L0["km"][:, :, 64:]))
        v = f(r["dbg_Vm"]).transpose(0, 2, 1, 3).reshape(8, T, 128)
        ve = np.concatenate([v[0::2, :, 0:64], v[1::2, :, 64:128]], 0)[[0, 4, 1, 5, 2, 6, 3, 7]]
        print("vm err", relerr(ve[:, NPAD:], L0["vm"]))
```

```python
import numpy as np
import concourse.bass as bass
import concourse.mybir as mybir
from concourse.bass_utils import run_bass_kernel_spmd

F32 = mybir.dt.float32
BF16 = mybir.dt.bfloat16
AF = mybir.ActivationFunctionType
ALU = mybir.AluOpType
AX = mybir.AxisListType

D = 1024
KC = 8
SEQ = 4096
NMETA = 16
NPAD = 112
T = SEQ + 128
NT = T // 128
DEPTH = 2
NE = 16
DE = 256
IN_COLS = 4264
ALPHA = (2 * DEPTH) ** 0.25
LN_EPS = 1e-5
RMS_EPS = 1e-6
NEG = -30000.0
GROUPS = [(g * 512, min(512, T - g * 512)) for g in range((T + 511) // 512)]
AGROUPS = [(0, 128)] + [(128 + 512 * g, 512) for g in range(8)]
OQ, OK_, OV, OF, OCQ, OCKV, OKR, OGF, OGM = 0, 512, 1024, 1536, 1544, 1928, 2184, 2216, 3240


class Op:
    __slots__ = ("eng", "meth", "args", "kw", "dma", "idx", "marked", "waits", "dsem", "dval", "ticket")

    def __init__(self, eng, meth, args, kw, dma):
        self.eng, self.meth, self.args, self.kw, self.dma = eng, meth, args, kw, dma
        self.marked = False
        self.waits = []
        self.dsem = None
        self.dval = 0


class Rec:
    ENGS = ("pe", "act", "dve", "pool", "sp")
    NQ = 12

    def __init__(self):
        self.ops = {e: [] for e in self.ENGS}
        self.state = {}
        self.seen = {e: {p: -1 for p in self.ENGS} for e in self.ENGS}
        self.seen_dma = {e: {} for e in self.ENGS}
        self.ndma = {e: 0 for e in self.ENGS}
        self.out_dmas = []
        self.pending = {}

    def _st(self, key):
        buf, sub = key if isinstance(key, tuple) else (key, None)
        d = self.state.setdefault(buf, {})
        return buf, sub, d

    def _deps(self, reads, writes):
        deps = []
        for key in reads:
            buf, sub, d = self._st(key)
            subs = list(d.keys()) if sub is None else [s for s in (sub, None) if s in d]
            for s in subs:
                if d[s][0] is not None:
                    deps.append(d[s][0])
        for key in writes:
            buf, sub, d = self._st(key)
            subs = list(d.keys()) if sub is None else [s for s in (sub, None) if s in d]
            for s in subs:
                if d[s][0] is not None:
                    deps.append(d[s][0])
                deps.extend(d[s][1])
        return deps

    def _update(self, op, reads, writes):
        for key in reads:
            buf, sub, d = self._st(key)
            if sub is None:
                for s in list(d.keys()):
                    d[s][1].append(op)
                d.setdefault(None, [None, []])
                if op not in d[None][1]:
                    d[None][1].append(op)
            else:
                d.setdefault(sub, [None, []])[1].append(op)
        for key in writes:
            buf, sub, d = self._st(key)
            if sub is None:
                d.clear()
                d[None] = [op, []]
            else:
                d[sub] = [op, []]

    def op(self, eng, meth, *args, r=(), w=(), dma=False, final=False, **kw):
        o = Op(eng, meth, args, kw, dma)
        lst = self.ops[eng]
        o.idx = len(lst)
        if self.pending.get(eng):
            o.waits.extend(self.pending[eng])
            self.pending[eng] = []
        for dpo in self._deps(r, w):
            if dpo is o:
                continue
            if dpo.dma:
                sd = self.seen_dma[eng]
                if sd.get(dpo.dsem, 0) >= dpo.dval:
                    continue
                sd[dpo.dsem] = dpo.dval
                o.waits.append(dpo)
            else:
                if dpo.eng == eng and eng in ("pe", "sp"):
                    continue
                if self.seen[eng][dpo.eng] >= dpo.idx:
                    continue
                self.seen[eng][dpo.eng] = dpo.idx
                dpo.marked = True
                o.waits.append(dpo)
        if dma:
            j = self.ndma[eng]
            self.ndma[eng] = j + 1
            o.dsem = (eng, j % self.NQ)
            o.dval = 16 * (j // self.NQ + 1)
            prev = 16 * (j // self.NQ)
            if prev > 0 and self.seen_dma[eng].get(o.dsem, 0) < prev:
                self.seen_dma[eng][o.dsem] = prev
                o.waits.append(("dma", o.dsem, prev))
            if final:
                self.out_dmas.append(o)
        lst.append(o)
        self._update(o, r, w)
        return o

    def barrier(self):
        lastop = {}
        for e in self.ENGS:
            for o in reversed(self.ops[e]):
                if not o.dma:
                    lastop[e] = o
                    break
        dmas = {}
        for e in self.ENGS:
            for o in self.ops[e]:
                if o.dma:
                    dmas[o.dsem] = max(dmas.get(o.dsem, 0), o.dval)
        for e in self.ENGS:
            pw = self.pending.setdefault(e, [])
            for p, o in lastop.items():
                if p == e and e in ("pe", "sp"):
                    continue
                if self.seen[e][p] >= o.idx:
                    continue
                self.seen[e][p] = o.idx
                o.marked = True
                pw.append(o)
            for ds, v in dmas.items():
                if self.seen_dma[e].get(ds, 0) < v:
                    self.seen_dma[e][ds] = v
                    pw.append(("dma", ds, v))
        self.state = {}

    def replay(self, nc, sems, dsems):
        engs = {"pe": "tensor", "act": "scalar", "dve": "vector", "pool": "gpsimd", "sp": "sync"}
        for e in self.ENGS:
            t = 0
            for o in self.ops[e]:
                if o.marked:
                    t += 1
                o.ticket = t
        with nc.Block() as block:
            for e in self.ENGS:
                ops = self.ops[e]
                last = (e == "sp")
                outs = self.out_dmas

                def body(eng, ops=ops, e=e, last=last):
                    for o in ops:
                        for wdep in o.waits:
                            if isinstance(wdep, tuple):
                                eng.wait_ge(dsems[wdep[1]], wdep[2])
                            elif wdep.dma:
                                eng.wait_ge(dsems[wdep.dsem], wdep.dval)
                            else:
                                eng.wait_ge(sems[wdep.eng], wdep.ticket)
                        ins = getattr(eng, o.meth)(*o.args, **o.kw)
                        if o.dma:
                            ins.then_inc(dsems[o.dsem], 16)
                        elif o.marked:
                            ins.then_inc(sems[o.eng], 1)
                    if last:
                        done = {}
                        for o in outs:
                            done[o.dsem] = max(done.get(o.dsem, 0), o.dval)
                        for s, v in done.items():
                            eng.wait_ge(dsems[s], v)

                getattr(block, engs[e])(body)


class K:
    def __init__(self, upto=99, debug=False):
        self.upto = upto
        self.debug = debug
        self.nc = nc = bass.Bass("TRN2", target_bir_lowering=False)
        self.R = Rec()
        self.sb_off = 17536
        self.ph_off = None
        self.nalloc = 0
        self.inp = {}
        self.tcount = 0

    def dram_in(self, name, shape, dt=F32):
        h = self.nc.dram_tensor(name, list(shape), dt, kind="ExternalInput")
        self.inp[name] = h
        return h.ap()

    def dram_out(self, name, shape, dt=F32):
        return self.nc.dram_tensor(name, list(shape), dt, kind="ExternalOutput").ap()

    def dram(self, name, shape, dt):
        return self.nc.dram_tensor(name, list(shape), dt).ap()

    def sb(self, name, shape, dt):
        return self._at(name, shape, dt, "persist")

    def _at(self, name, shape, dt, region):
        esz = {F32: 4, BF16: 2}[dt]
        nbytes = int(np.prod(shape[1:])) * esz
        nbytes = (nbytes + 63) // 64 * 64
        if region == "persist":
            off = self.sb_off
            self.sb_off += nbytes
            assert self.ph_off is None, "persistent alloc after phase alloc"
        else:
            if self.ph_off is None:
                self.ph_off = self.sb_off
            off = self.ph_off
            self.ph_off += nbytes
        assert off + nbytes <= 228000, ("SBUF overflow", name, off + nbytes)
        self.nalloc += 1
        return self.nc.alloc_sbuf_tensor_at("%s_%d" % (name, self.nalloc), list(shape), dt, offset=off)

    def psb(self, name, shape, dt):
        return self._at(name, shape, dt, "phase")

    def new_phase(self):
        self.R.barrier()
        self.ph_off = self.sb_off

    def ps(self, name, shape, dt=F32):
        return self.nc.alloc_psum_tensor(name, list(shape), dt)

    def pe(self, meth, *a, **k):
        return self.R.op("pe", meth, *a, **k)

    def act(self, meth, *a, **k):
        return self.R.op("act", meth, *a, **k)

    def dve(self, meth, *a, **k):
        return self.R.op("dve", meth, *a, **k)

    def pool(self, meth, *a, **k):
        return self.R.op("pool", meth, *a, **k)

    def dma(self, out, in_, r=(), w=(), cast=False, final=False):
        eng = "pool" if cast else "sp"
        return self.R.op(eng, "dma_start", out=out, in_=in_, r=r, w=w, dma=True, final=final)


def bcast_row(ap_1d, n, parts=128):
    return bass.AP(ap_1d.tensor, ap_1d.offset, [[0, parts], [1, n]])


class Ctx:
    pass


def setup(k):
    nc = k.nc
    C = Ctx()
    k.C = C
    C.x = k.dram_in("x", [T, D])
    C.ln_in_g = k.dram_in("ln_in_g", [D]); C.ln_in_b = k.dram_in("ln_in_b", [D])
    C.w_in = k.dram_in("w_in", [DEPTH, D, IN_COLS])
    C.fox_f_bias = k.dram_in("fox_f_bias", [DEPTH, 8])
    C.fox_w_o = k.dram_in("fox_w_o", [DEPTH, 512, D])
    C.mla_q_norm = k.dram_in("mla_q_norm", [DEPTH, 384])
    C.mla_w_uq = k.dram_in("mla_w_uq", [DEPTH, 384, 768])
    C.mla_kv_norm = k.dram_in("mla_kv_norm", [DEPTH, 256])
    C.mla_w_ukv = k.dram_in("mla_w_ukv", [DEPTH, 256, 1024])
    C.mla_w_o = k.dram_in("mla_w_o", [DEPTH, 512, D])
    C.w_out = k.dram_in("w_out", [DEPTH, D, D])
    C.ln1_g = k.dram_in("ln1_g", [DEPTH, D]); C.ln1_b = k.dram_in("ln1_b", [DEPTH, D])
    C.router_w = k.dram_in("router_w", [D, NE]); C.router_b = k.dram_in("router_b", [NE])
    C.w_gate = k.dram_in("w_gate", [DEPTH, NE, D, DE])
    C.w_up = k.dram_in("w_up", [DEPTH, NE, D, DE])
    C.w_down = k.dram_in("w_down", [DEPTH, NE, DE, D])
    C.ln2_g = k.dram_in("ln2_g", [DEPTH, D]); C.ln2_b = k.dram_in("ln2_b", [DEPTH, D])
    C.c_ident = k.dram_in("c_ident", [128, 128])
    C.c_cmask = k.dram_in("c_cmask", [128, 128])
    C.c_pmask = k.dram_in("c_pmask", [128, 512])
    C.c_cmask0 = k.dram_in("c_cmask0", [128, 128])
    C.c_ut = k.dram_in("c_ut", [128, 128])
    C.c_padcol = k.dram_in("c_padcol", [128, 1])
    C.c_sel = k.dram_in("c_sel", [48, NE * 128])
    C.c_ones8 = k.dram_in("c_ones8", [8, T])
    C.c_cs = k.dram_in("c_cs", [128, T])
    C.out = k.dram_out("out", [SEQ, D])
    C.H = [k.dram("Hs%d" % i, [T, D], F32) for i in range(2)]
    C.Rs = k.dram("Rs", [T, D], F32)
    C.QTf = k.dram("QTf", [8, 68, T], BF16); C.KTf = k.dram("KTf", [8, 68, T], BF16)
    C.QTm = k.dram("QTm", [8, 96, T], BF16); C.KTm = k.dram("KTm", [8, 96, T], BF16)
    C.Vf = k.dram("Vf", [8, 128, NT, 128], BF16); C.Vm = k.dram("Vm", [8, 128, NT, 128], BF16)
    C.OTf = k.dram("OTf", [128, 4, T], BF16); C.OTm = k.dram("OTm", [128, 4, T], BF16)
    C.psall = k.ps("psall", [128, 8, 512], F32)
    C.bank = [C.psall[:, i, :] for i in range(8)]
    C.hT = k.sb("hT", [128, KC, T], BF16)
    C.identb = k.sb("identb", [128, 128], BF16)
    C.identf = k.sb("identf", [128, 128], F32)
    C.padcol = k.sb("padcol", [128, 1], F32)
    C.onesf = k.sb("onesf", [128, 128], F32)
    C.utf = k.sb("utf", [128, 128], F32)
    C.cmask = k.sb("cmask", [128, 128], BF16)
    C.pmask = k.sb("pmask", [128, 512], BF16)
    C.cmask0 = k.sb("cmask0", [128, 128], BF16)
    C.ln_n = 0
    C.nhalf = k.sb("nhalf", [128, 2], F32)
    k.pool("memset", C.nhalf[:], -0.5, w=["nhalf"])
    k.dma(C.utf[:], C.c_ut, w=["utf"])
    k.dma(C.cmask[:], C.c_cmask, w=["cmask"], cast=True)
    k.dma(C.pmask[:], C.c_pmask, w=["pmask"], cast=True)
    k.dma(C.cmask0[:], C.c_cmask0, w=["cmask0"], cast=True)
    k.pool("memset", C.onesf[:], 1.0, w=["onesf"])
    for row in (64, 65, 66):
        k.dma(C.QTf[:, row, :], C.c_ones8, w=[("QTf", "ones%d" % row)], cast=True)
    k.dma(C.KTf[:, 67, :], C.c_ones8, w=[("KTf", "ones")], cast=True)
    k.dma(C.identb[:], C.c_ident, w=["identb"], cast=True)
    k.dma(C.identf[:], C.c_ident, w=["identf"])
    k.dma(C.padcol[:], C.c_padcol, w=["padcol"])
    return C


NLN = 4


def alloc_ln(k):
    C = k.C
    C.lnx = [k.psb("lnx%d" % i, [128, D], F32) for i in range(NLN)]
    C.hb = [k.psb("hb%d" % i, [128, D], BF16) for i in range(NLN)]
    C.lst = [k.psb("lst%d" % i, [128, 12], F32) for i in range(NLN)]
    C.lmv = [k.psb("lmv%d" % i, [128, 8], F32) for i in range(NLN)]
    C.gbc = k.psb("gbc", [128, D], F32); C.bbc = k.psb("bbc", [128, D], F32)
    C.ln_pending = []


def load_ln_params(k, g1d, b1d):
    C = k.C
    k.dma(C.gbc[:], bcast_row(g1d, D), w=["gbc"])
    k.dma(C.bbc[:], bcast_row(b1d, D), w=["bbc"])


def ln_slots(k, n):
    C = k.C
    ln_flush(k, 0)
    return list(range(n))


def ln_flush(k, keep=0):
    C = k.C
    while len(C.ln_pending) > keep:
        C.ln_pending.pop(0)()


def ln_batch(k, items):
    for _ in ln_batch_gen(k, items):
        pass


def ln_batch_gen(k, items):
    C = k.C
    def names(s):
        return C.lnx[s], C.lst[s], C.lmv[s], C.hb[s], ("lnx", s), ("lst", s), ("lmv", s), ("hb", s)
    half = (len(items) + 1) // 2
    for part in (items[:half], items[half:]):
        for (s, i, out_ap, outkey, final) in part:
            x, st, mv, hb, X, ST, MV, HB = names(s)
            k.dve("bn_stats", out=st[:, 0:6], in_=x[:, 0:512], r=[X], w=[ST])
            k.dve("bn_stats", out=st[:, 6:12], in_=x[:, 512:1024], r=[X], w=[ST])
            k.dve("bn_aggr", out=mv[:, 0:2], in_=st[:, 0:12], r=[ST], w=[MV])
        yield
    for (s, i, out_ap, outkey, final) in items:
        x, st, mv, hb, X, ST, MV, HB = names(s)
        k.pool("tensor_scalar", out=mv[:, 2:3], in0=mv[:, 1:2], scalar1=1.0, scalar2=LN_EPS, op0=ALU.mult, op1=ALU.add, r=[MV], w=[MV])
        k.pool("tensor_tensor", out=mv[:, 4:5], in0=mv[:, 2:3], in1=C.nhalf[:, 0:1], op=ALU.pow, r=[MV, "nhalf"], w=[MV])
    yield
    for (s, i, out_ap, outkey, final) in items:
        x, st, mv, hb, X, ST, MV, HB = names(s)
        k.dve("scalar_tensor_tensor", out=mv[:, 5:6], in0=mv[:, 0:1], scalar=-1.0, in1=mv[:, 4:5],
              op0=ALU.mult, op1=ALU.mult, r=[MV], w=[MV])
    for (s, i, out_ap, outkey, final) in items:
        x, st, mv, hb, X, ST, MV, HB = names(s)
        k.act("activation", out=x[:], in_=x[:], func=AF.Identity, scale=mv[:, 4:5], bias=mv[:, 5:6], r=[X, MV], w=[X])
    yield
    for (s, i, out_ap, outkey, final) in items:
        x, st, mv, hb, X, ST, MV, HB = names(s)
        k.dve("tensor_tensor", out=x[:], in0=x[:], in1=C.gbc[:], op=ALU.mult, r=[X, "gbc"], w=[X])
    yield
    for (s, i, out_ap, outkey, final) in items:
        x, st, mv, hb, X, ST, MV, HB = names(s)
        k.pool("tensor_tensor", out=x[:], in0=x[:], in1=C.bbc[:], op=ALU.add, r=[X, "bbc"], w=[X])
    yield
    for (s, i, out_ap, outkey, final) in items:
        x, st, mv, hb, X, ST, MV, HB = names(s)
        k.act("activation", out=hb[:], in_=x[:], func=AF.Copy, r=[X], w=[HB])
        k.dma(out_ap, x[:], r=[X], w=[outkey], final=final)
        C.ln_n += 1

        def deferred(i=i, hb=hb, HB=HB):
            bk = 6 + (i % 2)
            pst = C.bank[bk][:].bitcast(BF16)
            for kc in range(KC):
                k.pe("transpose", out=pst[:, kc * 128:(kc + 1) * 128], in_=hb[:, kc * 128:(kc + 1) * 128],
                     identity=C.identb[:], r=[HB, "identb"], w=[("bank", bk)])
            k.act("activation", out=C.hT[:, :, i * 128:(i + 1) * 128],
                  in_=pst.rearrange("p (k t) -> p k t", k=KC), func=AF.Copy,
                  r=[("bank", bk)], w=[("hT", i)])
        C.ln_pending.append(deferred)
    yield


def phase0(k):
    C = k.C
    k.new_phase()
    alloc_ln(k)
    load_ln_params(k, C.ln_in_g, C.ln_in_b)
    for i0 in range(0, NT, NLN):
        tiles = list(range(i0, min(NT, i0 + NLN)))
        sl = ln_slots(k, len(tiles))
        items = []
        for s, i in zip(sl, tiles):
            k.dma(C.lnx[s][:], C.x[i * 128:(i + 1) * 128, :], w=[("lnx", s)])
            items.append((s, i, C.H[0][i * 128:(i + 1) * 128, :], ("H0", i), False))
        ln_batch(k, items)
    ln_flush(k)


def evac_v(k, src_bank_ap, vst, s, bankkey, dst_dram, i):
    C = k.C
    sv = src_bank_ap.rearrange("p (m two d) -> p m two d", two=2, d=64)
    k.act("activation", out=vst[:, :, 0, 0:64], in_=sv[:, :, 0, :], func=AF.Copy, r=[bankkey], w=[("vst", s)])
    k.dve("tensor_copy", out=vst[:, :, 1, 64:128], in_=sv[:, :, 1, :], r=[bankkey], w=[("vst", s)])
    k.dma(dst_dram[:, :, i, :].rearrange("h p d -> p h d"), vst[:].rearrange("p m two d -> p (m two) d"),
          r=[("vst", s)], w=[("V", i)])


def phase1(k, l, with_p0=False):
    C = k.C
    k.new_phase()
    win = C.w_in[l].rearrange("(kc p) c -> p kc c", p=128)
    wv = k.psb("wv", [128, KC, 512], BF16)
    wcqf = k.psb("wcqf", [128, KC, 392], BF16)
    wckv = k.psb("wckv", [128, KC, 256], BF16)
    wt = [k.psb("wt%d" % i, [128, KC, 128], BF16) for i in range(2)]
    wkr = k.psb("wkr", [128, KC, 32], BF16); wkrR = k.psb("wkrR", [128, KC, 32], BF16)
    vst = [k.psb("vst%d" % i, [128, 4, 2, 128], BF16) for i in range(2)]
    cqnT = k.psb("cqnT", [128, 3, T], BF16); ckvnT = k.psb("ckvnT", [128, 2, T], BF16)
    augT = k.psb("augT", [32, T], BF16)
    fbias = k.psb("fbias", [128, 8], F32)
    sm = [k.psb("sm%d" % i, [128, 64], F32) for i in range(2)]
    junk = k.psb("junk", [128, 384], BF16)
    cqn = [k.psb("cqn%d" % i, [128, 640], BF16) for i in range(2)]
    stg = [k.psb("stg%d" % i, [128, 512], BF16) for i in range(3)]
    rt = [k.psb("rt%d" % i, [128, 512], F32) for i in range(2)]
    gq = k.psb("gq", [128, 8], F32)
    z_all = k.psb("z_all", [128, NT, 8], F32)
    lf_all = k.psb("lf_all", [128, NT, 8], F32)
    carry = k.psb("carry", [128, NT, 8], F32)
    aug_all = k.psb("aug_all", [128, NT, 32], F32)
    hb_all = k.psb("hb_all", [128, NT, 8], BF16)
    ov_off = k.ph_off
    cs = k.psb("cs", [128, T], F32)
    wuq = k.psb("wuq", [128, 3, 768], BF16); wuqR = k.psb("wuqR", [128, 3, 768], BF16)
    wukv = k.psb("wukv", [128, 2, 1024], BF16)
    stgw = k.psb("stgw", [128, 1024], F32)
    ALIAS = ["lnx", "hb", "lst", "lmv", "gbc", "bbc"] if with_p0 else []

    k.dma(wv[:], win[:, :, OV:OV + 512], w=["wv"], cast=True)
    k.dma(wcqf[:], win[:, :, OF:OF + 392], w=["wcqf"], cast=True)
    k.dma(wckv[:], win[:, :, OCKV:OCKV + 256], w=["wckv"], cast=True)
    k.dma(wkr[:], win[:, :, OKR:OKR + 32], w=["wkr"], cast=True)
    k.dma(fbias[:], bcast_row(C.fox_f_bias[l], 8), w=["fbias"])
    for kc in range(3):
        k.dma(gq[:, kc:kc + 1], C.mla_q_norm[l][kc * 128:(kc + 1) * 128].rearrange("(p o) -> p o", o=1), w=["gq"])
    for kc in range(2):
        k.dma(gq[:, 4 + kc:5 + kc], C.mla_kv_norm[l][kc * 128:(kc + 1) * 128].rearrange("(p o) -> p o", o=1), w=["gq"])
    for s_ in range(2):
        k.pool("memset", vst[s_][:, :, 0, 64:128], 1.0, w=[("vst", s_)])
        k.pool("memset", vst[s_][:, :, 1, 0:64], 1.0, w=[("vst", s_)])
    k.dve("tensor_scalar", out=wkrR[:, :, 0:16], in0=wkr[:, :, 16:32], scalar1=-1.0, scalar2=None, op0=ALU.mult, r=["wkr"], w=["wkrR"])
    k.dve("tensor_copy", out=wkrR[:, :, 16:32], in_=wkr[:, :, 0:16], r=["wkr"], w=["wkrR"])

    def mm_tile(i):
        s_ = i % 2
        tc = slice(i * 128, (i + 1) * 128)
        b0 = 3 * s_
        for (bk, w_, wn, ncol) in ((b0, wv, "wv", 512), (b0 + 1, wcqf, "wcqf", 392), (b0 + 2, wckv, "wckv", 256)):
            for kc in range(KC):
                k.pe("matmul", C.bank[bk][:, 0:ncol], lhsT=C.hT[:, kc, tc], rhs=w_[:, kc, :], start=(kc == 0), stop=(kc == KC - 1),
                     r=[("hT", i), wn], w=[("bank", bk)])

    def chain_tile(i):
        s_ = i % 2
        tc = slice(i * 128, (i + 1) * 128)
        b0 = 3 * s_
        bv, ba, bb = C.bank[b0], C.bank[b0 + 1], C.bank[b0 + 2]
        m = sm[s_]; M = ("sm", s_)
        k.act("activation", out=junk[:, 0:384], in_=ba[:, 8:392], func=AF.Square, scale=384 ** -0.5, accum_out=m[:, 0:1], r=[("bank", b0 + 1)], w=[M, "junk"])
        k.act("activation", out=junk[:, 0:256], in_=bb[:, 0:256], func=AF.Square, scale=256 ** -0.5, accum_out=m[:, 1:2], r=[("bank", b0 + 2)], w=[M, "junk"])
        evac_v(k, bv[:, 0:512], vst[s_], s_, ("bank", b0), C.Vf, i)
        k.dve("tensor_tensor", out=z_all[:, i, :], in0=ba[:, 0:8], in1=fbias[:], op=ALU.add, r=[("bank", b0 + 1), "fbias"], w=[("z_all", i)])
        k.pool("tensor_scalar", out=m[:, 2:4], in0=m[:, 0:2], scalar1=1.0, scalar2=RMS_EPS, op0=ALU.mult, op1=ALU.add, r=[M], w=[M])
        k.pool("tensor_tensor", out=m[:, 6:8], in0=m[:, 2:4], in1=C.nhalf[:, 0:2], op=ALU.pow, r=[M, "nhalf"], w=[M])
        cq = cqn[s_]; CQ = ("cqn", s_)
        k.dve("tensor_scalar", out=cq[:, 0:384], in0=ba[:, 8:392], scalar1=m[:, 6:7], scalar2=None, op0=ALU.mult, r=[M, ("bank", b0 + 1)], w=[CQ])
        k.dve("tensor_scalar", out=cq[:, 384:640], in0=bb[:, 0:256], scalar1=m[:, 7:8], scalar2=None, op0=ALU.mult, r=[M, ("bank", b0 + 2)], w=[CQ])
        pst = C.bank[6 + s_][:].bitcast(BF16)
        for j in range(5):
            k.pe("transpose", out=pst[:, j * 128:(j + 1) * 128], in_=cq[:, j * 128:(j + 1) * 128], identity=C.identb[:],
                 r=[CQ, "identb"], w=[("bank", 6 + s_)])
        k.act("activation", out=cqnT[:, :, tc], in_=pst[:, 0:384].rearrange("p (k t) -> p k t", k=3), func=AF.Copy,
              r=[("bank", 6 + s_)], w=[("cqnT", i)])
        k.act("activation", out=ckvnT[:, :, tc], in_=pst[:, 384:640].rearrange("p (k t) -> p k t", k=2), func=AF.Copy,
              r=[("bank", 6 + s_)], w=[("ckvnT", i)])

    pend_chain = []

    def stepA_push(i):
        mm_tile(i)
        if pend_chain:
            chain_tile(pend_chain.pop(0))
        pend_chain.append(i)

    if with_p0:
        end_off = k.ph_off
        k.ph_off = ov_off
        alloc_ln(k)
        assert k.ph_off <= end_off, "LN overlay too large"
        k.ph_off = end_off
        load_ln_params(k, C.ln_in_g, C.ln_in_b)
        prev_tiles = []
        for i0 in range(0, NT, NLN):
            tiles = list(range(i0, min(NT, i0 + NLN)))
            sl = ln_slots(k, len(tiles))
            items = []
            for s__, i in zip(sl, tiles):
                k.dma(C.lnx[s__][:], C.x[i * 128:(i + 1) * 128, :], w=[("lnx", s__)])
                items.append((s__, i, C.H[0][i * 128:(i + 1) * 128, :], ("H0", i), False))
            gen = ln_batch_gen(k, items)
            for i in prev_tiles:
                stepA_push(i)
                next(gen, None)
                next(gen, None)
            for _ in gen:
                pass
            prev_tiles = tiles
        ln_flush(k)
        for i in prev_tiles:
            stepA_push(i)
    else:
        for i in range(NT):
            stepA_push(i)
    chain_tile(pend_chain.pop(0))

    NF = NT * 8
    zf = z_all[:].rearrange("p i h -> p (i h)")
    lff = lf_all[:].rearrange("p i h -> p (i h)")
    k.act("activation", out=lff, in_=zf, func=AF.Exp, scale=-1.0, r=["z_all"], w=["lf_all"])
    k.act("activation", out=lff, in_=lff, func=AF.Ln, bias=1.0, r=["lf_all"], w=["lf_all"])
    k.dve("tensor_scalar", out=lff, in0=lff, scalar1=-1.0, scalar2=None, op0=ALU.mult, r=["lf_all"], w=["lf_all"])
    k.dve("tensor_scalar", out=lf_all[:, 0, :], in0=lf_all[:, 0, :], scalar1=C.padcol[:, 0:1], scalar2=None, op0=ALU.mult, r=["lf_all", "padcol"], w=["lf_all"])
    k.pe("matmul", C.bank[0][:, 0:NF], lhsT=C.utf[:], rhs=lff, start=True, stop=True, r=["lf_all", "utf"], w=[("bank", 0)])
    k.pe("matmul", C.bank[1][:, 0:NF], lhsT=C.onesf[:], rhs=lff, start=True, stop=True, r=["lf_all", "onesf"], w=[("bank", 1)])
    tot = C.bank[1][:, 0:NF].rearrange("p (i h) -> p i h", h=8)
    k.dve("memset", carry[:, 0, :], 0.0, w=["carry"])
    for i in range(1, NT):
        k.dve("tensor_tensor", out=carry[:, i, :], in0=carry[:, i - 1, :], in1=tot[:, i - 1, :], op=ALU.add, r=["carry", ("bank", 1)], w=["carry"])
    au = aug_all
    c3 = C.bank[0][:, 0:NF].rearrange("p (i h) -> p i h", h=8)
    k.dve("tensor_tensor", out=au[:, :, 0:8], in0=c3, in1=carry[:], op=ALU.add, r=[("bank", 0), "carry"], w=["aug_all"])
    k.dve("tensor_scalar", out=lf_all[:], in0=au[:, :, 0:8], scalar1=-1.0, scalar2=None, op0=ALU.mult, r=["aug_all"], w=["lf_all"])
    k.dve("tensor_copy", out=hb_all[:], in_=lf_all[:], r=["lf_all"], w=["hb_all"])
    k.dve("tensor_copy", out=au[:, :, 8:16], in_=hb_all[:], r=["hb_all"], w=["aug_all"])
    k.dve("tensor_tensor", out=lf_all[:], in0=lf_all[:], in1=au[:, :, 8:16], op=ALU.subtract, r=["lf_all", "aug_all"], w=["lf_all"])
    k.dve("tensor_copy", out=hb_all[:], in_=lf_all[:], r=["lf_all"], w=["hb_all"])
    k.dve("tensor_copy", out=au[:, :, 16:24], in_=hb_all[:], r=["hb_all"], w=["aug_all"])
    k.dve("tensor_tensor", out=au[:, :, 24:32], in0=lf_all[:], in1=au[:, :, 16:24], op=ALU.subtract, r=["lf_all", "aug_all"], w=["aug_all"])
    for i0 in range(0, NT, 4):
        n4 = min(4, NT - i0)
        bk = 2 + (i0 // 4) % 2
        for j in range(n4):
            k.pe("transpose", out=C.bank[bk][0:32, j * 128:(j + 1) * 128], in_=au[:, i0 + j, :], identity=C.identf[:],
                 r=["aug_all", "identf"], w=[("bank", bk)])
        k.dve("tensor_copy", out=augT[:, i0 * 128:(i0 + n4) * 128], in_=C.bank[bk][0:32, 0:n4 * 128], r=[("bank", bk)], w=["augT"])
    k.dma(C.QTf[:, 67, :], augT[0:8, :], r=["augT"], w=[("QTf", "c")])
    k.dma(C.KTf[:, 64, :], augT[8:16, :], r=["augT"], w=[("KTf", "hi")])
    k.dma(C.KTf[:, 65, :], augT[16:24, :], r=["augT"], w=[("KTf", "mid")])
    k.dma(C.KTf[:, 66, :], augT[24:32, :], r=["augT"], w=[("KTf", "lo")])

    k.dma(cs[:], C.c_cs, w=["cs"] + ALIAS)
    cnt = 0
    for c in range(8):
        s_ = c % 2
        k.dma(wt[s_][:], win[:, :, c * 128:(c + 1) * 128], w=[("wt", s_)], cast=True)
        dst = C.QTf if c < 4 else C.KTf
        dn = "QTf" if c < 4 else "KTf"
        h0 = 2 * (c % 4)
        for gi, (g0, n) in enumerate(GROUPS):
            bk = cnt % 6
            st_ = stg[cnt % 3]; SG = ("stg", cnt % 3)
            cnt += 1
            for kc in range(KC):
                k.pe("matmul", C.bank[bk][:, 0:n], lhsT=wt[s_][:, kc, :], rhs=C.hT[:, kc, g0:g0 + n], start=(kc == 0), stop=(kc == KC - 1),
                     r=[("wt", s_), "hT"], w=[("bank", bk)])
            if c < 4:
                k.act("activation", out=st_[:, 0:n], in_=C.bank[bk][:, 0:n], func=AF.Copy, scale=0.125, r=[("bank", bk)], w=[SG])
            else:
                k.dve("tensor_copy", out=st_[:, 0:n], in_=C.bank[bk][:, 0:n], r=[("bank", bk)], w=[SG])
            k.dma(dst[h0, 0:64, g0:g0 + n], st_[0:64, 0:n], r=[SG], w=[(dn, (h0, gi))])
            k.dma(dst[h0 + 1, 0:64, g0:g0 + n], st_[64:128, 0:n], r=[SG], w=[(dn, (h0 + 1, gi))])
    for gi, (g0, n) in enumerate(GROUPS):
        bA, bB = 6, 7
        for kc in range(KC):
            k.pe("matmul", C.bank[bA][0:32, 0:n], lhsT=wkr[:, kc, :], rhs=C.hT[:, kc, g0:g0 + n], start=(kc == 0), stop=(kc == KC - 1),
                 r=["wkr", "hT"], w=[("bank", bA)])
        for kc in range(KC):
            k.pe("matmul", C.bank[bB][0:32, 0:n], lhsT=wkrR[:, kc, :], rhs=C.hT[:, kc, g0:g0 + n], start=(kc == 0), stop=(kc == KC - 1),
                 r=["wkrR", "hT"], w=[("bank", bB)])
        st_ = stg[gi % 3]; SG = ("stg", gi % 3)
        k.dve("tensor_tensor", out=rt[0][0:32, 0:n], in0=C.bank[bA][0:32, 0:n], in1=cs[0:32, g0:g0 + n], op=ALU.mult, r=[("bank", bA), "cs"], w=[("rt", 0)])
        k.dve("tensor_tensor", out=rt[1][0:32, 0:n], in0=C.bank[bB][0:32, 0:n], in1=cs[32:64, g0:g0 + n], op=ALU.mult, r=[("bank", bB), "cs"], w=[("rt", 1)])
        k.dve("tensor_tensor", out=st_[0:32, 0:n], in0=rt[0][0:32, 0:n], in1=rt[1][0:32, 0:n], op=ALU.add, r=[("rt", 0), ("rt", 1)], w=[SG])
        for h in range(8):
            k.dma(C.KTm[h, 64:96, g0:g0 + n], st_[0:32, 0:n], r=[SG], w=[("KTm", (h, gi, "r"))])

    for kc in range(3):
        k.dma(stgw[:, 0:768], C.mla_w_uq[l][kc * 128:(kc + 1) * 128, :], w=["stgw"] + ALIAS)
        k.dve("tensor_scalar", out=wuq[:, kc, :], in0=stgw[:, 0:768], scalar1=gq[:, kc:kc + 1], scalar2=None, op0=ALU.mult, r=["stgw", "gq"], w=["wuq"] + ALIAS)
        k.dve("tensor_copy", out=wuqR[:, kc, :], in_=wuq[:, kc, :], r=["wuq"], w=["wuqR"] + ALIAS)
        wv_ = wuq[:, kc, :].rearrange("p (h d) -> p h d", d=96)
        wr_ = wuqR[:, kc, :].rearrange("p (h d) -> p h d", d=96)
        k.dve("tensor_scalar", out=wr_[:, :, 64:80], in0=wv_[:, :, 80:96], scalar1=-1.0, scalar2=None, op0=ALU.mult, r=["wuq"], w=["wuqR"])
        k.dve("tensor_copy", out=wr_[:, :, 80:96], in_=wv_[:, :, 64:80], r=["wuq"], w=["wuqR"])
    for kc in range(2):
        k.dma(stgw[:, 0:1024], C.mla_w_ukv[l][kc * 128:(kc + 1) * 128, :], w=["stgw"])
        k.dve("tensor_scalar", out=wukv[:, kc, :], in0=stgw[:, 0:1024], scalar1=gq[:, 4 + kc:5 + kc], scalar2=None, op0=ALU.mult, r=["stgw", "gq"], w=["wukv"] + ALIAS)
    cnt = 0
    for h in range(8):
        for gi, (g0, n) in enumerate(GROUPS):
            bA, bB, bC = 3 * (cnt % 2), 3 * (cnt % 2) + 1, 3 * (cnt % 2) + 2
            qs = stg[cnt % 3]; QS = ("stg", cnt % 3)
            cnt += 1
            for kc in range(3):
                k.pe("matmul", C.bank[bA][0:96, 0:n], lhsT=wuq[:, kc, h * 96:(h + 1) * 96], rhs=cqnT[:, kc, g0:g0 + n], start=(kc == 0), stop=(kc == 2),
                     r=["wuq", "cqnT"], w=[("bank", bA)])
            for kc in range(3):
                k.pe("matmul", C.bank[bB][0:96, 0:n], lhsT=wuqR[:, kc, h * 96:(h + 1) * 96], rhs=cqnT[:, kc, g0:g0 + n], start=(kc == 0), stop=(kc == 2),
                     r=["wuqR", "cqnT"], w=[("bank", bB)])
            for kc in range(2):
                k.pe("matmul", C.bank[bC][0:64, 0:n], lhsT=wukv[:, kc, h * 64:(h + 1) * 64], rhs=ckvnT[:, kc, g0:g0 + n], start=(kc == 0), stop=(kc == 1),
                     r=["wukv", "ckvnT"], w=[("bank", bC)])
            k.act("activation", out=qs[0:64, 0:n], in_=C.bank[bA][0:64, 0:n], func=AF.Copy, r=[("bank", bA)], w=[QS])
            k.dve("tensor_tensor", out=rt[0][64:96, 0:n], in0=C.bank[bA][64:96, 0:n], in1=cs[64:96, g0:g0 + n], op=ALU.mult, r=[("bank", bA), "cs"], w=[("rt", 0)])
            k.dve("tensor_tensor", out=rt[1][64:96, 0:n], in0=C.bank[bB][64:96, 0:n], in1=cs[96:128, g0:g0 + n], op=ALU.mult, r=[("bank", bB), "cs"], w=[("rt", 1)])
            k.dve("tensor_tensor", out=qs[64:96, 0:n], in0=rt[0][64:96, 0:n], in1=rt[1][64:96, 0:n], op=ALU.add, r=[("rt", 0), ("rt", 1)], w=[QS])
            k.dma(C.QTm[h, :, g0:g0 + n], qs[0:96, 0:n], r=[QS], w=[("QTm", (h, gi))])
            ks = stg[cnt % 3]; KS = ("stg", cnt % 3)
            cnt += 1
            k.act("activation", out=ks[0:64, 0:n], in_=C.bank[bC][0:64, 0:n], func=AF.Copy, r=[("bank", bC)], w=[KS])
            k.dma(C.KTm[h, 0:64, g0:g0 + n], ks[0:64, 0:n], r=[KS], w=[("KTm", (h, gi, "n"))])
    for i in range(NT):
        s_ = i % 2
        tc = slice(i * 128, (i + 1) * 128)
        bk = 6 + s_
        for kc in range(2):
            k.pe("matmul", C.bank[bk][:, 0:512], lhsT=ckvnT[:, kc, tc], rhs=wukv[:, kc, 512:1024], start=(kc == 0), stop=(kc == 1),
                 r=["wukv", ("ckvnT", i)], w=[("bank", bk)])
        evac_v(k, C.bank[bk][:, 0:512], vst[s_], s_, ("bank", bk), C.Vm, i)


def attention(k, calls):
    C = k.C
    k.new_phase()
    DKM = max(c[3] for c in calls)
    qT = [k.psb("qT%d" % i, [DKM, 2, T], BF16) for i in range(2)]
    kT = [k.psb("kT%d" % i, [DKM, 2, T], BF16) for i in range(2)]
    vv = [k.psb("vv%d" % i, [128, 2, NT, 128], BF16) for i in range(2)]
    pb = [k.psb("pb%d" % i, [128, 2, 512], BF16) for i in range(2)]
    rr = [k.psb("rr%d" % i, [128, 512], F32) for i in range(2)]
    osb = [k.psb("osb%d" % i, [128, 512], F32) for i in range(2)]
    pb.append(k.psb("pb2", [128, 2, 512], BF16))
    ost = [k.psb("ost%d" % i, [128, 512], BF16) for i in range(6)]
    ocnt = [0]
    step = 0
    pend = []

    def drain(keep):
        while len(pend) > keep:
            f, post = pend.pop(0)
            f()
            if post is not None:
                post()

    NHP = 4 * len(calls)

    def load_pair(hpg, part=None):
        QT, KT, V, Dk, scale, OT, nm = calls[hpg // 4]
        s_ = hpg % 2
        n_ = 0
        for j in range(2):
            h = 2 * (hpg % 4) + j
            for (dst, src, rk, wk) in ((qT[s_][0:Dk, j, :], QT[h], ["QTf", "QTm"], ("qT", s_)),
                                       (kT[s_][0:Dk, j, :], KT[h], ["KTf", "KTm"], ("kT", s_)),
                                       (vv[s_][:, j, :, :], V[h], ["V"], ("vv", s_))):
                if part is None or part == n_:
                    k.dma(dst, src, r=rk, w=[wk])
                n_ += 1

    load_pair(0)
    for hpg in range(NHP):
        QT, KT, V, Dk, scale, OT, nm = calls[hpg // 4]
        hp = hpg % 4
        s_ = hpg % 2
        for gi, (g0, n) in enumerate(AGROUPS):
            if 2 <= gi <= 7 and hpg + 1 < NHP:
                load_pair(hpg + 1, gi - 2)
            qb0 = g0 // 128
            nkb = qb0 + n // 128
            ob = 6

            def make_pv(pkb, plo, pnn, psl, last, s_=s_):
                def f():
                    for j in range(2):
                        k.pe("matmul", C.bank[ob + j][:, plo:plo + pnn], lhsT=vv[s_][:, j, pkb, :], rhs=pb[psl][:, j, 0:pnn],
                             start=(pkb == 0), stop=last, r=[("vv", s_), ("pb", psl)], w=[("bank", ob + j)])
                return f

            def make_post(hp=hp, gi=gi, g0=g0, n=n, OT=OT, nm=nm):
                def post():
                    slots = []
                    for j in range(2):
                        r_ = rr[j]; RR = ("rr", j)
                        if j == 0:
                            po, pz = slice(0, 64), slice(64, 128)
                        else:
                            po, pz = slice(64, 128), slice(0, 64)
                        k.dve("tensor_copy", out=osb[j][po, 0:n], in_=C.bank[ob + j][po, 0:n], r=[("bank", ob + j)], w=[("osb", j)])
                        k.dve("tensor_copy", out=r_[po, 0:n], in_=C.bank[ob + j][pz, 0:n], r=[("bank", ob + j)], w=[RR])
                        slots.append((j, r_, RR, po))
                    oc = ocnt[0] % 3
                    ocnt[0] += 1
                    for (j, r_, RR, po) in slots:
                        h = 2 * hp + j
                        os_ = ost[2 * oc + j]; OS = ("ost", 2 * oc + j)
                        k.dve("reciprocal", out=r_[po, 0:n], in_=r_[po, 0:n], r=[RR], w=[RR])
                        k.dve("tensor_tensor", out=os_[po, 0:n], in0=osb[j][po, 0:n], in1=r_[po, 0:n], op=ALU.mult, r=[("osb", j), RR], w=[OS])
                        k.dma(OT[po, hp, g0:g0 + n], os_[po, 0:n], r=[OS], w=[(nm, (h, gi))])
                return post

            for kb in range(nkb):
                lo = max(0, kb * 128 - g0)
                nn = n - lo
                sl_ = step % 3
                step += 1
                diag = kb >= qb0
                for j in range(2):
                    bk = 2 * sl_ + j
                    k.pe("matmul", C.bank[bk][:, 0:nn], lhsT=kT[s_][0:Dk, j, kb * 128:(kb + 1) * 128], rhs=qT[s_][0:Dk, j, g0 + lo:g0 + n],
                         start=True, stop=not (diag or kb == 0), r=[("kT", s_), ("qT", s_)], w=[("bank", bk)])
                    if kb == 0:
                        k.pe("matmul", C.bank[bk][:, 0:nn], lhsT=C.identb[:], rhs=C.pmask[:, 0:nn], start=False, stop=not diag,
                             r=["identb", "pmask"], w=[("bank", bk)])
                    if diag:
                        cm = C.cmask0 if kb == 0 else C.cmask
                        k.pe("matmul", C.bank[bk][:, 0:128], lhsT=C.identb[:], rhs=cm[:], start=False, stop=True,
                             r=["identb", "cmask", "cmask0"], w=[("bank", bk)])
                k.act("activation", out=pb[sl_][:, :, 0:nn], in_=C.psall[:, 2 * sl_:2 * sl_ + 2, 0:nn], func=AF.Exp, scale=scale,
                      r=[("bank", 2 * sl_), ("bank", 2 * sl_ + 1)], w=[("pb", sl_)])
                last = (kb == nkb - 1)
                pend.append((make_pv(kb, lo, nn, sl_, last), make_post() if last else None))
                drain(2)
    drain(0)


def phase3(k, l, Hin, hin_name, Hout, hout_name):
    C = k.C
    k.new_phase()
    alloc_ln(k)
    load_ln_params(k, C.ln1_g[l], C.ln1_b[l])
    win = C.w_in[l].rearrange("(kc p) c -> p kc c", p=128)
    wgf = k.psb("wgf", [128, KC, D], BF16); wgm = k.psb("wgm", [128, KC, D], BF16)
    wof = k.psb("wof", [128, 4, D], BF16); wom = k.psb("wom", [128, 4, D], BF16)
    wo = k.psb("wo", [128, KC, D], BF16)
    otf = [k.psb("otf%d" % i, [128, 4, 512], BF16) for i in range(2)]
    otm = [k.psb("otm%d" % i, [128, 4, 512], BF16) for i in range(2)]
    mT = k.psb("mT", [128, KC, 512], BF16)
    sg = [k.psb("sg%d" % i, [128, 512], BF16) for i in range(4)]
    tt = [k.psb("tt%d" % i, [128, 512], F32) for i in range(4)]
    k.dma(wgf[:], win[:, :, OGF:OGF + D], w=["wgf"], cast=True)
    k.dma(wgm[:], win[:, :, OGM:OGM + D], w=["wgm"], cast=True)
    k.dma(wof[:], C.fox_w_o[l].rearrange("(m p) c -> p m c", p=128), w=["wof"], cast=True)
    k.dma(wom[:], C.mla_w_o[l].rearrange("(m p) c -> p m c", p=128), w=["wom"], cast=True)
    k.dma(wo[:], C.w_out[l].rearrange("(kc p) c -> p kc c", p=128), w=["wo"], cast=True)
    cc = 0
    lngen = [None]

    def load_ot(gi_):
        g0_, n_ = GROUPS[gi_]
        k.dma(otf[gi_ % 2][:, :, 0:n_], C.OTf[:, :, g0_:g0_ + n_], r=["OTf"], w=[("otf", gi_ % 2)])
        k.dma(otm[gi_ % 2][:, :, 0:n_], C.OTm[:, :, g0_:g0_ + n_], r=["OTm"], w=[("otm", gi_ % 2)])

    for gi, (g0, n) in enumerate(GROUPS):
        s_ = gi % 2
        tiles = list(range(g0 // 128, (g0 + n) // 128))
        hk = [("hT", i) for i in tiles]
        if gi == 0:
            load_ot(0)
        if gi + 1 < len(GROUPS):
            load_ot(gi + 1)
        for c in range(KC):
            b0 = 4 * (cc % 2)
            q_ = 2 * (cc % 2)
            cc += 1
            cs_ = slice(c * 128, (c + 1) * 128)
            for kc in range(KC):
                k.pe("matmul", C.bank[b0][:, 0:n], lhsT=wgf[:, kc, cs_], rhs=C.hT[:, kc, g0:g0 + n], start=(kc == 0), stop=(kc == KC - 1),
                     r=["wgf"] + hk, w=[("bank", b0)])
            for kc in range(KC):
                k.pe("matmul", C.bank[b0 + 1][:, 0:n], lhsT=wgm[:, kc, cs_], rhs=C.hT[:, kc, g0:g0 + n], start=(kc == 0), stop=(kc == KC - 1),
                     r=["wgm"] + hk, w=[("bank", b0 + 1)])
            for m in range(4):
                k.pe("matmul", C.bank[b0 + 2][:, 0:n], lhsT=wof[:, m, cs_], rhs=otf[s_][:, m, 0:n], start=(m == 0), stop=(m == 3),
                     r=["wof", ("otf", s_)], w=[("bank", b0 + 2)])
            for m in range(4):
                k.pe("matmul", C.bank[b0 + 3][:, 0:n], lhsT=wom[:, m, cs_], rhs=otm[s_][:, m, 0:n], start=(m == 0), stop=(m == 3),
                     r=["wom", ("otm", s_)], w=[("bank", b0 + 3)])
            k.act("activation", out=sg[q_][:, 0:n], in_=C.bank[b0][:, 0:n], func=AF.Sigmoid, r=[("bank", b0)], w=[("sg", q_)])
            k.act("activation", out=sg[q_ + 1][:, 0:n], in_=C.bank[b0 + 1][:, 0:n], func=AF.Sigmoid, r=[("bank", b0 + 1)], w=[("sg", q_ + 1)])
            k.dve("tensor_tensor", out=tt[q_][:, 0:n], in0=C.bank[b0 + 2][:, 0:n], in1=sg[q_][:, 0:n], op=ALU.mult,
                  r=[("bank", b0 + 2), ("sg", q_)], w=[("tt", q_)])
            k.dve("tensor_tensor", out=tt[q_ + 1][:, 0:n], in0=C.bank[b0 + 3][:, 0:n], in1=sg[q_ + 1][:, 0:n], op=ALU.mult,
                  r=[("bank", b0 + 3), ("sg", q_ + 1)], w=[("tt", q_ + 1)])
            k.pool("tensor_tensor", out=mT[:, c, 0:n], in0=tt[q_][:, 0:n], in1=tt[q_ + 1][:, 0:n], op=ALU.add,
                   r=[("tt", q_), ("tt", q_ + 1)], w=[("mT", c)])
            if lngen[0] is not None:
                if next(lngen[0], "done") == "done":
                    lngen[0] = None
        if lngen[0] is not None:
            for _ in lngen[0]:
                pass
            lngen[0] = None
        sl = ln_slots(k, len(tiles))
        items = []
        for j, i in enumerate(tiles):
            ls = sl[j]
            k.dma(C.lnx[ls][:], Hin[i * 128:(i + 1) * 128, :], r=[(hin_name, i)], w=[("lnx", ls)])
            for half in range(2):
                bk = 2 * (j % 2) + half
                for kc in range(KC):
                    k.pe("matmul", C.bank[bk][:, 0:512], lhsT=mT[:, kc, j * 128:(j + 1) * 128], rhs=wo[:, kc, half * 512:(half + 1) * 512],
                         start=(kc == 0), stop=(kc == KC - 1), r=["mT", "wo"], w=[("bank", bk)])
                hs = slice(half * 512, (half + 1) * 512)
                k.dve("scalar_tensor_tensor", out=C.lnx[ls][:, hs], in0=C.lnx[ls][:, hs], scalar=ALPHA, in1=C.bank[bk][:, 0:512],
                      op0=ALU.mult, op1=ALU.add, r=[("lnx", ls), ("bank", bk)], w=[("lnx", ls)])
            items.append((ls, i, Hout[i * 128:(i + 1) * 128, :], (hout_name, i), False))
        lngen[0] = ln_batch_gen(k, items)
    for _ in lngen[0]:
        pass
    ln_flush(k)


def phase4(k, l, Hin, hin_name, Hout, hout_name, last):
    C = k.C
    k.new_phase()
    alloc_ln(k)
    load_ln_params(k, C.ln2_g[l], C.ln2_b[l])
    rw = k.psb("rw", [128, KC, NE], BF16)
    rb = k.psb("rb", [128, NE], F32)
    comb = k.psb("comb", [128, NT, NE], F32)
    wg = [k.psb("wg%d" % i, [128, KC, DE], BF16) for i in range(2)]
    wu = [k.psb("wu%d" % i, [128, KC, DE], BF16) for i in range(2)]
    wd = [k.psb("wd%d" % i, [128, 2, D], BF16) for i in range(2)]
    NSG = 11
    yacc = k.psb("yacc", [128, NSG, D], F32)
    hid = [k.psb("hid%d" % i, [128, 2, 512], BF16) for i in range(2)]
    sl = [k.psb("sl%d" % i, [128, 512], F32) for i in range(2)]
    k.dma(rw[:], C.router_w.rearrange("(kc p) e -> p kc e", p=128), w=["rw"], cast=True)
    k.dma(rb[:], bcast_row(C.router_b, NE), w=["rb"])
    NR = NT * NE
    ra = [k.psb("ra%d" % i, [128, NR], F32) for i in range(5)]
    rsm = k.psb("rsm", [128, NT * 4 * 4], F32)
    lg = C.psall[:, 6:8, :].rearrange("p b c -> p (b c)")
    for i in range(NT):
        tc = slice(i * 128, (i + 1) * 128)
        for kc in range(KC):
            k.pe("matmul", lg[:, i * NE:(i + 1) * NE], lhsT=C.hT[:, kc, tc], rhs=rw[:, kc, :], start=(kc == 0), stop=(kc == KC - 1),
                 r=[("hT", i), "rw"], w=[("bank", 6), ("bank", 7)])
    sc, b_, eq, b2, ge = [t[:] for t in ra]
    G4 = NT * 4
    m1 = rsm[:, 0:G4]; m2 = rsm[:, G4:2 * G4]; gs = rsm[:, 2 * G4:3 * G4]; gsel = rsm[:, 3 * G4:4 * G4]
    gmax = k.psb("gmax", [128, NT], F32); den = k.psb("den", [128, NT], F32)
    v3 = lambda ap: ap.rearrange("p (g e) -> p g e", e=4)
    bc4 = lambda ap: ap.unsqueeze(2).broadcast_to([128, G4, 4])
    k.act("activation", out=sc, in_=lg[:, 0:NR], func=AF.Sigmoid, r=[("bank", 6), ("bank", 7)], w=["ra0"])
    k.dve("tensor_tensor", out=b_.rearrange("p (i e) -> p i e", e=NE), in0=sc.rearrange("p (i e) -> p i e", e=NE),
          in1=rb[:].unsqueeze(1).broadcast_to([128, NT, NE]), op=ALU.add, r=["ra0", "rb"], w=["ra1"])
    k.dve("tensor_reduce", out=m1, in_=v3(b_), axis=AX.X, op=ALU.max, r=["ra1"], w=["rsm"])
    k.dve("tensor_tensor", out=v3(eq), in0=v3(b_), in1=bc4(m1), op=ALU.is_equal, r=["ra1", "rsm"], w=["ra2"])
    k.dve("scalar_tensor_tensor", out=b2, in0=eq, scalar=-1e9, in1=b_, op0=ALU.mult, op1=ALU.add, r=["ra2", "ra1"], w=["ra3"])
    k.dve("tensor_reduce", out=m2, in_=v3(b2), axis=AX.X, op=ALU.max, r=["ra3"], w=["rsm"])
    k.dve("tensor_tensor", out=gs, in0=m1, in1=m2, op=ALU.add, r=["rsm"], w=["rsm"])
    k.dve("tensor_reduce", out=gmax[:], in_=gs.rearrange("p (i g) -> p i g", g=4), axis=AX.X, op=ALU.max, r=["rsm"], w=["gmax"])
    k.dve("tensor_tensor", out=gsel.rearrange("p (i g) -> p i g", g=4), in0=gs.rearrange("p (i g) -> p i g", g=4),
          in1=gmax[:].unsqueeze(2).broadcast_to([128, NT, 4]), op=ALU.is_equal, r=["rsm", "gmax"], w=["rsm"])
    k.dve("tensor_tensor", out=v3(ge), in0=v3(b_), in1=bc4(m2), op=ALU.is_ge, r=["ra1", "rsm"], w=["ra4"])
    k.dve("tensor_tensor", out=v3(ge), in0=v3(ge), in1=bc4(gsel), op=ALU.mult, r=["ra4", "rsm"], w=["ra4"])
    k.dve("tensor_tensor", out=eq, in0=ge, in1=sc, op=ALU.mult, r=["ra4", "ra0"], w=["ra2"])
    k.dve("tensor_reduce", out=den[:], in_=eq.rearrange("p (i e) -> p i e", e=NE), axis=AX.X, op=ALU.add, r=["ra2"], w=["den"])
    k.dve("reciprocal", out=den[:], in_=den[:], r=["den"], w=["den"])
    k.dve("tensor_tensor", out=comb[:], in0=eq.rearrange("p (i e) -> p i e", e=NE),
          in1=den[:].unsqueeze(2).broadcast_to([128, NT, NE]), op=ALU.mult, r=["ra2", "den"], w=["comb"])
    subs = []
    for t0 in range(0, NT, NSG):
        nt_ = min(NSG, NT - t0)
        for e in range(NE):
            for j0 in range(0, nt_, 4):
                subs.append((t0, nt_, e, j0, min(4, nt_ - j0)))
    wslot = {}
    wcnt = [0]

    def load_w(t0, e):
        if (t0, e) in wslot:
            return
        ws = wcnt[0] % 2
        wcnt[0] += 1
        wslot[(t0, e)] = ws
        k.dma(wg[ws][:], C.w_gate[l, e].rearrange("(kc p) f -> p kc f", p=128), w=[("wg", ws)], cast=True)
        k.dma(wu[ws][:], C.w_up[l, e].rearrange("(kc p) f -> p kc f", p=128), w=[("wu", ws)], cast=True)
        k.dma(wd[ws][:], C.w_down[l, e].rearrange("(kc p) f -> p kc f", p=128), w=[("wd", ws)], cast=True)

    def gu_ops(si):
        t0, nt_, e, j0, ntile = subs[si]
        ws = wslot[(t0, e)]
        n = ntile * 128
        c0 = (t0 + j0) * 128
        hk = [("hT", i) for i in range(t0 + j0, t0 + j0 + ntile)]
        q_ = si % 2
        ops = []
        for ch in range(2):
            for (wt_, wn, bkk) in ((wg, "wg", ch), (wu, "wu", 2 + ch)):
                for kc in range(KC):
                    ops.append(lambda wt_=wt_, wn=wn, bkk=bkk, kc=kc, ch=ch: k.pe(
                        "matmul", C.bank[bkk][:, 0:n], lhsT=wt_[ws][:, kc, ch * 128:(ch + 1) * 128], rhs=C.hT[:, kc, c0:c0 + n],
                        start=(kc == 0), stop=(kc == KC - 1), r=[(wn, ws)] + hk, w=[("bank", bkk)]))

        def post(ch):
            k.act("activation", out=sl[ch][:, 0:n], in_=C.bank[ch][:, 0:n], func=AF.Silu, r=[("bank", ch)], w=[("sl", ch)])
            k.dve("tensor_tensor", out=hid[q_][:, ch, 0:n], in0=C.bank[2 + ch][:, 0:n], in1=sl[ch][:, 0:n], op=ALU.mult,
                  r=[("bank", 2 + ch), ("sl", ch)], w=[("hid", (q_, ch))])
        return ops, post

    ycnt = [0]

    def down_ops(si):
        t0, nt_, e, j0, ntile = subs[si]
        ws = wslot[(t0, e)]
        q_ = si % 2
        ops = []
        for j in range(ntile):
            tl = j0 + j
            for half in range(2):
                def f(j=j, tl=tl, half=half):
                    bk = 4 + ycnt[0] % 4
                    ycnt[0] += 1
                    for ch in range(2):
                        k.pe("matmul", C.bank[bk][:, 0:512], lhsT=hid[q_][:, ch, j * 128:(j + 1) * 128], rhs=wd[ws][:, ch, half * 512:(half + 1) * 512],
                             start=(ch == 0), stop=(ch == 1), r=[("hid", (q_, ch)), ("wd", ws)], w=[("bank", bk)])
                    ya = yacc[:, tl, half * 512:(half + 1) * 512]
                    cw = comb[:, t0 + tl, e:e + 1]
                    if e == 0:
                        k.dve("tensor_scalar", out=ya, in0=C.bank[bk][:, 0:512], scalar1=cw, scalar2=None, op0=ALU.mult,
                              r=[("bank", bk), ("comb", t0 + tl)], w=[("yacc", tl)])
                    else:
                        k.dve("scalar_tensor_tensor", out=ya, in0=C.bank[bk][:, 0:512], scalar=cw, in1=ya, op0=ALU.mult, op1=ALU.add,
                              r=[("bank", bk), ("comb", t0 + tl), ("yacc", tl)], w=[("yacc", tl)])
                ops.append(f)
        return ops

    def ln_tail(t0, nt_):
        tls = [tl for tl in range(nt_) if not (last and t0 + tl == 0)]
        for b0 in range(0, len(tls), NLN):
            bt = tls[b0:b0 + NLN]
            sl_ = ln_slots(k, len(bt))
            items = []
            for ls, tl in zip(sl_, bt):
                i = t0 + tl
                k.dma(C.lnx[ls][:], Hin[i * 128:(i + 1) * 128, :], r=[(hin_name, i)], w=[("lnx", ls)])
                k.dve("scalar_tensor_tensor", out=C.lnx[ls][:], in0=C.lnx[ls][:], scalar=ALPHA, in1=yacc[:, tl, :],
                      op0=ALU.mult, op1=ALU.add, r=[("lnx", ls), ("yacc", tl)], w=[("lnx", ls)])
                if last:
                    items.append((ls, i, C.out[(i - 1) * 128:i * 128, :], ("out", i), True))
                else:
                    items.append((ls, i, Hout[i * 128:(i + 1) * 128, :], (hout_name, i), False))
            ln_batch(k, items)
        ln_flush(k)

    jobs = []

    def tail_async(t0, nt_):
        tls = [tl for tl in range(nt_) if not (last and t0 + tl == 0)]
        for tl in tls:
            i = t0 + tl
            k.dma(C.Rs[i * 128:(i + 1) * 128, :], yacc[:, tl, :], r=[("yacc", tl)], w=[("Rs", i)])
        for b0 in range(0, len(tls), 2):
            bt = tls[b0:b0 + 2]
            state = {}

            def start(bt=bt, state=state):
                ln_slots(k, 4)
                items = []
                for ls, tl in enumerate(bt):
                    i = t0 + tl
                    k.dma(C.lnx[ls][:], C.Rs[i * 128:(i + 1) * 128, :], r=[("Rs", i)], w=[("lnx", ls)])
                    k.dma(C.lnx[ls + 2][:], Hin[i * 128:(i + 1) * 128, :], r=[(hin_name, i)], w=[("lnx", ls + 2)])
                    k.dve("scalar_tensor_tensor", out=C.lnx[ls][:], in0=C.lnx[ls + 2][:], scalar=ALPHA, in1=C.lnx[ls][:],
                          op0=ALU.mult, op1=ALU.add, r=[("lnx", ls), ("lnx", ls + 2)], w=[("lnx", ls)])
                    if last:
                        items.append((ls, i, C.out[(i - 1) * 128:i * 128, :], ("out", i), True))
                    else:
                        items.append((ls, i, Hout[i * 128:(i + 1) * 128, :], (hout_name, i), False))
                state["gen"] = ln_batch_gen(k, items)
            jobs.append(start)
            for _ in range(7):
                jobs.append(lambda state=state: next(state["gen"], None))

    def job_step():
        if jobs:
            jobs.pop(0)()

    NS = len(subs)
    load_w(subs[0][0], subs[0][2])
    gops, gpost = gu_ops(0)
    for f in gops[0:16]:
        f()
    gpost(0)
    for f in gops[16:32]:
        f()
    gpost(1)
    for si in range(NS):
        t0, nt_, e, j0, ntile = subs[si]
        job_step()
        if j0 == 0:
            nxt = [x for x in subs[si + 1:] if (x[0], x[2]) != (t0, e)]
            if nxt:
                load_w(nxt[0][0], nxt[0][2])
        dops = down_ops(si)
        last_of_sg = (si + 1 == NS) or (subs[si + 1][0] != t0)
        if si + 1 < NS and not last_of_sg:
            gops, gpost = gu_ops(si + 1)
            gi_ = 0
            nd = len(dops)
            early = max(0, nd - 3)
            per = (32 + early - 1) // max(1, early) if early else 32
            di = 0
            while gi_ < 32:
                stop_at = min(32, gi_ + per)
                while gi_ < stop_at:
                    gops[gi_]()
                    gi_ += 1
                    if gi_ == 16:
                        gpost(0)
                if di < early:
                    dops[di]()
                    di += 1
            gpost(1)
            while di < nd:
                dops[di]()
                di += 1
        else:
            for f in dops:
                f()
            if last_of_sg:
                if si + 1 < NS:
                    tail_async(t0, nt_)
                else:
                    while jobs:
                        job_step()
                    ln_tail(t0, nt_)
            if si + 1 < NS:
                gops, gpost = gu_ops(si + 1)
                for f in gops[0:16]:
                    f()
                gpost(0)
                for f in gops[16:32]:
                    f()
                gpost(1)


def build(upto=99, debug=False):
    k = K(upto, debug)
    nc = k.nc
    C = setup(k)
    if upto >= 1:
        phase1(k, 0, with_p0=True)
    else:
        phase0(k)
    if debug and upto == 1:
        for nm, ap in (("QTf", C.QTf), ("KTf", C.KTf), ("QTm", C.QTm), ("KTm", C.KTm), ("Vf", C.Vf), ("Vm", C.Vm)):
            o = k.dram_out("dbg_" + nm, list(ap.shape), BF16)
            k.dma(o, ap, r=[nm, "V"], w=["dbg_" + nm], final=True)
    if upto >= 2:
        attention(k, [(C.QTf, C.KTf, C.Vf, 68, 1.0, C.OTf, "OTf"), (C.QTm, C.KTm, C.Vm, 96, 96 ** -0.5, C.OTm, "OTm")])
    if debug and upto == 2:
        for nm, ap in (("OTf", C.OTf), ("OTm", C.OTm)):
            o = k.dram_out("dbg_" + nm, list(ap.shape), BF16)
            k.dma(o, ap, r=[nm], w=["dbg_" + nm], final=True)
    if upto >= 3:
        phase3(k, 0, C.H[0], "H0", C.H[1], "H1")
    if debug and upto == 3:
        o = k.dram_out("dbg_H1", [T, D], F32)
        k.dma(o, C.H[1], r=["H1"], w=["dbg_H1"], final=True)
    if upto >= 4:
        phase4(k, 0, C.H[1], "H1", C.H[0], "H0", False)
    if debug and upto == 4:
        o = k.dram_out("dbg_H2", [T, D], F32)
        k.dma(o, C.H[0], r=["H0"], w=["dbg_H2"], final=True)
    if upto >= 5:
        phase1(k, 1)
        attention(k, [(C.QTf, C.KTf, C.Vf, 68, 1.0, C.OTf, "OTf"), (C.QTm, C.KTm, C.Vm, 96, 96 ** -0.5, C.OTm, "OTm")])
        phase3(k, 1, C.H[0], "H0", C.H[1], "H1")
        phase4(k, 1, C.H[1], "H1", None, None, True)
    if debug and upto == 0:
        C.dbg_hT = k.dram_out("dbg_hT", [128, KC, T], BF16)
        k.dma(C.dbg_hT, C.hT[:], r=["hT"], w=["dbg_hT"], final=True)
        C.dbg_H = k.dram_out("dbg_H", [T, D], F32)
        k.dma(C.dbg_H, C.H[0], r=["H0"], w=["dbg_H"], final=True)
    finish(k)
    return k


def finish(k):
    nc = k.nc
    R = k.R
    sems = {e: nc.alloc_semaphore("sem_" + e) for e in R.ENGS}
    dsems = {}
    for e in ("sp", "pool"):
        for j in range(R.NQ):
            dsems[(e, j)] = nc.alloc_semaphore("dsem_%s_%d" % (e, j))
    R.replay(nc, sems, dsems)


def host_consts():
    c = {}
    c["c_ident"] = np.eye(128, dtype=np.float32)
    kk = np.arange(128)[:, None]; qq = np.arange(128)[None, :]
    c["c_cmask"] = np.where(kk > qq, NEG, 0.0).astype(np.float32)
    cm0 = np.where(kk > qq, NEG, 0.0).astype(np.float32)
    cm0[NPAD, :NPAD] = 0.0
    c["c_cmask0"] = cm0
    c["c_pmask"] = np.where(np.broadcast_to(kk, (128, 512)) < NPAD, NEG, 0.0).astype(np.float32)
    c["c_ut"] = (kk <= qq).astype(np.float32)
    c["c_padcol"] = (np.arange(128) >= NPAD).astype(np.float32)[:, None]
    inv = (10000.0 ** (-np.arange(16, dtype=np.float32) / 16)).astype(np.float32)
    pos = (np.arange(T) - NPAD).astype(np.float32)
    ang = pos[None, :] * inv[np.arange(128) % 16][:, None]
    cs = np.cos(ang).astype(np.float32)
    sn = np.sin(ang).astype(np.float32)
    sel_sin = ((np.arange(128) // 32) % 2 == 1)[:, None]
    c["c_cs"] = np.where(sel_sin, sn, cs).astype(np.float32)
    c["c_ones8"] = np.ones((8, T), np.float32)
    sel = np.zeros((48, NE, 128), np.float32)
    for e in range(NE):
        sel[e, e, :] = 1.0
        sel[32 + e, e, :] = 1.0
    c["c_sel"] = sel.reshape(48, NE * 128)
    return c


def make_in_maps(inputs):
    x = np.asarray(inputs["x"], np.float32)
    meta = np.asarray(inputs["meta_tokens"], np.float32)
    shared = {}
    for name in ["ln_in_g", "ln_in_b", "w_in", "fox_f_bias", "fox_w_o", "mla_q_norm", "mla_w_uq",
                 "mla_kv_norm", "mla_w_o", "w_out", "ln1_g", "ln1_b", "router_w", "router_b",
                 "w_gate", "w_up", "w_down", "ln2_g", "ln2_b"]:
        shared[name] = np.ascontiguousarray(np.asarray(inputs[name], np.float32))
    wkv = np.asarray(inputs["mla_w_ukv"], np.float32).reshape(DEPTH, 256, 8, 2, 64)
    shared["mla_w_ukv"] = np.ascontiguousarray(
        np.concatenate([wkv[:, :, :, 0, :].reshape(DEPTH, 256, 512), wkv[:, :, :, 1, :].reshape(DEPTH, 256, 512)], axis=2))
    shared.update(host_consts())
    maps = []
    for b in range(8):
        xp = np.zeros((T, D), np.float32)
        xp[NPAD:128] = meta
        xp[128:] = x[b]
        m = dict(shared)
        m["x"] = xp
        maps.append(m)
    return maps


_CACHE = {}


def kernel(**inputs):
    if "k" not in _CACHE:
        _CACHE["k"] = build()
    k = _CACHE["k"]
    maps = make_in_maps(inputs)
    maps = [{n: m[n] for n in k.inp} for m in maps]
    res = run_bass_kernel_spmd(k.nc, maps, core_ids=list(range(8)))
    out = np.stack([np.asarray(r["out"], np.float32) for r in res.results], axis=0)
    return out


def check_debug(r, R, relerr):
    f = lambda a: np.asarray(a).astype(np.float32)
    if "dbg_H" in r:
        print("H0 err", relerr(f(r["dbg_H"])[NPAD:], R["h0"]))
    if "dbg_hT" in r:
        hT = f(r["dbg_hT"]).transpose(2, 1, 0).reshape(T, D)
        print("hT err (vs h0)", relerr(hT[NPAD:], R["h0"]))
    L0 = R["L0"]
    if "dbg_H1" in r:
        print("H1 err", relerr(f(r["dbg_H1"])[NPAD:], L0["h1"]), np.isfinite(f(r["dbg_H1"])).all())
    if "dbg_H2" in r:
        print("H2 err", relerr(f(r["dbg_H2"])[NPAD:], L0["h2"]), np.isfinite(f(r["dbg_H2"])).all())
    if "out" in r and "dbg_H" not in r and "dbg_H1" not in r and "dbg_H2" not in r and "dbg_QTf" not in r and "dbg_OTf" not in r:
        print("OUT err", relerr(f(r["out"]), R["L1"]["h2"][NMETA:]), np.isfinite(f(r["out"])).all())
    for nm, rn in (("dbg_OTf", "ofox"), ("dbg_OTm", "omla")):
        if nm in r:
            o = f(r[nm])
            oh = np.stack([o[(hh % 2) * 64:(hh % 2) * 64 + 64, hh // 2, :] for hh in range(8)], 0)
            print(nm, "err", relerr(oh[:, :, NPAD:].transpose(0, 2, 1), L0[rn]), "finite", np.isfinite(o).all())
    if "dbg_QTf" in r:
        q = f(r["dbg_QTf"]); kk = f(r["dbg_KTf"])
        print("qf err", relerr(q[:, 0:64, NPAD:].transpose(0, 2, 1) * 8, L0["qf"]))
        print("kf err", relerr(kk[:, 0:64, NPAD:].transpose(0, 2, 1), L0["kf"]))
        print("q ones rows", q[:, 64:67].min(), q[:, 64:67].max(), "k ones row", kk[:, 67].min(), kk[:, 67].max())
        print("c row err", relerr(q[:, 67, NPAD:], L0["dec"]), np.abs(q[:, 67, :NPAD]).max())
        nc_ = kk[:, 64, NPAD:] + kk[:, 65, NPAD:] + kk[:, 66, NPAD:]
        print("-c split err", relerr(-nc_, L0["dec"]), np.abs(nc_ + L0["dec"]).max())
        v = f(r["dbg_Vf"])
        v = v.transpose(0, 2, 1, 3).reshape(8, T, 128)
        ve = np.concatenate([v[0::2, :, 0:64], v[1::2, :, 64:128]], 0)[[0, 4, 1, 5, 2, 6, 3, 7]]
        print("vf err", relerr(ve[:, NPAD:], L0["vf"]))
        on = np.concatenate([v[0::2, :, 64:128], v[1::2, :, 0:64]], 0)
        print("v ones", on.min(), on.max())
        q = f(r["dbg_QTm"]); kk = f(r["dbg_KTm"])
        print("qm err", relerr(q[:, :, NPAD:].transpose(0, 2, 1), L0["qm"]), relerr(q[:, 64:, NPAD:].transpose(0, 2, 1), L0["qm"][:, :, 64:]))
        print("km err", relerr(kk[:, :, NPAD:].transpose(0, 2, 1), L0["km"]), relerr(kk[:, 64:, NPAD:].transpose(0, 2, 1), L0["km"][:, :, 64:]))
        v = f(r["dbg_Vm"]).transpose(0, 2, 1, 3).reshape(8, T, 128)
        ve = np.concatenate([v[0::2, :, 0:64], v[1::2, :, 64:128]], 0)[[0, 4, 1, 5, 2, 6, 3, 7]]
        print("vm err", relerr(ve[:, NPAD:], L0["vm"]))
```

```python
import numpy as np
import concourse.bass as bass
import concourse.mybir as mybir
from concourse.bass_utils import run_bass_kernel_spmd

F32 = mybir.dt.float32
BF16 = mybir.dt.bfloat16
AF = mybir.ActivationFunctionType
ALU = mybir.AluOpType
AX = mybir.AxisListType

D = 1024
KC = 8
SEQ = 4096
NMETA = 16
NPAD = 112
T = SEQ + 128
NT = T // 128
DEPTH = 2
NE = 16
DE = 256
IN_COLS = 4264
ALPHA = (2 * DEPTH) ** 0.25
LN_EPS = 1e-5
RMS_EPS = 1e-6
NEG = -30000.0
GROUPS = [(g * 512, min(512, T - g * 512)) for g in range((T + 511) // 512)]
AGROUPS = [(0, 128)] + [(128 + 512 * g, 512) for g in range(8)]
OQ, OK_, OV, OF, OCQ, OCKV, OKR, OGF, OGM = 0, 512, 1024, 1536, 1544, 1928, 2184, 2216, 3240


class Op:
    __slots__ = ("eng", "meth", "args", "kw", "dma", "idx", "marked", "waits", "dsem", "dval", "ticket")

    def __init__(self, eng, meth, args, kw, dma):
        self.eng, self.meth, self.args, self.kw, self.dma = eng, meth, args, kw, dma
        self.marked = False
        self.waits = []
        self.dsem = None
        self.dval = 0


class Rec:
    ENGS = ("pe", "act", "dve", "pool", "sp")
    NQ = 12

    def __init__(self):
        self.ops = {e: [] for e in self.ENGS}
        self.state = {}
        self.seen = {e: {p: -1 for p in self.ENGS} for e in self.ENGS}
        self.seen_dma = {e: {} for e in self.ENGS}
        self.ndma = {e: 0 for e in self.ENGS}
        self.out_dmas = []
        self.pending = {}

    def _st(self, key):
        buf, sub = key if isinstance(key, tuple) else (key, None)
        d = self.state.setdefault(buf, {})
        return buf, sub, d

    def _deps(self, reads, writes):
        deps = []
        for key in reads:
            buf, sub, d = self._st(key)
            subs = list(d.keys()) if sub is None else [s for s in (sub, None) if s in d]
            for s in subs:
                if d[s][0] is not None:
                    deps.append(d[s][0])
        for key in writes:
            buf, sub, d = self._st(key)
            subs = list(d.keys()) if sub is None else [s for s in (sub, None) if s in d]
            for s in subs:
                if d[s][0] is not None:
                    deps.append(d[s][0])
                deps.extend(d[s][1])
        return deps

    def _update(self, op, reads, writes):
        for key in reads:
            buf, sub, d = self._st(key)
            if sub is None:
                for s in list(d.keys()):
                    d[s][1].append(op)
                d.setdefault(None, [None, []])
                if op not in d[None][1]:
                    d[None][1].append(op)
            else:
                d.setdefault(sub, [None, []])[1].append(op)
        for key in writes:
            buf, sub, d = self._st(key)
            if sub is None:
                d.clear()
                d[None] = [op, []]
            else:
                d[sub] = [op, []]

    def op(self, eng, meth, *args, r=(), w=(), dma=False, final=False, **kw):
        o = Op(eng, meth, args, kw, dma)
        lst = self.ops[eng]
        o.idx = len(lst)
        if self.pending.get(eng):
            o.waits.extend(self.pending[eng])
            self.pending[eng] = []
        for dpo in self._deps(r, w):
            if dpo is o:
                continue
            if dpo.dma:
                sd = self.seen_dma[eng]
                if sd.get(dpo.dsem, 0) >= dpo.dval:
                    continue
                sd[dpo.dsem] = dpo.dval
                o.waits.append(dpo)
            else:
                if dpo.eng == eng and eng in ("pe", "sp"):
                    continue
                if self.seen[eng][dpo.eng] >= dpo.idx:
                    continue
                self.seen[eng][dpo.eng] = dpo.idx
                dpo.marked = True
                o.waits.append(dpo)
        if dma:
            j = self.ndma[eng]
            self.ndma[eng] = j + 1
            o.dsem = (eng, j % self.NQ)
            o.dval = 16 * (j // self.NQ + 1)
            prev = 16 * (j // self.NQ)
            if prev > 0 and self.seen_dma[eng].get(o.dsem, 0) < prev:
                self.seen_dma[eng][o.dsem] = prev
                o.waits.append(("dma", o.dsem, prev))
            if final:
                self.out_dmas.append(o)
        lst.append(o)
        self._update(o, r, w)
        return o

    def barrier(self):
        lastop = {}
        for e in self.ENGS:
            for o in reversed(self.ops[e]):
                if not o.dma:
                    lastop[e] = o
                    break
        dmas = {}
        for e in self.ENGS:
            for o in self.ops[e]:
                if o.dma:
                    dmas[o.dsem] = max(dmas.get(o.dsem, 0), o.dval)
        for e in self.ENGS:
            pw = self.pending.setdefault(e, [])
            for p, o in lastop.items():
                if p == e and e in ("pe", "sp"):
                    continue
                if self.seen[e][p] >= o.idx:
                    continue
                self.seen[e][p] = o.idx
                o.marked = True
                pw.append(o)
            for ds, v in dmas.items():
                if self.seen_dma[e].get(ds, 0) < v:
                    self.seen_dma[e][ds] = v
                    pw.append(("dma", ds, v))
        self.state = {}

    def replay(self, nc, sems, dsems):
        engs = {"pe": "tensor", "act": "scalar", "dve": "vector", "pool": "gpsimd", "sp": "sync"}
        for e in self.ENGS:
            t = 0
            for o in self.ops[e]:
                if o.marked:
                    t += 1
                o.ticket = t
        with nc.Block() as block:
            for e in self.ENGS:
                ops = self.ops[e]
                last = (e == "sp")
                outs = self.out_dmas

                def body(eng, ops=ops, e=e, last=last):
                    for o in ops:
                        for wdep in o.waits:
                            if isinstance(wdep, tuple):
                                eng.wait_ge(dsems[wdep[1]], wdep[2])
                            elif wdep.dma:
                                eng.wait_ge(dsems[wdep.dsem], wdep.dval)
                            else:
                                eng.wait_ge(sems[wdep.eng], wdep.ticket)
                        ins = getattr(eng, o.meth)(*o.args, **o.kw)
                        if o.dma:
                            ins.then_inc(dsems[o.dsem], 16)
                        elif o.marked:
                            ins.then_inc(sems[o.eng], 1)
                    if last:
                        done = {}
                        for o in outs:
                            done[o.dsem] = max(done.get(o.dsem, 0), o.dval)
                        for s, v in done.items():
                            eng.wait_ge(dsems[s], v)

                getattr(block, engs[e])(body)


class K:
    def __init__(self, upto=99, debug=False):
        self.upto = upto
        self.debug = debug
        self.nc = nc = bass.Bass("TRN2", target_bir_lowering=False)
        self.R = Rec()
        self.sb_off = 17536
        self.ph_off = None
        self.nalloc = 0
        self.inp = {}
        self.tcount = 0

    def dram_in(self, name, shape, dt=F32):
        h = self.nc.dram_tensor(name, list(shape), dt, kind="ExternalInput")
        self.inp[name] = h
        return h.ap()

    def dram_out(self, name, shape, dt=F32):
        return self.nc.dram_tensor(name, list(shape), dt, kind="ExternalOutput").ap()

    def dram(self, name, shape, dt):
        return self.nc.dram_tensor(name, list(shape), dt).ap()

    def sb(self, name, shape, dt):
        return self._at(name, shape, dt, "persist")

    def _at(self, name, shape, dt, region):
        esz = {F32: 4, BF16: 2}[dt]
        nbytes = int(np.prod(shape[1:])) * esz
        nbytes = (nbytes + 63) // 64 * 64
        if region == "persist":
            off = self.sb_off
            self.sb_off += nbytes
            assert self.ph_off is None, "persistent alloc after phase alloc"
        else:
            if self.ph_off is None:
                self.ph_off = self.sb_off
            off = self.ph_off
            self.ph_off += nbytes
        assert off + nbytes <= 228000, ("SBUF overflow", name, off + nbytes)
        self.nalloc += 1
        return self.nc.alloc_sbuf_tensor_at("%s_%d" % (name, self.nalloc), list(shape), dt, offset=off)

    def psb(self, name, shape, dt):
        return self._at(name, shape, dt, "phase")

    def new_phase(self):
        self.R.barrier()
        self.ph_off = self.sb_off

    def ps(self, name, shape, dt=F32):
        return self.nc.alloc_psum_tensor(name, list(shape), dt)

    def pe(self, meth, *a, **k):
        return self.R.op("pe", meth, *a, **k)

    def act(self, meth, *a, **k):
        return self.R.op("act", meth, *a, **k)

    def dve(self, meth, *a, **k):
        return self.R.op("dve", meth, *a, **k)

    def pool(self, meth, *a, **k):
        return self.R.op("pool", meth, *a, **k)

    def dma(self, out, in_, r=(), w=(), cast=False, final=False):
        eng = "pool" if cast else "sp"
        return self.R.op(eng, "dma_start", out=out, in_=in_, r=r, w=w, dma=True, final=final)


def bcast_row(ap_1d, n, parts=128):
    return bass.AP(ap_1d.tensor, ap_1d.offset, [[0, parts], [1, n]])


class Ctx:
    pass


def setup(k):
    nc = k.nc
    C = Ctx()
    k.C = C
    C.x = k.dram_in("x", [T, D])
    C.ln_in_g = k.dram_in("ln_in_g", [D]); C.ln_in_b = k.dram_in("ln_in_b", [D])
    C.w_in = k.dram_in("w_in", [DEPTH, D, IN_COLS])
    C.fox_f_bias = k.dram_in("fox_f_bias", [DEPTH, 8])
    C.fox_w_o = k.dram_in("fox_w_o", [DEPTH, 512, D])
    C.mla_q_norm = k.dram_in("mla_q_norm", [DEPTH, 384])
    C.mla_w_uq = k.dram_in("mla_w_uq", [DEPTH, 384, 768])
    C.mla_kv_norm = k.dram_in("mla_kv_norm", [DEPTH, 256])
    C.mla_w_ukv = k.dram_in("mla_w_ukv", [DEPTH, 256, 1024])
    C.mla_w_o = k.dram_in("mla_w_o", [DEPTH, 512, D])
    C.w_out = k.dram_in("w_out", [DEPTH, D, D])
    C.ln1_g = k.dram_in("ln1_g", [DEPTH, D]); C.ln1_b = k.dram_in("ln1_b", [DEPTH, D])
    C.router_w = k.dram_in("router_w", [D, NE]); C.router_b = k.dram_in("router_b", [NE])
    C.w_gate = k.dram_in("w_gate", [DEPTH, NE, D, DE])
    C.w_up = k.dram_in("w_up", [DEPTH, NE, D, DE])
    C.w_down = k.dram_in("w_down", [DEPTH, NE, DE, D])
    C.ln2_g = k.dram_in("ln2_g", [DEPTH, D]); C.ln2_b = k.dram_in("ln2_b", [DEPTH, D])
    C.c_ident = k.dram_in("c_ident", [128, 128])
    C.c_cmask = k.dram_in("c_cmask", [128, 128])
    C.c_pmask = k.dram_in("c_pmask", [128, 512])
    C.c_cmask0 = k.dram_in("c_cmask0", [128, 128])
    C.c_ut = k.dram_in("c_ut", [128, 128])
    C.c_padcol = k.dram_in("c_padcol", [128, 1])
    C.c_sel = k.dram_in("c_sel", [48, NE * 128])
    C.c_ones8 = k.dram_in("c_ones8", [8, T])
    C.c_cs = k.dram_in("c_cs", [128, T])
    C.out = k.dram_out("out", [SEQ, D])
    C.H = [k.dram("Hs%d" % i, [T, D], F32) for i in range(2)]
    C.Rs = k.dram("Rs", [T, D], F32)
    C.QTf = k.dram("QTf", [8, 68, T], BF16); C.KTf = k.dram("KTf", [8, 68, T], BF16)
    C.QTm = k.dram("QTm", [8, 96, T], BF16); C.KTm = k.dram("KTm", [8, 96, T], BF16)
    C.Vf = k.dram("Vf", [8, 128, NT, 128], BF16); C.Vm = k.dram("Vm", [8, 128, NT, 128], BF16)
    C.OTf = k.dram("OTf", [128, 4, T], BF16); C.OTm = k.dram("OTm", [128, 4, T], BF16)
    C.psall = k.ps("psall", [128, 8, 512], F32)
    C.bank = [C.psall[:, i, :] for i in range(8)]
    C.hT = k.sb("hT", [128, KC, T], BF16)
    C.identb = k.sb("identb", [128, 128], BF16)
    C.identf = k.sb("identf", [128, 128], F32)
    C.padcol = k.sb("padcol", [128, 1], F32)
    C.onesf = k.sb("onesf", [128, 128], F32)
    C.utf = k.sb("utf", [128, 128], F32)
    C.cmask = k.sb("cmask", [128, 128], BF16)
    C.pmask = k.sb("pmask", [128, 512], BF16)
    C.cmask0 = k.sb("cmask0", [128, 128], BF16)
    C.ln_n = 0
    C.nhalf = k.sb("nhalf", [128, 2], F32)
    k.pool("memset", C.nhalf[:], -0.5, w=["nhalf"])
    k.dma(C.utf[:], C.c_ut, w=["utf"])
    k.dma(C.cmask[:], C.c_cmask, w=["cmask"], cast=True)
    k.dma(C.pmask[:], C.c_pmask, w=["pmask"], cast=True)
    k.dma(C.cmask0[:], C.c_cmask0, w=["cmask0"], cast=True)
    k.pool("memset", C.onesf[:], 1.0, w=["onesf"])
    for row in (64, 65, 66):
        k.dma(C.QTf[:, row, :], C.c_ones8, w=[("QTf", "ones%d" % row)], cast=True)
    k.dma(C.KTf[:, 67, :], C.c_ones8, w=[("KTf", "ones")], cast=True)
    k.dma(C.identb[:], C.c_ident, w=["identb"], cast=True)
    k.dma(C.identf[:], C.c_ident, w=["identf"])
    k.dma(C.padcol[:], C.c_padcol, w=["padcol"])
    return C


NLN = 4


def alloc_ln(k):
    C = k.C
    C.lnx = [k.psb("lnx%d" % i, [128, D], F32) for i in range(NLN)]
    C.hb = [k.psb("hb%d" % i, [128, D], BF16) for i in range(NLN)]
    C.lst = [k.psb("lst%d" % i, [128, 12], F32) for i in range(NLN)]
    C.lmv = [k.psb("lmv%d" % i, [128, 8], F32) for i in range(NLN)]
    C.gbc = k.psb("gbc", [128, D], F32); C.bbc = k.psb("bbc", [128, D], F32)
    C.ln_pending = []


def load_ln_params(k, g1d, b1d):
    C = k.C
    k.dma(C.gbc[:], bcast_row(g1d, D), w=["gbc"])
    k.dma(C.bbc[:], bcast_row(b1d, D), w=["bbc"])


def ln_slots(k, n):
    C = k.C
    ln_flush(k, 0)
    return list(range(n))


def ln_flush(k, keep=0):
    C = k.C
    while len(C.ln_pending) > keep:
        C.ln_pending.pop(0)()


def ln_batch(k, items):
    for _ in ln_batch_gen(k, items):
        pass


def ln_batch_gen(k, items):
    C = k.C
    def names(s):
        return C.lnx[s], C.lst[s], C.lmv[s], C.hb[s], ("lnx", s), ("lst", s), ("lmv", s), ("hb", s)
    half = (len(items) + 1) // 2
    for part in (items[:half], items[half:]):
        for (s, i, out_ap, outkey, final) in part:
            x, st, mv, hb, X, ST, MV, HB = names(s)
            k.dve("bn_stats", out=st[:, 0:6], in_=x[:, 0:512], r=[X], w=[ST])
            k.dve("bn_stats", out=st[:, 6:12], in_=x[:, 512:1024], r=[X], w=[ST])
            k.dve("bn_aggr", out=mv[:, 0:2], in_=st[:, 0:12], r=[ST], w=[MV])
        yield
    for (s, i, out_ap, outkey, final) in items:
        x, st, mv, hb, X, ST, MV, HB = names(s)
        k.pool("tensor_scalar", out=mv[:, 2:3], in0=mv[:, 1:2], scalar1=1.0, scalar2=LN_EPS, op0=ALU.mult, op1=ALU.add, r=[MV], w=[MV])
        k.pool("tensor_tensor", out=mv[:, 4:5], in0=mv[:, 2:3], in1=C.nhalf[:, 0:1], op=ALU.pow, r=[MV, "nhalf"], w=[MV])
    yield
    for (s, i, out_ap, outkey, final) in items:
        x, st, mv, hb, X, ST, MV, HB = names(s)
        k.dve("scalar_tensor_tensor", out=mv[:, 5:6], in0=mv[:, 0:1], scalar=-1.0, in1=mv[:, 4:5],
              op0=ALU.mult, op1=ALU.mult, r=[MV], w=[MV])
    for (s, i, out_ap, outkey, final) in items:
        x, st, mv, hb, X, ST, MV, HB = names(s)
        k.act("activation", out=x[:], in_=x[:], func=AF.Identity, scale=mv[:, 4:5], bias=mv[:, 5:6], r=[X, MV], w=[X])
    yield
    for (s, i, out_ap, outkey, final) in items:
        x, st, mv, hb, X, ST, MV, HB = names(s)
        k.dve("tensor_tensor", out=x[:], in0=x[:], in1=C.gbc[:], op=ALU.mult, r=[X, "gbc"], w=[X])
    yield
    for (s, i, out_ap, outkey, final) in items:
        x, st, mv, hb, X, ST, MV, HB = names(s)
        k.pool("tensor_tensor", out=x[:], in0=x[:], in1=C.bbc[:], op=ALU.add, r=[X, "bbc"], w=[X])
    yield
    for (s, i, out_ap, outkey, final) in items:
        x, st, mv, hb, X, ST, MV, HB = names(s)
        k.act("activation", out=hb[:], in_=x[:], func=AF.Copy, r=[X], w=[HB])
        k.dma(out_ap, x[:], r=[X], w=[outkey], final=final)
        C.ln_n += 1

        def deferred(i=i, hb=hb, HB=HB):
            bk = 6 + (i % 2)
            pst = C.bank[bk][:].bitcast(BF16)
            for kc in range(KC):
                k.pe("transpose", out=pst[:, kc * 128:(kc + 1) * 128], in_=hb[:, kc * 128:(kc + 1) * 128],
                     identity=C.identb[:], r=[HB, "identb"], w=[("bank", bk)])
            k.act("activation", out=C.hT[:, :, i * 128:(i + 1) * 128],
                  in_=pst.rearrange("p (k t) -> p k t", k=KC), func=AF.Copy,
                  r=[("bank", bk)], w=[("hT", i)])
        C.ln_pending.append(deferred)
    yield


def phase0(k):
    C = k.C
    k.new_phase()
    alloc_ln(k)
    load_ln_params(k, C.ln_in_g, C.ln_in_b)
    for i0 in range(0, NT, NLN):
        tiles = list(range(i0, min(NT, i0 + NLN)))
        sl = ln_slots(k, len(tiles))
        items = []
        for s, i in zip(sl, tiles):
            k.dma(C.lnx[s][:], C.x[i * 128:(i + 1) * 128, :], w=[("lnx", s)])
            items.append((s, i, C.H[0][i * 128:(i + 1) * 128, :], ("H0", i), False))
        ln_batch(k, items)
    ln_flush(k)


def evac_v(k, src_bank_ap, vst, s, bankkey, dst_dram, i):
    C = k.C
    sv = src_bank_ap.rearrange("p (m two d) -> p m two d", two=2, d=64)
    k.act("activation", out=vst[:, :, 0, 0:64], in_=sv[:, :, 0, :], func=AF.Copy, r=[bankkey], w=[("vst", s)])
    k.dve("tensor_copy", out=vst[:, :, 1, 64:128], in_=sv[:, :, 1, :], r=[bankkey], w=[("vst", s)])
    k.dma(dst_dram[:, :, i, :].rearrange("h p d -> p h d"), vst[:].rearrange("p m two d -> p (m two) d"),
          r=[("vst", s)], w=[("V", i)])


def phase1(k, l, with_p0=False):
    C = k.C
    k.new_phase()
    win = C.w_in[l].rearrange("(kc p) c -> p kc c", p=128)
    wv = k.psb("wv", [128, KC, 512], BF16)
    wcqf = k.psb("wcqf", [128, KC, 392], BF16)
    wckv = k.psb("wckv", [128, KC, 256], BF16)
    wt = [k.psb("wt%d" % i, [128, KC, 128], BF16) for i in range(2)]
    wkr = k.psb("wkr", [128, KC, 32], BF16); wkrR = k.psb("wkrR", [128, KC, 32], BF16)
    vst = [k.psb("vst%d" % i, [128, 4, 2, 128], BF16) for i in range(2)]
    cqnT = k.psb("cqnT", [128, 3, T], BF16); ckvnT = k.psb("ckvnT", [128, 2, T], BF16)
    augT = k.psb("augT", [32, T], BF16)
    fbias = k.psb("fbias", [128, 8], F32)
    sm = [k.psb("sm%d" % i, [128, 64], F32) for i in range(2)]
    junk = k.psb("junk", [128, 384], BF16)
    cqn = [k.psb("cqn%d" % i, [128, 640], BF16) for i in range(2)]
    stg = [k.psb("stg%d" % i, [128, 512], BF16) for i in range(3)]
    rt = [k.psb("rt%d" % i, [128, 512], F32) for i in range(2)]
    gq = k.psb("gq", [128, 8], F32)
    z_all = k.psb("z_all", [128, NT, 8], F32)
    lf_all = k.psb("lf_all", [128, NT, 8], F32)
    carry = k.psb("carry", [128, NT, 8], F32)
    aug_all = k.psb("aug_all", [128, NT, 32], F32)
    hb_all = k.psb("hb_all", [128, NT, 8], BF16)
    ov_off = k.ph_off
    cs = k.psb("cs", [128, T], F32)
    wuq = k.psb("wuq", [128, 3, 768], BF16); wuqR = k.psb("wuqR", [128, 3, 768], BF16)
    wukv = k.psb("wukv", [128, 2, 1024], BF16)
    stgw = k.psb("stgw", [128, 1024], F32)
    ALIAS = ["lnx", "hb", "lst", "lmv", "gbc", "bbc"] if with_p0 else []

    k.dma(wv[:], win[:, :, OV:OV + 512], w=["wv"], cast=True)
    k.dma(wcqf[:], win[:, :, OF:OF + 392], w=["wcqf"], cast=True)
    k.dma(wckv[:], win[:, :, OCKV:OCKV + 256], w=["wckv"], cast=True)
    k.dma(wkr[:], win[:, :, OKR:OKR + 32], w=["wkr"], cast=True)
    k.dma(fbias[:], bcast_row(C.fox_f_bias[l], 8), w=["fbias"])
    for kc in range(3):
        k.dma(gq[:, kc:kc + 1], C.mla_q_norm[l][kc * 128:(kc + 1) * 128].rearrange("(p o) -> p o", o=1), w=["gq"])
    for kc in range(2):
        k.dma(gq[:, 4 + kc:5 + kc], C.mla_kv_norm[l][kc * 128:(kc + 1) * 128].rearrange("(p o) -> p o", o=1), w=["gq"])
    for s_ in range(2):
        k.pool("memset", vst[s_][:, :, 0, 64:128], 1.0, w=[("vst", s_)])
        k.pool("memset", vst[s_][:, :, 1, 0:64], 1.0, w=[("vst", s_)])
    k.dve("tensor_scalar", out=wkrR[:, :, 0:16], in0=wkr[:, :, 16:32], scalar1=-1.0, scalar2=None, op0=ALU.mult, r=["wkr"], w=["wkrR"])
    k.dve("tensor_copy", out=wkrR[:, :, 16:32], in_=wkr[:, :, 0:16], r=["wkr"], w=["wkrR"])

    def mm_tile(i):
        s_ = i % 2
        tc = slice(i * 128, (i + 1) * 128)
        b0 = 3 * s_
        for (bk, w_, wn, ncol) in ((b0, wv, "wv", 512), (b0 + 1, wcqf, "wcqf", 392), (b0 + 2, wckv, "wckv", 256)):
            for kc in range(KC):
                k.pe("matmul", C.bank[bk][:, 0:ncol], lhsT=C.hT[:, kc, tc], rhs=w_[:, kc, :], start=(kc == 0), stop=(kc == KC - 1),
                     r=[("hT", i), wn], w=[("bank", bk)])

    def chain_tile(i):
        s_ = i % 2
        tc = slice(i * 128, (i + 1) * 128)
        b0 = 3 * s_
        bv, ba, bb = C.bank[b0], C.bank[b0 + 1], C.bank[b0 + 2]
        m = sm[s_]; M = ("sm", s_)
        k.act("activation", out=junk[:, 0:384], in_=ba[:, 8:392], func=AF.Square, scale=384 ** -0.5, accum_out=m[:, 0:1], r=[("bank", b0 + 1)], w=[M, "junk"])
        k.act("activation", out=junk[:, 0:256], in_=bb[:, 0:256], func=AF.Square, scale=256 ** -0.5, accum_out=m[:, 1:2], r=[("bank", b0 + 2)], w=[M, "junk"])
        evac_v(k, bv[:, 0:512], vst[s_], s_, ("bank", b0), C.Vf, i)
        k.dve("tensor_tensor", out=z_all[:, i, :], in0=ba[:, 0:8], in1=fbias[:], op=ALU.add, r=[("bank", b0 + 1), "fbias"], w=[("z_all", i)])
        k.pool("tensor_scalar", out=m[:, 2:4], in0=m[:, 0:2], scalar1=1.0, scalar2=RMS_EPS, op0=ALU.mult, op1=ALU.add, r=[M], w=[M])
        k.pool("tensor_tensor", out=m[:, 6:8], in0=m[:, 2:4], in1=C.nhalf[:, 0:2], op=ALU.pow, r=[M, "nhalf"], w=[M])
        cq = cqn[s_]; CQ = ("cqn", s_)
        k.dve("tensor_scalar", out=cq[:, 0:384], in0=ba[:, 8:392], scalar1=m[:, 6:7], scalar2=None, op0=ALU.mult, r=[M, ("bank", b0 + 1)], w=[CQ])
        k.dve("tensor_scalar", out=cq[:, 384:640], in0=bb[:, 0:256], scalar1=m[:, 7:8], scalar2=None, op0=ALU.mult, r=[M, ("bank", b0 + 2)], w=[CQ])
        pst = C.bank[6 + s_][:].bitcast(BF16)
        for j in range(5):
            k.pe("transpose", out=pst[:, j * 128:(j + 1) * 128], in_=cq[:, j * 128:(j + 1) * 128], identity=C.identb[:],
                 r=[CQ, "identb"], w=[("bank", 6 + s_)])
        k.act("activation", out=cqnT[:, :, tc], in_=pst[:, 0:384].rearrange("p (k t) -> p k t", k=3), func=AF.Copy,
              r=[("bank", 6 + s_)], w=[("cqnT", i)])
        k.act("activation", out=ckvnT[:, :, tc], in_=pst[:, 384:640].rearrange("p (k t) -> p k t", k=2), func=AF.Copy,
              r=[("bank", 6 + s_)], w=[("ckvnT", i)])

    pend_chain = []

    def stepA_push(i):
        mm_tile(i)
        if pend_chain:
            chain_tile(pend_chain.pop(0))
        pend_chain.append(i)

    if with_p0:
        end_off = k.ph_off
        k.ph_off = ov_off
        alloc_ln(k)
        assert k.ph_off <= end_off, "LN overlay too large"
        k.ph_off = end_off
        load_ln_params(k, C.ln_in_g, C.ln_in_b)
        prev_tiles = []
        for i0 in range(0, NT, NLN):
            tiles = list(range(i0, min(NT, i0 + NLN)))
            sl = ln_slots(k, len(tiles))
            items = []
            for s__, i in zip(sl, tiles):
                k.dma(C.lnx[s__][:], C.x[i * 128:(i + 1) * 128, :], w=[("lnx", s__)])
                items.append((s__, i, C.H[0][i * 128:(i + 1) * 128, :], ("H0", i), False))
            gen = ln_batch_gen(k, items)
            for i in prev_tiles:
                stepA_push(i)
                next(gen, None)
                next(gen, None)
            for _ in gen:
                pass
            prev_tiles = tiles
        ln_flush(k)
        for i in prev_tiles:
            stepA_push(i)
    else:
        for i in range(NT):
            stepA_push(i)
    chain_tile(pend_chain.pop(0))

    NF = NT * 8
    zf = z_all[:].rearrange("p i h -> p (i h)")
    lff = lf_all[:].rearrange("p i h -> p (i h)")
    k.act("activation", out=lff, in_=zf, func=AF.Exp, scale=-1.0, r=["z_all"], w=["lf_all"])
    k.act("activation", out=lff, in_=lff, func=AF.Ln, bias=1.0, r=["lf_all"], w=["lf_all"])
    k.dve("tensor_scalar", out=lff, in0=lff, scalar1=-1.0, scalar2=None, op0=ALU.mult, r=["lf_all"], w=["lf_all"])
    k.dve("tensor_scalar", out=lf_all[:, 0, :], in0=lf_all[:, 0, :], scalar1=C.padcol[:, 0:1], scalar2=None, op0=ALU.mult, r=["lf_all", "padcol"], w=["lf_all"])
    k.pe("matmul", C.bank[0][:, 0:NF], lhsT=C.utf[:], rhs=lff, start=True, stop=True, r=["lf_all", "utf"], w=[("bank", 0)])
    k.pe("matmul", C.bank[1][:, 0:NF], lhsT=C.onesf[:], rhs=lff, start=True, stop=True, r=["lf_all", "onesf"], w=[("bank", 1)])
    tot = C.bank[1][:, 0:NF].rearrange("p (i h) -> p i h", h=8)
    k.dve("memset", carry[:, 0, :], 0.0, w=["carry"])
    for i in range(1, NT):
        k.dve("tensor_tensor", out=carry[:, i, :], in0=carry[:, i - 1, :], in1=tot[:, i - 1, :], op=ALU.add, r=["carry", ("bank", 1)], w=["carry"])
    au = aug_all
    c3 = C.bank[0][:, 0:NF].rearrange("p (i h) -> p i h", h=8)
    k.dve("tensor_tensor", out=au[:, :, 0:8], in0=c3, in1=carry[:], op=ALU.add, r=[("bank", 0), "carry"], w=["aug_all"])
    k.dve("tensor_scalar", out=lf_all[:], in0=au[:, :, 0:8], scalar1=-1.0, scalar2=None, op0=ALU.mult, r=["aug_all"], w=["lf_all"])
    k.dve("tensor_copy", out=hb_all[:], in_=lf_all[:], r=["lf_all"], w=["hb_all"])
    k.dve("tensor_copy", out=au[:, :, 8:16], in_=hb_all[:], r=["hb_all"], w=["aug_all"])
    k.dve("tensor_tensor", out=lf_all[:], in0=lf_all[:], in1=au[:, :, 8:16], op=ALU.subtract, r=["lf_all", "aug_all"], w=["lf_all"])
    k.dve("tensor_copy", out=hb_all[:], in_=lf_all[:], r=["lf_all"], w=["hb_all"])
    k.dve("tensor_copy", out=au[:, :, 16:24], in_=hb_all[:], r=["hb_all"], w=["aug_all"])
    k.dve("tensor_tensor", out=au[:, :, 24:32], in0=lf_all[:], in1=au[:, :, 16:24], op=ALU.subtract, r=["lf_all", "aug_all"], w=["aug_all"])
    for i0 in range(0, NT, 4):
        n4 = min(4, NT - i0)
        bk = 2 + (i0 // 4) % 2
        for j in range(n4):
            k.pe("transpose", out=C.bank[bk][0:32, j * 128:(j + 1) * 128], in_=au[:, i0 + j, :], identity=C.identf[:],
                 r=["aug_all", "identf"], w=[("bank", bk)])
        k.dve("tensor_copy", out=augT[:, i0 * 128:(i0 + n4) * 128], in_=C.bank[bk][0:32, 0:n4 * 128], r=[("bank", bk)], w=["augT"])
    k.dma(C.QTf[:, 67, :], augT[0:8, :], r=["augT"], w=[("QTf", "c")])
    k.dma(C.KTf[:, 64, :], augT[8:16, :], r=["augT"], w=[("KTf", "hi")])
    k.dma(C.KTf[:, 65, :], augT[16:24, :], r=["augT"], w=[("KTf", "mid")])
    k.dma(C.KTf[:, 66, :], augT[24:32, :], r=["augT"], w=[("KTf", "lo")])

    k.dma(cs[:], C.c_cs, w=["cs"] + ALIAS)
    cnt = 0
    k.dma(wt[0][:], win[:, :, 0:128], w=[("wt", 0)], cast=True)
    for c in range(8):
        s_ = c % 2
        if c + 1 < 8:
            k.dma(wt[(c + 1) % 2][:], win[:, :, (c + 1) * 128:(c + 2) * 128], w=[("wt", (c + 1) % 2)], cast=True)
        dst = C.QTf if c < 4 else C.KTf
        dn = "QTf" if c < 4 else "KTf"
        h0 = 2 * (c % 4)
        for gi, (g0, n) in enumerate(GROUPS):
            bk = cnt % 6
            st_ = stg[cnt % 3]; SG = ("stg", cnt % 3)
            cnt += 1
            for kc in range(KC):
                k.pe("matmul", C.bank[bk][:, 0:n], lhsT=wt[s_][:, kc, :], rhs=C.hT[:, kc, g0:g0 + n], start=(kc == 0), stop=(kc == KC - 1),
                     r=[("wt", s_), "hT"], w=[("bank", bk)])
            if c < 4:
                k.act("activation", out=st_[:, 0:n], in_=C.bank[bk][:, 0:n], func=AF.Copy, scale=0.125, r=[("bank", bk)], w=[SG])
            else:
                k.dve("tensor_copy", out=st_[:, 0:n], in_=C.bank[bk][:, 0:n], r=[("bank", bk)], w=[SG])
            k.dma(dst[h0, 0:64, g0:g0 + n], st_[0:64, 0:n], r=[SG], w=[(dn, (h0, gi))])
            k.dma(dst[h0 + 1, 0:64, g0:g0 + n], st_[64:128, 0:n], r=[SG], w=[(dn, (h0 + 1, gi))])
    for gi, (g0, n) in enumerate(GROUPS):
        bA, bB = 6, 7
        for kc in range(KC):
            k.pe("matmul", C.bank[bA][0:32, 0:n], lhsT=wkr[:, kc, :], rhs=C.hT[:, kc, g0:g0 + n], start=(kc == 0), stop=(kc == KC - 1),
                 r=["wkr", "hT"], w=[("bank", bA)])
        for kc in range(KC):
            k.pe("matmul", C.bank[bB][0:32, 0:n], lhsT=wkrR[:, kc, :], rhs=C.hT[:, kc, g0:g0 + n], start=(kc == 0), stop=(kc == KC - 1),
                 r=["wkrR", "hT"], w=[("bank", bB)])
        st_ = stg[gi % 3]; SG = ("stg", gi % 3)
        k.dve("tensor_tensor", out=rt[0][0:32, 0:n], in0=C.bank[bA][0:32, 0:n], in1=cs[0:32, g0:g0 + n], op=ALU.mult, r=[("bank", bA), "cs"], w=[("rt", 0)])
        k.dve("tensor_tensor", out=rt[1][0:32, 0:n], in0=C.bank[bB][0:32, 0:n], in1=cs[32:64, g0:g0 + n], op=ALU.mult, r=[("bank", bB), "cs"], w=[("rt", 1)])
        k.dve("tensor_tensor", out=st_[0:32, 0:n], in0=rt[0][0:32, 0:n], in1=rt[1][0:32, 0:n], op=ALU.add, r=[("rt", 0), ("rt", 1)], w=[SG])
        for h in range(8):
            k.dma(C.KTm[h, 64:96, g0:g0 + n], st_[0:32, 0:n], r=[SG], w=[("KTm", (h, gi, "r"))])

    for kc in range(3):
        k.dma(stgw[:, 0:768], C.mla_w_uq[l][kc * 128:(kc + 1) * 128, :], w=["stgw"] + ALIAS)
        k.dve("tensor_scalar", out=wuq[:, kc, :], in0=stgw[:, 0:768], scalar1=gq[:, kc:kc + 1], scalar2=None, op0=ALU.mult, r=["stgw", "gq"], w=["wuq"] + ALIAS)
        k.dve("tensor_copy", out=wuqR[:, kc, :], in_=wuq[:, kc, :], r=["wuq"], w=["wuqR"] + ALIAS)
        wv_ = wuq[:, kc, :].rearrange("p (h d) -> p h d", d=96)
        wr_ = wuqR[:, kc, :].rearrange("p (h d) -> p h d", d=96)
        k.dve("tensor_scalar", out=wr_[:, :, 64:80], in0=wv_[:, :, 80:96], scalar1=-1.0, scalar2=None, op0=ALU.mult, r=["wuq"], w=["wuqR"])
        k.dve("tensor_copy", out=wr_[:, :, 80:96], in_=wv_[:, :, 64:80], r=["wuq"], w=["wuqR"])
    for kc in range(2):
        k.dma(stgw[:, 0:1024], C.mla_w_ukv[l][kc * 128:(kc + 1) * 128, :], w=["stgw"])
        k.dve("tensor_scalar", out=wukv[:, kc, :], in0=stgw[:, 0:1024], scalar1=gq[:, 4 + kc:5 + kc], scalar2=None, op0=ALU.mult, r=["stgw", "gq"], w=["wukv"] + ALIAS)
    cnt = 0
    for h in range(8):
        for gi, (g0, n) in enumerate(GROUPS):
            bA, bB, bC = 3 * (cnt % 2), 3 * (cnt % 2) + 1, 3 * (cnt % 2) + 2
            qs = stg[cnt % 3]; QS = ("stg", cnt % 3)
            cnt += 1
            for kc in range(3):
                k.pe("matmul", C.bank[bA][0:96, 0:n], lhsT=wuq[:, kc, h * 96:(h + 1) * 96], rhs=cqnT[:, kc, g0:g0 + n], start=(kc == 0), stop=(kc == 2),
                     r=["wuq", "cqnT"], w=[("bank", bA)])
            for kc in range(3):
                k.pe("matmul", C.bank[bB][0:96, 0:n], lhsT=wuqR[:, kc, h * 96:(h + 1) * 96], rhs=cqnT[:, kc, g0:g0 + n], start=(kc == 0), stop=(kc == 2),
                     r=["wuqR", "cqnT"], w=[("bank", bB)])
            for kc in range(2):
                k.pe("matmul", C.bank[bC][0:64, 0:n], lhsT=wukv[:, kc, h * 64:(h + 1) * 64], rhs=ckvnT[:, kc, g0:g0 + n], start=(kc == 0), stop=(kc == 1),
                     r=["wukv", "ckvnT"], w=[("bank", bC)])
            k.act("activation", out=qs[0:64, 0:n], in_=C.bank[bA][0:64, 0:n], func=AF.Copy, r=[("bank", bA)], w=[QS])
            k.dve("tensor_tensor", out=rt[0][64:96, 0:n], in0=C.bank[bA][64:96, 0:n], in1=cs[64:96, g0:g0 + n], op=ALU.mult, r=[("bank", bA), "cs"], w=[("rt", 0)])
            k.dve("tensor_tensor", out=rt[1][64:96, 0:n], in0=C.bank[bB][64:96, 0:n], in1=cs[96:128, g0:g0 + n], op=ALU.mult, r=[("bank", bB), "cs"], w=[("rt", 1)])
            k.dve("tensor_tensor", out=qs[64:96, 0:n], in0=rt[0][64:96, 0:n], in1=rt[1][64:96, 0:n], op=ALU.add, r=[("rt", 0), ("rt", 1)], w=[QS])
            k.dma(C.QTm[h, :, g0:g0 + n], qs[0:96, 0:n], r=[QS], w=[("QTm", (h, gi))])
            ks = stg[cnt % 3]; KS = ("stg", cnt % 3)
            cnt += 1
            k.act("activation", out=ks[0:64, 0:n], in_=C.bank[bC][0:64, 0:n], func=AF.Copy, r=[("bank", bC)], w=[KS])
            k.dma(C.KTm[h, 0:64, g0:g0 + n], ks[0:64, 0:n], r=[KS], w=[("KTm", (h, gi, "n"))])
    for i in range(NT):
        s_ = i % 2
        tc = slice(i * 128, (i + 1) * 128)
        bk = 6 + s_
        for kc in range(2):
            k.pe("matmul", C.bank[bk][:, 0:512], lhsT=ckvnT[:, kc, tc], rhs=wukv[:, kc, 512:1024], start=(kc == 0), stop=(kc == 1),
                 r=["wukv", ("ckvnT", i)], w=[("bank", bk)])
        evac_v(k, C.bank[bk][:, 0:512], vst[s_], s_, ("bank", bk), C.Vm, i)


def attention(k, calls):
    C = k.C
    k.new_phase()
    DKM = max(c[3] for c in calls)
    qT = [k.psb("qT%d" % i, [DKM, 2, T], BF16) for i in range(2)]
    kT = [k.psb("kT%d" % i, [DKM, 2, T], BF16) for i in range(2)]
    vv = [k.psb("vv%d" % i, [128, 2, NT, 128], BF16) for i in range(2)]
    pb = [k.psb("pb%d" % i, [128, 2, 512], BF16) for i in range(2)]
    rr = [k.psb("rr%d" % i, [128, 512], F32) for i in range(2)]
    osb = [k.psb("osb%d" % i, [128, 512], F32) for i in range(2)]
    pb.append(k.psb("pb2", [128, 2, 512], BF16))
    ost = [k.psb("ost%d" % i, [128, 512], BF16) for i in range(6)]
    ocnt = [0]
    step = 0
    pend = []

    def drain(keep):
        while len(pend) > keep:
            f, post = pend.pop(0)
            f()
            if post is not None:
                post()

    NHP = 4 * len(calls)

    def load_pair(hpg, part=None):
        QT, KT, V, Dk, scale, OT, nm = calls[hpg // 4]
        s_ = hpg % 2
        n_ = 0
        for j in range(2):
            h = 2 * (hpg % 4) + j
            for (dst, src, rk, wk) in ((qT[s_][0:Dk, j, :], QT[h], ["QTf", "QTm"], ("qT", s_)),
                                       (kT[s_][0:Dk, j, :], KT[h], ["KTf", "KTm"], ("kT", s_)),
                                       (vv[s_][:, j, :, :], V[h], ["V"], ("vv", s_))):
                if part is None or part == n_:
                    k.dma(dst, src, r=rk, w=[wk])
                n_ += 1

    load_pair(0)
    for hpg in range(NHP):
        QT, KT, V, Dk, scale, OT, nm = calls[hpg // 4]
        hp = hpg % 4
        s_ = hpg % 2
        for gi, (g0, n) in enumerate(AGROUPS):
            if 2 <= gi <= 7 and hpg + 1 < NHP:
                load_pair(hpg + 1, gi - 2)
            qb0 = g0 // 128
            nkb = qb0 + n // 128
            ob = 6

            def make_pv(pkb, plo, pnn, psl, last, s_=s_):
                def f():
                    for j in range(2):
                        k.pe("matmul", C.bank[ob + j][:, plo:plo + pnn], lhsT=vv[s_][:, j, pkb, :], rhs=pb[psl][:, j, 0:pnn],
                             start=(pkb == 0), stop=last, r=[("vv", s_), ("pb", psl)], w=[("bank", ob + j)])
                return f

            def make_post(hp=hp, gi=gi, g0=g0, n=n, OT=OT, nm=nm):
                def post():
                    slots = []
                    for j in range(2):
                        r_ = rr[j]; RR = ("rr", j)
                        if j == 0:
                            po, pz = slice(0, 64), slice(64, 128)
                        else:
                            po, pz = slice(64, 128), slice(0, 64)
                        k.dve("tensor_copy", out=osb[j][po, 0:n], in_=C.bank[ob + j][po, 0:n], r=[("bank", ob + j)], w=[("osb", j)])
                        k.dve("tensor_copy", out=r_[po, 0:n], in_=C.bank[ob + j][pz, 0:n], r=[("bank", ob + j)], w=[RR])
                        slots.append((j, r_, RR, po))
                    oc = ocnt[0] % 3
                    ocnt[0] += 1
                    for (j, r_, RR, po) in slots:
                        h = 2 * hp + j
                        os_ = ost[2 * oc + j]; OS = ("ost", 2 * oc + j)
                        k.dve("reciprocal", out=r_[po, 0:n], in_=r_[po, 0:n], r=[RR], w=[RR])
                        k.dve("tensor_tensor", out=os_[po, 0:n], in0=osb[j][po, 0:n], in1=r_[po, 0:n], op=ALU.mult, r=[("osb", j), RR], w=[OS])
                        k.dma(OT[po, hp, g0:g0 + n], os_[po, 0:n], r=[OS], w=[(nm, (h, gi))])
                return post

            for kb in range(nkb):
                lo = max(0, kb * 128 - g0)
                nn = n - lo
                sl_ = step % 3
                step += 1
                diag = kb >= qb0
                for j in range(2):
                    bk = 2 * sl_ + j
                    k.pe("matmul", C.bank[bk][:, 0:nn], lhsT=kT[s_][0:Dk, j, kb * 128:(kb + 1) * 128], rhs=qT[s_][0:Dk, j, g0 + lo:g0 + n],
                         start=True, stop=not (diag or kb == 0), r=[("kT", s_), ("qT", s_)], w=[("bank", bk)])
                    if kb == 0:
                        k.pe("matmul", C.bank[bk][:, 0:nn], lhsT=C.identb[:], rhs=C.pmask[:, 0:nn], start=False, stop=not diag,
                             r=["identb", "pmask"], w=[("bank", bk)])
                    if diag:
                        cm = C.cmask0 if kb == 0 else C.cmask
                        k.pe("matmul", C.bank[bk][:, 0:128], lhsT=C.identb[:], rhs=cm[:], start=False, stop=True,
                             r=["identb", "cmask", "cmask0"], w=[("bank", bk)])
                k.act("activation", out=pb[sl_][:, :, 0:nn], in_=C.psall[:, 2 * sl_:2 * sl_ + 2, 0:nn], func=AF.Exp, scale=scale,
                      r=[("bank", 2 * sl_), ("bank", 2 * sl_ + 1)], w=[("pb", sl_)])
                last = (kb == nkb - 1)
                pend.append((make_pv(kb, lo, nn, sl_, last), make_post() if last else None))
                drain(2)
    drain(0)


def phase3(k, l, Hin, hin_name, Hout, hout_name):
    C = k.C
    k.new_phase()
    alloc_ln(k)
    load_ln_params(k, C.ln1_g[l], C.ln1_b[l])
    win = C.w_in[l].rearrange("(kc p) c -> p kc c", p=128)
    wgf = k.psb("wgf", [128, KC, D], BF16); wgm = k.psb("wgm", [128, KC, D], BF16)
    wof = k.psb("wof", [128, 4, D], BF16); wom = k.psb("wom", [128, 4, D], BF16)
    wo = k.psb("wo", [128, KC, D], BF16)
    otf = [k.psb("otf%d" % i, [128, 4, 512], BF16) for i in range(2)]
    otm = [k.psb("otm%d" % i, [128, 4, 512], BF16) for i in range(2)]
    mT = k.psb("mT", [128, KC, 512], BF16)
    sg = [k.psb("sg%d" % i, [128, 512], BF16) for i in range(4)]
    tt = [k.psb("tt%d" % i, [128, 512], F32) for i in range(4)]
    fwo = C.fox_w_o[l].rearrange("(m p) c -> p m c", p=128)
    mwo = C.mla_w_o[l].rearrange("(m p) c -> p m c", p=128)
    for c in range(KC):
        cs_ = slice(c * 128, (c + 1) * 128)
        k.dma(wgf[:, :, cs_], win[:, :, OGF + c * 128:OGF + (c + 1) * 128], w=[("wgf", c)], cast=True)
        k.dma(wgm[:, :, cs_], win[:, :, OGM + c * 128:OGM + (c + 1) * 128], w=[("wgm", c)], cast=True)
        k.dma(wof[:, :, cs_], fwo[:, :, cs_], w=[("wof", c)], cast=True)
        k.dma(wom[:, :, cs_], mwo[:, :, cs_], w=[("wom", c)], cast=True)
    k.dma(wo[:], C.w_out[l].rearrange("(kc p) c -> p kc c", p=128), w=["wo"], cast=True)
    cc = 0
    lngen = [None]

    def load_ot(gi_):
        g0_, n_ = GROUPS[gi_]
        k.dma(otf[gi_ % 2][:, :, 0:n_], C.OTf[:, :, g0_:g0_ + n_], r=["OTf"], w=[("otf", gi_ % 2)])
        k.dma(otm[gi_ % 2][:, :, 0:n_], C.OTm[:, :, g0_:g0_ + n_], r=["OTm"], w=[("otm", gi_ % 2)])

    for gi, (g0, n) in enumerate(GROUPS):
        s_ = gi % 2
        tiles = list(range(g0 // 128, (g0 + n) // 128))
        hk = [("hT", i) for i in tiles]
        if gi == 0:
            load_ot(0)
        if gi + 1 < len(GROUPS):
            load_ot(gi + 1)
        for c in range(KC):
            b0 = 4 * (cc % 2)
            q_ = 2 * (cc % 2)
            cc += 1
            cs_ = slice(c * 128, (c + 1) * 128)
            for kc in range(KC):
                k.pe("matmul", C.bank[b0][:, 0:n], lhsT=wgf[:, kc, cs_], rhs=C.hT[:, kc, g0:g0 + n], start=(kc == 0), stop=(kc == KC - 1),
                     r=[("wgf", c)] + hk, w=[("bank", b0)])
            for kc in range(KC):
                k.pe("matmul", C.bank[b0 + 1][:, 0:n], lhsT=wgm[:, kc, cs_], rhs=C.hT[:, kc, g0:g0 + n], start=(kc == 0), stop=(kc == KC - 1),
                     r=[("wgm", c)] + hk, w=[("bank", b0 + 1)])
            for m in range(4):
                k.pe("matmul", C.bank[b0 + 2][:, 0:n], lhsT=wof[:, m, cs_], rhs=otf[s_][:, m, 0:n], start=(m == 0), stop=(m == 3),
                     r=[("wof", c), ("otf", s_)], w=[("bank", b0 + 2)])
            for m in range(4):
                k.pe("matmul", C.bank[b0 + 3][:, 0:n], lhsT=wom[:, m, cs_], rhs=otm[s_][:, m, 0:n], start=(m == 0), stop=(m == 3),
                     r=[("wom", c), ("otm", s_)], w=[("bank", b0 + 3)])
            k.act("activation", out=sg[q_][:, 0:n], in_=C.bank[b0][:, 0:n], func=AF.Sigmoid, r=[("bank", b0)], w=[("sg", q_)])
            k.act("activation", out=sg[q_ + 1][:, 0:n], in_=C.bank[b0 + 1][:, 0:n], func=AF.Sigmoid, r=[("bank", b0 + 1)], w=[("sg", q_ + 1)])
            k.dve("tensor_tensor", out=tt[q_][:, 0:n], in0=C.bank[b0 + 2][:, 0:n], in1=sg[q_][:, 0:n], op=ALU.mult,
                  r=[("bank", b0 + 2), ("sg", q_)], w=[("tt", q_)])
            k.dve("tensor_tensor", out=tt[q_ + 1][:, 0:n], in0=C.bank[b0 + 3][:, 0:n], in1=sg[q_ + 1][:, 0:n], op=ALU.mult,
                  r=[("bank", b0 + 3), ("sg", q_ + 1)], w=[("tt", q_ + 1)])
            k.pool("tensor_tensor", out=mT[:, c, 0:n], in0=tt[q_][:, 0:n], in1=tt[q_ + 1][:, 0:n], op=ALU.add,
                   r=[("tt", q_), ("tt", q_ + 1)], w=[("mT", c)])
            if lngen[0] is not None:
                if next(lngen[0], "done") == "done":
                    lngen[0] = None
        if lngen[0] is not None:
            for _ in lngen[0]:
                pass
            lngen[0] = None
        sl = ln_slots(k, len(tiles))
        items = []
        for j, i in enumerate(tiles):
            ls = sl[j]
            k.dma(C.lnx[ls][:], Hin[i * 128:(i + 1) * 128, :], r=[(hin_name, i)], w=[("lnx", ls)])
            for half in range(2):
                bk = 2 * (j % 2) + half
                for kc in range(KC):
                    k.pe("matmul", C.bank[bk][:, 0:512], lhsT=mT[:, kc, j * 128:(j + 1) * 128], rhs=wo[:, kc, half * 512:(half + 1) * 512],
                         start=(kc == 0), stop=(kc == KC - 1), r=["mT", "wo"], w=[("bank", bk)])
                hs = slice(half * 512, (half + 1) * 512)
                k.dve("scalar_tensor_tensor", out=C.lnx[ls][:, hs], in0=C.lnx[ls][:, hs], scalar=ALPHA, in1=C.bank[bk][:, 0:512],
                      op0=ALU.mult, op1=ALU.add, r=[("lnx", ls), ("bank", bk)], w=[("lnx", ls)])
            items.append((ls, i, Hout[i * 128:(i + 1) * 128, :], (hout_name, i), False))
        lngen[0] = ln_batch_gen(k, items)
    for _ in lngen[0]:
        pass
    ln_flush(k)


def phase4(k, l, Hin, hin_name, Hout, hout_name, last):
    C = k.C
    k.new_phase()
    alloc_ln(k)
    load_ln_params(k, C.ln2_g[l], C.ln2_b[l])
    rw = k.psb("rw", [128, KC, NE], BF16)
    rb = k.psb("rb", [128, NE], F32)
    comb = k.psb("comb", [128, NT, NE], F32)
    wg = [k.psb("wg%d" % i, [128, KC, DE], BF16) for i in range(2)]
    wu = [k.psb("wu%d" % i, [128, KC, DE], BF16) for i in range(2)]
    wd = [k.psb("wd%d" % i, [128, 2, D], BF16) for i in range(2)]
    NSG = 11
    yacc = k.psb("yacc", [128, NSG, D], F32)
    hid = [k.psb("hid%d" % i, [128, 2, 512], BF16) for i in range(2)]
    sl = [k.psb("sl%d" % i, [128, 512], F32) for i in range(2)]
    k.dma(rw[:], C.router_w.rearrange("(kc p) e -> p kc e", p=128), w=["rw"], cast=True)
    k.dma(rb[:], bcast_row(C.router_b, NE), w=["rb"])
    NR = NT * NE
    ra = [k.psb("ra%d" % i, [128, NR], F32) for i in range(5)]
    rsm = k.psb("rsm", [128, NT * 4 * 4], F32)
    lg = C.psall[:, 6:8, :].rearrange("p b c -> p (b c)")
    for i in range(NT):
        tc = slice(i * 128, (i + 1) * 128)
        for kc in range(KC):
            k.pe("matmul", lg[:, i * NE:(i + 1) * NE], lhsT=C.hT[:, kc, tc], rhs=rw[:, kc, :], start=(kc == 0), stop=(kc == KC - 1),
                 r=[("hT", i), "rw"], w=[("bank", 6), ("bank", 7)])
    sc, b_, eq, b2, ge = [t[:] for t in ra]
    G4 = NT * 4
    m1 = rsm[:, 0:G4]; m2 = rsm[:, G4:2 * G4]; gs = rsm[:, 2 * G4:3 * G4]; gsel = rsm[:, 3 * G4:4 * G4]
    gmax = k.psb("gmax", [128, NT], F32); den = k.psb("den", [128, NT], F32)
    v3 = lambda ap: ap.rearrange("p (g e) -> p g e", e=4)
    bc4 = lambda ap: ap.unsqueeze(2).broadcast_to([128, G4, 4])
    k.act("activation", out=sc, in_=lg[:, 0:NR], func=AF.Sigmoid, r=[("bank", 6), ("bank", 7)], w=["ra0"])
    k.dve("tensor_tensor", out=b_.rearrange("p (i e) -> p i e", e=NE), in0=sc.rearrange("p (i e) -> p i e", e=NE),
          in1=rb[:].unsqueeze(1).broadcast_to([128, NT, NE]), op=ALU.add, r=["ra0", "rb"], w=["ra1"])
    k.dve("tensor_reduce", out=m1, in_=v3(b_), axis=AX.X, op=ALU.max, r=["ra1"], w=["rsm"])
    k.dve("tensor_tensor", out=v3(eq), in0=v3(b_), in1=bc4(m1), op=ALU.is_equal, r=["ra1", "rsm"], w=["ra2"])
    k.dve("scalar_tensor_tensor", out=b2, in0=eq, scalar=-1e9, in1=b_, op0=ALU.mult, op1=ALU.add, r=["ra2", "ra1"], w=["ra3"])
    k.dve("tensor_reduce", out=m2, in_=v3(b2), axis=AX.X, op=ALU.max, r=["ra3"], w=["rsm"])
    k.dve("tensor_tensor", out=gs, in0=m1, in1=m2, op=ALU.add, r=["rsm"], w=["rsm"])
    k.dve("tensor_reduce", out=gmax[:], in_=gs.rearrange("p (i g) -> p i g", g=4), axis=AX.X, op=ALU.max, r=["rsm"], w=["gmax"])
    k.dve("tensor_tensor", out=gsel.rearrange("p (i g) -> p i g", g=4), in0=gs.rearrange("p (i g) -> p i g", g=4),
          in1=gmax[:].unsqueeze(2).broadcast_to([128, NT, 4]), op=ALU.is_equal, r=["rsm", "gmax"], w=["rsm"])
    k.dve("tensor_tensor", out=v3(ge), in0=v3(b_), in1=bc4(m2), op=ALU.is_ge, r=["ra1", "rsm"], w=["ra4"])
    k.dve("tensor_tensor", out=v3(ge), in0=v3(ge), in1=bc4(gsel), op=ALU.mult, r=["ra4", "rsm"], w=["ra4"])
    k.dve("tensor_tensor", out=eq, in0=ge, in1=sc, op=ALU.mult, r=["ra4", "ra0"], w=["ra2"])
    k.dve("tensor_reduce", out=den[:], in_=eq.rearrange("p (i e) -> p i e", e=NE), axis=AX.X, op=ALU.add, r=["ra2"], w=["den"])
    k.dve("reciprocal", out=den[:], in_=den[:], r=["den"], w=["den"])
    k.dve("tensor_tensor", out=comb[:], in0=eq.rearrange("p (i e) -> p i e", e=NE),
          in1=den[:].unsqueeze(2).broadcast_to([128, NT, NE]), op=ALU.mult, r=["ra2", "den"], w=["comb"])
    subs = []
    for t0 in range(0, NT, NSG):
        nt_ = min(NSG, NT - t0)
        for e in range(NE):
            for j0 in range(0, nt_, 4):
                subs.append((t0, nt_, e, j0, min(4, nt_ - j0)))
    wslot = {}
    wcnt = [0]

    def load_w(t0, e):
        if (t0, e) in wslot:
            return
        ws = wcnt[0] % 2
        wcnt[0] += 1
        wslot[(t0, e)] = ws
        k.dma(wg[ws][:], C.w_gate[l, e].rearrange("(kc p) f -> p kc f", p=128), w=[("wg", ws)], cast=True)
        k.dma(wu[ws][:], C.w_up[l, e].rearrange("(kc p) f -> p kc f", p=128), w=[("wu", ws)], cast=True)
        k.dma(wd[ws][:], C.w_down[l, e].rearrange("(kc p) f -> p kc f", p=128), w=[("wd", ws)], cast=True)

    def gu_ops(si):
        t0, nt_, e, j0, ntile = subs[si]
        ws = wslot[(t0, e)]
        n = ntile * 128
        c0 = (t0 + j0) * 128
        hk = [("hT", i) for i in range(t0 + j0, t0 + j0 + ntile)]
        q_ = si % 2
        ops = []
        for ch in range(2):
            for (wt_, wn, bkk) in ((wg, "wg", ch), (wu, "wu", 2 + ch)):
                for kc in range(KC):
                    ops.append(lambda wt_=wt_, wn=wn, bkk=bkk, kc=kc, ch=ch: k.pe(
                        "matmul", C.bank[bkk][:, 0:n], lhsT=wt_[ws][:, kc, ch * 128:(ch + 1) * 128], rhs=C.hT[:, kc, c0:c0 + n],
                        start=(kc == 0), stop=(kc == KC - 1), r=[(wn, ws)] + hk, w=[("bank", bkk)]))

        def post(ch):
            k.act("activation", out=sl[ch][:, 0:n], in_=C.bank[ch][:, 0:n], func=AF.Silu, r=[("bank", ch)], w=[("sl", ch)])
            k.dve("tensor_tensor", out=hid[q_][:, ch, 0:n], in0=C.bank[2 + ch][:, 0:n], in1=sl[ch][:, 0:n], op=ALU.mult,
                  r=[("bank", 2 + ch), ("sl", ch)], w=[("hid", (q_, ch))])
        return ops, post

    ycnt = [0]

    def down_ops(si):
        t0, nt_, e, j0, ntile = subs[si]
        ws = wslot[(t0, e)]
        q_ = si % 2
        ops = []
        for j in range(ntile):
            tl = j0 + j
            for half in range(2):
                def f(j=j, tl=tl, half=half):
                    bk = 4 + ycnt[0] % 4
                    ycnt[0] += 1
                    for ch in range(2):
                        k.pe("matmul", C.bank[bk][:, 0:512], lhsT=hid[q_][:, ch, j * 128:(j + 1) * 128], rhs=wd[ws][:, ch, half * 512:(half + 1) * 512],
                             start=(ch == 0), stop=(ch == 1), r=[("hid", (q_, ch)), ("wd", ws)], w=[("bank", bk)])
                    ya = yacc[:, tl, half * 512:(half + 1) * 512]
                    cw = comb[:, t0 + tl, e:e + 1]
                    if e == 0:
                        k.dve("tensor_scalar", out=ya, in0=C.bank[bk][:, 0:512], scalar1=cw, scalar2=None, op0=ALU.mult,
                              r=[("bank", bk), ("comb", t0 + tl)], w=[("yacc", tl)])
                    else:
                        k.dve("scalar_tensor_tensor", out=ya, in0=C.bank[bk][:, 0:512], scalar=cw, in1=ya, op0=ALU.mult, op1=ALU.add,
                              r=[("bank", bk), ("comb", t0 + tl), ("yacc", tl)], w=[("yacc", tl)])
                ops.append(f)
        return ops

    def ln_tail(t0, nt_):
        tls = [tl for tl in range(nt_) if not (last and t0 + tl == 0)]
        for b0 in range(0, len(tls), NLN):
            bt = tls[b0:b0 + NLN]
            sl_ = ln_slots(k, len(bt))
            items = []
            for ls, tl in zip(sl_, bt):
                i = t0 + tl
                k.dma(C.lnx[ls][:], Hin[i * 128:(i + 1) * 128, :], r=[(hin_name, i)], w=[("lnx", ls)])
                k.dve("scalar_tensor_tensor", out=C.lnx[ls][:], in0=C.lnx[ls][:], scalar=ALPHA, in1=yacc[:, tl, :],
                      op0=ALU.mult, op1=ALU.add, r=[("lnx", ls), ("yacc", tl)], w=[("lnx", ls)])
                if last:
                    items.append((ls, i, C.out[(i - 1) * 128:i * 128, :], ("out", i), True))
                else:
                    items.append((ls, i, Hout[i * 128:(i + 1) * 128, :], (hout_name, i), False))
            ln_batch(k, items)
        ln_flush(k)

    jobs = []

    def tail_async(t0, nt_):
        tls = [tl for tl in range(nt_) if not (last and t0 + tl == 0)]
        for tl in tls:
            i = t0 + tl
            k.dma(C.Rs[i * 128:(i + 1) * 128, :], yacc[:, tl, :], r=[("yacc", tl)], w=[("Rs", i)])
        for b0 in range(0, len(tls), 2):
            bt = tls[b0:b0 + 2]
            state = {}

            def start(bt=bt, state=state):
                ln_slots(k, 4)
                items = []
                for ls, tl in enumerate(bt):
                    i = t0 + tl
                    k.dma(C.lnx[ls][:], C.Rs[i * 128:(i + 1) * 128, :], r=[("Rs", i)], w=[("lnx", ls)])
                    k.dma(C.lnx[ls + 2][:], Hin[i * 128:(i + 1) * 128, :], r=[(hin_name, i)], w=[("lnx", ls + 2)])
                    k.dve("scalar_tensor_tensor", out=C.lnx[ls][:], in0=C.lnx[ls + 2][:], scalar=ALPHA, in1=C.lnx[ls][:],
                          op0=ALU.mult, op1=ALU.add, r=[("lnx", ls), ("lnx", ls + 2)], w=[("lnx", ls)])
                    if last:
                        items.append((ls, i, C.out[(i - 1) * 128:i * 128, :], ("out", i), True))
                    else:
                        items.append((ls, i, Hout[i * 128:(i + 1) * 128, :], (hout_name, i), False))
                state["gen"] = ln_batch_gen(k, items)
            jobs.append(start)
            for _ in range(7):
                jobs.append(lambda state=state: next(state["gen"], None))

    def job_step():
        if jobs:
            jobs.pop(0)()

    NS = len(subs)
    load_w(subs[0][0], subs[0][2])
    gops, gpost = gu_ops(0)
    for f in gops[0:16]:
        f()
    gpost(0)
    for f in gops[16:32]:
        f()
    gpost(1)
    for si in range(NS):
        t0, nt_, e, j0, ntile = subs[si]
        job_step()
        if j0 == 0:
            nxt = [x for x in subs[si + 1:] if (x[0], x[2]) != (t0, e)]
            if nxt:
                load_w(nxt[0][0], nxt[0][2])
        dops = down_ops(si)
        last_of_sg = (si + 1 == NS) or (subs[si + 1][0] != t0)
        if si + 1 < NS and not last_of_sg:
            gops, gpost = gu_ops(si + 1)
            gi_ = 0
            nd = len(dops)
            early = max(0, nd - 3)
            per = (32 + early - 1) // max(1, early) if early else 32
            di = 0
            while gi_ < 32:
                stop_at = min(32, gi_ + per)
                while gi_ < stop_at:
                    gops[gi_]()
                    gi_ += 1
                    if gi_ == 16:
                        gpost(0)
                if di < early:
                    dops[di]()
                    di += 1
            gpost(1)
            while di < nd:
                dops[di]()
                di += 1
        else:
            for f in dops:
                f()
            if last_of_sg:
                if si + 1 < NS:
                    tail_async(t0, nt_)
                else:
                    while jobs:
                        job_step()
                    ln_tail(t0, nt_)
            if si + 1 < NS:
                gops, gpost = gu_ops(si + 1)
                for f in gops[0:16]:
                    f()
                gpost(0)
                for f in gops[16:32]:
                    f()
                gpost(1)


def build(upto=99, debug=False):
    k = K(upto, debug)
    nc = k.nc
    C = setup(k)
    if upto >= 1:
        phase1(k, 0, with_p0=True)
    else:
        phase0(k)
    if debug and upto == 1:
        for nm, ap in (("QTf", C.QTf), ("KTf", C.KTf), ("QTm", C.QTm), ("KTm", C.KTm), ("Vf", C.Vf), ("Vm", C.Vm)):
            o = k.dram_out("dbg_" + nm, list(ap.shape), BF16)
            k.dma(o, ap, r=[nm, "V"], w=["dbg_" + nm], final=True)
    if upto >= 2:
        attention(k, [(C.QTf, C.KTf, C.Vf, 68, 1.0, C.OTf, "OTf"), (C.QTm, C.KTm, C.Vm, 96, 96 ** -0.5, C.OTm, "OTm")])
    if debug and upto == 2:
        for nm, ap in (("OTf", C.OTf), ("OTm", C.OTm)):
            o = k.dram_out("dbg_" + nm, list(ap.shape), BF16)
            k.dma(o, ap, r=[nm], w=["dbg_" + nm], final=True)
    if upto >= 3:
        phase3(k, 0, C.H[0], "H0", C.H[1], "H1")
    if debug and upto == 3:
        o = k.dram_out("dbg_H1", [T, D], F32)
        k.dma(o, C.H[1], r=["H1"], w=["dbg_H1"], final=True)
    if upto >= 4:
        phase4(k, 0, C.H[1], "H1", C.H[0], "H0", False)
    if debug and upto == 4:
        o = k.dram_out("dbg_H2", [T, D], F32)
        k.dma(o, C.H[0], r=["H0"], w=["dbg_H2"], final=True)
    if upto >= 5:
        phase1(k, 1)
        attention(k, [(C.QTf, C.KTf, C.Vf, 68, 1.0, C.OTf, "OTf"), (C.QTm, C.KTm, C.Vm, 96, 96 ** -0.5, C.OTm, "OTm")])
        phase3(k, 1, C.H[0], "H0", C.H[1], "H1")
        phase4(k, 1, C.H[1], "H1", None, None, True)
    if debug and upto == 0:
        C.dbg_hT = k.dram_out("dbg_hT", [128, KC, T], BF16)
        k.dma(C.dbg_hT, C.hT[:], r=["hT"], w=["dbg_hT"], final=True)
        C.dbg_H = k.dram_out("dbg_H", [T, D], F32)
        k.dma(C.dbg_H, C.H[0], r=["H0"], w=["dbg_H"], final=True)
    finish(k)
    return k


def finish(k):
    nc = k.nc
    R = k.R
    sems = {e: nc.alloc_semaphore("sem_" + e) for e in R.ENGS}
    dsems = {}
    for e in ("sp", "pool"):
        for j in range(R.NQ):
            dsems[(e, j)] = nc.alloc_semaphore("dsem_%s_%d" % (e, j))
    R.replay(nc, sems, dsems)


def host_consts():
    c = {}
    c["c_ident"] = np.eye(128, dtype=np.float32)
    kk = np.arange(128)[:, None]; qq = np.arange(128)[None, :]
    c["c_cmask"] = np.where(kk > qq, NEG, 0.0).astype(np.float32)
    cm0 = np.where(kk > qq, NEG, 0.0).astype(np.float32)
    cm0[NPAD, :NPAD] = 0.0
    c["c_cmask0"] = cm0
    c["c_pmask"] = np.where(np.broadcast_to(kk, (128, 512)) < NPAD, NEG, 0.0).astype(np.float32)
    c["c_ut"] = (kk <= qq).astype(np.float32)
    c["c_padcol"] = (np.arange(128) >= NPAD).astype(np.float32)[:, None]
    inv = (10000.0 ** (-np.arange(16, dtype=np.float32) / 16)).astype(np.float32)
    pos = (np.arange(T) - NPAD).astype(np.float32)
    ang = pos[None, :] * inv[np.arange(128) % 16][:, None]
    cs = np.cos(ang).astype(np.float32)
    sn = np.sin(ang).astype(np.float32)
    sel_sin = ((np.arange(128) // 32) % 2 == 1)[:, None]
    c["c_cs"] = np.where(sel_sin, sn, cs).astype(np.float32)
    c["c_ones8"] = np.ones((8, T), np.float32)
    sel = np.zeros((48, NE, 128), np.float32)
    for e in range(NE):
        sel[e, e, :] = 1.0
        sel[32 + e, e, :] = 1.0
    c["c_sel"] = sel.reshape(48, NE * 128)
    return c


def make_in_maps(inputs):
    x = np.asarray(inputs["x"], np.float32)
    meta = np.asarray(inputs["meta_tokens"], np.float32)
    shared = {}
    for name in ["ln_in_g", "ln_in_b", "w_in", "fox_f_bias", "fox_w_o", "mla_q_norm", "mla_w_uq",
                 "mla_kv_norm", "mla_w_o", "w_out", "ln1_g", "ln1_b", "router_w", "router_b",
                 "w_gate", "w_up", "w_down", "ln2_g", "ln2_b"]:
        shared[name] = np.ascontiguousarray(np.asarray(inputs[name], np.float32))
    wkv = np.asarray(inputs["mla_w_ukv"], np.float32).reshape(DEPTH, 256, 8, 2, 64)
    shared["mla_w_ukv"] = np.ascontiguousarray(
        np.concatenate([wkv[:, :, :, 0, :].reshape(DEPTH, 256, 512), wkv[:, :, :, 1, :].reshape(DEPTH, 256, 512)], axis=2))
    shared.update(host_consts())
    maps = []
    for b in range(8):
        xp = np.zeros((T, D), np.float32)
        xp[NPAD:128] = meta
        xp[128:] = x[b]
        m = dict(shared)
        m["x"] = xp
        maps.append(m)
    return maps


_CACHE = {}


def kernel(**inputs):
    if "k" not in _CACHE:
        _CACHE["k"] = build()
    k = _CACHE["k"]
    maps = make_in_maps(inputs)
    maps = [{n: m[n] for n in k.inp} for m in maps]
    res = run_bass_kernel_spmd(k.nc, maps, core_ids=list(range(8)))
    out = np.stack([np.asarray(r["out"], np.float32) for r in res.results], axis=0)
    return out


def check_debug(r, R, relerr):
    f = lambda a: np.asarray(a).astype(np.float32)
    if "dbg_H" in r:
        print("H0 err", relerr(f(r["dbg_H"])[NPAD:], R["h0"]))
    if "dbg_hT" in r:
        hT = f(r["dbg_hT"]).transpose(2, 1, 0).reshape(T, D)
        print("hT err (vs h0)", relerr(hT[NPAD:], R["h0"]))
    L0 = R["L0"]
    if "dbg_H1" in r:
        print("H1 err", relerr(f(r["dbg_H1"])[NPAD:], L0["h1"]), np.isfinite(f(r["dbg_H1"])).all())
    if "dbg_H2" in r:
        print("H2 err", relerr(f(r["dbg_H2"])[NPAD:], L0["h2"]), np.isfinite(f(r["dbg_H2"])).all())
    if "out" in r and "dbg_H" not in r and "dbg_H1" not in r and "dbg_H2" not in r and "dbg_QTf" not in r and "dbg_OTf" not in r:
        print("OUT err", relerr(f(r["out"]), R["L1"]["h2"][NMETA:]), np.isfinite(f(r["out"])).all())
    for nm, rn in (("dbg_OTf", "ofox"), ("dbg_OTm", "omla")):
        if nm in r:
            o = f(r[nm])
            oh = np.stack([o[(hh % 2) * 64:(hh % 2) * 64 + 64, hh // 2, :] for hh in range(8)], 0)
            print(nm, "err", relerr(oh[:, :, NPAD:].transpose(0, 2, 1), L0[rn]), "finite", np.isfinite(o).all())
    if "dbg_QTf" in r:
        q = f(r["dbg_QTf"]); kk = f(r["dbg_KTf"])
        print("qf err", relerr(q[:, 0:64, NPAD:].transpose(0, 2, 1) * 8, L0["qf"]))
        print("kf err", relerr(kk[:, 0:64, NPAD:].transpose(0, 2, 1), L0["kf"]))
        print("q ones rows", q[:, 64:67].min(), q[:, 64:67].max(), "k ones row", kk[:, 67].min(), kk[:, 67].max())
        print("c row err", relerr(q[:, 67, NPAD:], L0["dec"]), np.abs(q[:, 67, :NPAD]).max())
        nc_ = kk[:, 64, NPAD:] + kk[:, 65, NPAD:] + kk[:, 66, NPAD:]
        print("-c split err", relerr(-nc_, L0["dec"]), np.abs(nc_ + L0["dec"]).max())
        v = f(r["dbg_Vf"])
        v = v.transpose(0, 2, 1, 3).reshape(8, T, 128)
        ve = np.concatenate([v[0::2, :, 0:64], v[1::2, :, 64:128]], 0)[[0, 4, 1, 5, 2, 6, 3, 7]]
        print("vf err", relerr(ve[:, NPAD:], L0["vf"]))
        on = np.concatenate([v[0::2, :, 64:128], v[1::2, :, 0:64]], 0)
        print("v ones", on.min(), on.max())
        q = f(r["dbg_QTm"]); kk = f(r["dbg_KTm"])
        print("qm err", relerr(q[:, :, NPAD:].transpose(0, 2, 1), L0["qm"]), relerr(q[:, 64:, NPAD:].transpose(0, 2, 1), L0["qm"][:, :, 64:]))
        print("km err", relerr(kk[:, :, NPAD:].transpose(0, 2, 1), L0["km"]), relerr(kk[:, 64:, NPAD:].transpose(0, 2, 1), L0["km"][:, :, 64:]))
        v = f(r["dbg_Vm"]).transpose(0, 2, 1, 3).reshape(8, T, 128)
        ve = np.concatenate([v[0::2, :, 0:64], v[1::2, :, 64:128]], 0)[[0, 4, 1, 5, 2, 6, 3, 7]]
        print("vm err", relerr(ve[:, NPAD:], L0["vm"]))
```
